# Optimizing a Trainium2 kernel written in Bass

```python
import math
import jax
import jax.numpy as jnp
from jax import lax
import numpy as np

D_MODEL = 1024
BATCH = 16
SEQ = 2048
DEPTH = 2

MEM_LEN = 256

GROUP_WIDTH = D_MODEL // 4
MIX_WIDTH = 4 * GROUP_WIDTH

HG_HEADS = 4
HG_DK = GROUP_WIDTH // HG_HEADS
HG_DV = GROUP_WIDTH // HG_HEADS
HG_LB_FLOOR = 1e-20
GLA_HEADS = 4
GLA_DK = GROUP_WIDTH // (2 * GLA_HEADS)
GLA_DV = GROUP_WIDTH // GLA_HEADS
GLA_RANK = 16
GLA_TAU = 16.0
DA_HEADS = 4
DA_HD = GROUP_WIDTH // DA_HEADS
DA_ROT = DA_HD // 4
ROPE_THETA = 500000.0
DA_PATTERNS = ((128, 1), (512, 4), (2048, 16))
MASK_VALUE = -1e30
CONV_CH = GROUP_WIDTH
CONV_K = 31
X_HEADS = 4
X_HD = D_MODEL // X_HEADS
D_FF = ((int(8 * D_MODEL / 3) + 255) // 256) * 256

CHUNK = 64
EPS = 1e-6

IN_SPLITS = (
    HG_HEADS * HG_DK,
    HG_HEADS * HG_DK,
    HG_HEADS * HG_DV,
    HG_HEADS * HG_DV,
    GLA_HEADS * GLA_DK,
    GLA_HEADS * GLA_DK,
    GLA_HEADS * GLA_DV,
    GLA_RANK,
    GLA_HEADS * GLA_DV,
    DA_HEADS * DA_HD,
    DA_HEADS * DA_HD,
    DA_HEADS * DA_HD,
    CONV_CH,
    CONV_CH,
)
IN_WIDTH = sum(IN_SPLITS)

kernel_name = "hymba_style_hybrid_hgrn2_gla_dilated_conformer"


def rms_norm(x, g):
    xf = x.astype(jnp.float32)
    y = xf * lax.rsqrt(jnp.mean(xf * xf, axis=-1, keepdims=True) + EPS)
    return (y * g.astype(jnp.float32)).astype(x.dtype)


def swiglu_ffn(x, w_up, w_down):
    gate, up = jnp.split(x @ w_up, 2, axis=-1)
    return (jax.nn.silu(gate) * up) @ w_down


def chunked_gated_linear_attention(q, k, v, log_g):
    B, S, H, DK = q.shape
    DV = v.shape[-1]
    n = S // CHUNK

    def to_chunks(t):
        return t.astype(jnp.float32).reshape(B, n, CHUNK, H, t.shape[-1]).transpose(1, 0, 3, 2, 4)

    causal = jnp.tril(jnp.ones((CHUNK, CHUNK), dtype=bool))[:, :, None]

    def step(state, inp):
        qc, kc, vc, gc = inp
        b = jnp.cumsum(gc, axis=2)
        rel = b[:, :, :, None, :] - b[:, :, None, :, :]
        decay = jnp.where(causal, jnp.exp(jnp.where(causal, rel, 0.0)), 0.0)
        scores = jnp.einsum('bhtk,bhsk,bhtsk->bhts', qc, kc, decay)
        o = (jnp.einsum('bhts,bhsv->bhtv', scores, vc)
             + jnp.einsum('bhtk,bhkv->bhtv', qc * jnp.exp(b), state))
        b_last = b[:, :, -1:, :]
        state = (state * jnp.exp(b_last[:, :, 0, :, None])
                 + jnp.einsum('bhsk,bhsv->bhkv', kc * jnp.exp(b_last - b), vc))
        return state, o

    state0 = jnp.zeros((B, H, DK, DV), jnp.float32)
    _, o = lax.scan(step, state0, (to_chunks(q), to_chunks(k), to_chunks(v), to_chunks(log_g)))
    return o.transpose(1, 0, 3, 2, 4).reshape(B, S, H, DV).astype(v.dtype)


def hgrn2_mixer(q, f_pre, i, g, lb, out_gain):
    B, S, _ = q.shape
    q = q.reshape(B, S, HG_HEADS, HG_DK)
    z = f_pre.reshape(B, S, HG_HEADS, HG_DK).astype(jnp.float32)
    lb = lb.reshape(HG_HEADS, HG_DK)
    log_f = jnp.logaddexp(jnp.log(jnp.maximum(lb, HG_LB_FLOOR)),
                          jnp.log1p(-lb) + jax.nn.log_sigmoid(z))
    k = -jnp.expm1(log_f)
    v = i.reshape(B, S, HG_HEADS, HG_DV)
    o = chunked_gated_linear_attention(q, k, v, log_f)
    o = rms_norm(o, out_gain.reshape(HG_HEADS, HG_DV)) * jax.nn.silu(g.reshape(B, S, HG_HEADS, HG_DV))
    return o.reshape(B, S, HG_HEADS * HG_DV)


def gla_mixer(q, k, v, lr, r, gate_w, gate_b, out_gain):
    B, S, _ = q.shape
    q = q.reshape(B, S, GLA_HEADS, GLA_DK) * (GLA_DK ** -0.5)
    k = k.reshape(B, S, GLA_HEADS, GLA_DK)
    v = v.reshape(B, S, GLA_HEADS, GLA_DV)
    log_a = jax.nn.log_sigmoid((lr @ gate_w + gate_b).astype(jnp.float32)) / GLA_TAU
    log_a = log_a.reshape(B, S, GLA_HEADS, GLA_DK)
    o = chunked_gated_linear_attention(q, k, v, log_a)
    o = rms_norm(o, out_gain.reshape(GLA_HEADS, GLA_DV)) * jax.nn.silu(r.reshape(B, S, GLA_HEADS, GLA_DV))
    return o.reshape(B, S, GLA_HEADS * GLA_DV)


def partial_rope(t, cos, sin):
    half = DA_ROT // 2
    t1 = t[..., :half]
    t2 = t[..., half:DA_ROT]
    return jnp.concatenate([t1 * cos - t2 * sin, t2 * cos + t1 * sin, t[..., DA_ROT:]], axis=-1)


def dilated_branch(q, k, v, dilation, steps):
    B, S, H, hd = q.shape
    L = S // dilation
    n_blk = -(-L // steps)
    Lp = n_blk * steps

    def to_blocks(t):
        t = t.reshape(B, L, dilation, H, hd).transpose(0, 2, 3, 1, 4)
        t = jnp.pad(t, ((0, 0), (0, 0), (0, 0), (0, Lp - L), (0, 0)))
        return t.reshape(B, dilation, H, n_blk, steps, hd)

    def with_prev(t):
        prev = jnp.pad(t, ((0, 0), (0, 0), (0, 0), (1, 0), (0, 0), (0, 0)))[:, :, :, :-1]
        return jnp.concatenate([prev, t], axis=4)

    qb = to_blocks(q)
    kb = with_prev(to_blocks(k))
    vb = with_prev(to_blocks(v))
    s = jnp.einsum('bdhnqe,bdhnke->bdhnqk', qb, kb).astype(jnp.float32) * (hd ** -0.5)
    qi = jnp.arange(steps)[:, None]
    kj = jnp.arange(2 * steps)[None, :]
    steps_back = qi + steps - kj
    key_pos = jnp.arange(n_blk)[:, None, None] * steps - steps + kj
    valid = (steps_back >= 0) & (steps_back <= steps) & (key_pos >= 0)
    s = jnp.where(valid, s, MASK_VALUE)
    lse = jax.nn.logsumexp(s, axis=-1)
    p = jnp.exp(s - lse[..., None])
    o = jnp.einsum('bdhnqk,bdhnke->bdhnqe', p, vb.astype(jnp.float32))
    o = o.reshape(B, dilation, H, Lp, hd)[:, :, :, :L].transpose(0, 3, 1, 2, 4).reshape(B, S, H, hd)
    lse = lse.reshape(B, dilation, H, Lp)[..., :L].transpose(0, 3, 1, 2).reshape(B, S, H)
    return o, lse


def dilated_attention_mixer(q, k, v, cos, sin):
    B, S, _ = q.shape
    q = partial_rope(q.reshape(B, S, DA_HEADS, DA_HD), cos, sin)
    k = partial_rope(k.reshape(B, S, DA_HEADS, DA_HD), cos, sin)
    v = v.reshape(B, S, DA_HEADS, DA_HD)
    outs, lses = [], []
    for window, dilation in DA_PATTERNS:
        o, lse = dilated_branch(q, k, v, dilation, window // dilation)
        outs.append(o)
        lses.append(lse)
    weights = jax.nn.softmax(jnp.stack(lses, axis=0), axis=0)
    o = jnp.sum(weights[..., None] * jnp.stack(outs, axis=0), axis=0)
    return o.reshape(B, S, DA_HEADS * DA_HD).astype(q.dtype)


def conformer_conv_mixer(a, gate, conv_w, conv_b, ln_g, ln_b):
    u = a * jax.nn.sigmoid(gate)
    y = lax.conv_general_dilated(
        u, conv_w[:, None, :].astype(u.dtype), window_strides=(1,),
        padding=[(CONV_K - 1, 0)], dimension_numbers=('NWC', 'WIO', 'NWC'),
        feature_group_count=CONV_CH) + conv_b
    yf = y.astype(jnp.float32)
    mu = jnp.mean(yf, axis=-1, keepdims=True)
    var = jnp.mean(jnp.square(yf - mu), axis=-1, keepdims=True)
    yn = (yf - mu) * lax.rsqrt(var + EPS) * ln_g.astype(jnp.float32) + ln_b.astype(jnp.float32)
    return jax.nn.silu(yn).astype(a.dtype)


def memory_cross_attention(h, m, wq, wkv, wo):
    B, S, _ = h.shape
    M = m.shape[1]
    q = (h @ wq).reshape(B, S, X_HEADS, X_HD)
    k, v = jnp.split(m @ wkv, 2, axis=-1)
    k = k.reshape(B, M, X_HEADS, X_HD)
    v = v.reshape(B, M, X_HEADS, X_HD)
    s = jnp.einsum('bshd,bmhd->bhsm', q, k).astype(jnp.float32) * (X_HD ** -0.5)
    p = jax.nn.softmax(s, axis=-1)
    o = jnp.einsum('bhsm,bmhd->bshd', p.astype(v.dtype), v).reshape(B, S, D_MODEL)
    return o @ wo


def setup_inputs(seed: int = 0) -> dict:
    key = jax.random.key(seed)
    ks = iter(jax.random.split(key, 40))

    def nrm(shape, scale):
        return jax.random.normal(next(ks), shape, jnp.float32) * scale

    def gain(shape):
        return 1.0 + 0.02 * jax.random.normal(next(ks), shape, jnp.float32)

    offsets = jax.random.randint(next(ks), (BATCH, 1), 0, 1024, dtype=jnp.int32)
    positions = offsets + jnp.arange(SEQ, dtype=jnp.int32)[None, :]
    return {
        "x": nrm((BATCH, SEQ, D_MODEL), 1.0),
        "mem": nrm((BATCH, MEM_LEN, D_MODEL), 1.0),
        "positions": positions,
        "hgrn_lb_logits": nrm((DEPTH, HG_HEADS * HG_DK), 0.5),
        "ffn1_norm": gain((DEPTH, D_MODEL)),
        "ffn1_w_up": nrm((DEPTH, D_MODEL, 2 * D_FF), D_MODEL ** -0.5),
        "ffn1_w_down": nrm((DEPTH, D_FF, D_MODEL), D_FF ** -0.5),
        "mix_norm": gain((DEPTH, D_MODEL)),
        "w_in": nrm((DEPTH, D_MODEL, IN_WIDTH), D_MODEL ** -0.5),
        "hgrn_out_norm": gain((DEPTH, HG_HEADS * HG_DV)),
        "gla_gate_w": nrm((DEPTH, GLA_RANK, GLA_HEADS * GLA_DK), GLA_RANK ** -0.5),
        "gla_gate_b": nrm((DEPTH, GLA_HEADS * GLA_DK), 0.1),
        "gla_out_norm": gain((DEPTH, GLA_HEADS * GLA_DV)),
        "conv_w": nrm((DEPTH, CONV_K, CONV_CH), CONV_K ** -0.5),
        "conv_b": nrm((DEPTH, CONV_CH), 0.02),
        "conv_ln_g": gain((DEPTH, CONV_CH)),
        "conv_ln_b": nrm((DEPTH, CONV_CH), 0.02),
        "w_out": nrm((DEPTH, MIX_WIDTH, D_MODEL), MIX_WIDTH ** -0.5),
        "cross_norm": gain((DEPTH, D_MODEL)),
        "mem_norm": gain((DEPTH, D_MODEL)),
        "cross_wq": nrm((DEPTH, D_MODEL, D_MODEL), D_MODEL ** -0.5),
        "cross_wkv": nrm((DEPTH, D_MODEL, 2 * D_MODEL), D_MODEL ** -0.5),
        "cross_wo": nrm((DEPTH, D_MODEL, D_MODEL), D_MODEL ** -0.5),
        "ffn2_norm": gain((DEPTH, D_MODEL)),
        "ffn2_w_up": nrm((DEPTH, D_MODEL, 2 * D_FF), D_MODEL ** -0.5),
        "ffn2_w_down": nrm((DEPTH, D_FF, D_MODEL), D_FF ** -0.5),
        "final_norm": gain((D_MODEL,)),
    }


def reference(x, mem, positions, hgrn_lb_logits, ffn1_norm, ffn1_w_up, ffn1_w_down,
              mix_norm, w_in, hgrn_out_norm, gla_gate_w, gla_gate_b, gla_out_norm,
              conv_w, conv_b, conv_ln_g, conv_ln_b, w_out, cross_norm, mem_norm,
              cross_wq, cross_wkv, cross_wo, ffn2_norm, ffn2_w_up, ffn2_w_down, final_norm):
    inv_freq = jnp.power(jnp.float32(ROPE_THETA),
                         -jnp.arange(0, DA_ROT, 2, dtype=jnp.float32) / DA_ROT)
    ang = positions.astype(jnp.float32)[..., None] * inv_freq
    cos = jnp.cos(ang)[:, :, None, :].astype(x.dtype)
    sin = jnp.sin(ang)[:, :, None, :].astype(x.dtype)

    p_lb = jax.nn.softmax(hgrn_lb_logits.astype(jnp.float32), axis=0)
    lower_bounds = jnp.cumsum(p_lb, axis=0) - p_lb[0:1]

    split_points = [int(i) for i in np.cumsum(IN_SPLITS)[:-1]]

    for l in range(DEPTH):
        x = x + 0.5 * swiglu_ffn(rms_norm(x, ffn1_norm[l]), ffn1_w_up[l], ffn1_w_down[l])

        h = rms_norm(x, mix_norm[l])
        (hg_q, hg_f, hg_i, hg_g, gl_q, gl_k, gl_v, gl_lr, gl_r,
         da_q, da_k, da_v, cv_a, cv_g) = jnp.split(h @ w_in[l], split_points, axis=-1)

        o_a = hgrn2_mixer(hg_q, hg_f, hg_i, hg_g, lower_bounds[l], hgrn_out_norm[l])
        o_b = gla_mixer(gl_q, gl_k, gl_v, gl_lr, gl_r, gla_gate_w[l], gla_gate_b[l], gla_out_norm[l])
        o_c = dilated_attention_mixer(da_q, da_k, da_v, cos, sin)
        o_d = conformer_conv_mixer(cv_a, cv_g, conv_w[l], conv_b[l], conv_ln_g[l], conv_ln_b[l])
        x = x + jnp.concatenate([o_a, o_b, o_c, o_d], axis=-1) @ w_out[l]

        x = x + memory_cross_attention(rms_norm(x, cross_norm[l]), rms_norm(mem, mem_norm[l]),
                                       cross_wq[l], cross_wkv[l], cross_wo[l])

        x = x + 0.5 * swiglu_ffn(rms_norm(x, ffn2_norm[l]), ffn2_w_up[l], ffn2_w_down[l])

    return rms_norm(x, final_norm)
```

```python
import numpy as np
import ml_dtypes
from contextlib import ExitStack
import concourse.bass as bass
import concourse.mybir as mybir
from concourse.bass_utils import run_bass_kernel_spmd

F32 = mybir.dt.float32
BF16 = mybir.dt.bfloat16
I32 = mybir.dt.int32
AF = mybir.ActivationFunctionType
ALU = mybir.AluOpType

D = 1024
S = 2048
NB = 16
DEPTH = 2
MEM = 256
DFF = 2816
INW = 3088
EPS = 1e-6
NT = 4
TT = 512
NCORES = 8
SEQ_PER_CORE = NB // NCORES


class Node:
    __slots__ = ("id", "eng", "fns", "deps", "succ", "dur", "dma", "signal", "ndep", "ready", "finish", "tok", "vc")

    def __init__(self, id, eng, dma, signal):
        self.id, self.eng, self.dma, self.signal = id, eng, dma, signal
        self.fns = []
        self.deps = set()
        self.succ = []
        self.dur = 0.0
        self.ready = 0.0
        self.finish = None
        self.tok = None
        self.vc = None


def _nelem(ap):
    n = 1
    for d in ap.shape[1:]:
        n *= d
    return n


class Em:
    ENGS = ("pe", "act", "dve", "pool", "sp")
    WINDOW = 40
    LAT = 1.3

    def __init__(self, n_dma_sems=12):
        self.nodes = []
        self.lastw = {}
        self.readers = {}
        self.open_pe = None
        self.dma_sems = {"sp": [["dma_sp_%d" % i, 0, None] for i in range(n_dma_sems)]}
        self.ninstr = 0
        self.prog = {e: [] for e in self.ENGS}

    def sem_names(self):
        return list(self.ENGS) + [s[0] for s in self.dma_sems["sp"]]

    @staticmethod
    def _cost(eng, fn, dma):
        op, args, kw = fn
        try:
            if dma:
                return 2.0 + _nelem(kw["out"]) * 128 * 4 / 150e3
            n = _nelem(args[0])
            if eng == "pe":
                return 0.035 + n / 2800.0
            if eng == "act":
                return 0.2 + n / 1200.0
            if eng == "dve":
                return 0.1 + (2 * n if op == "tensor_tensor_scan" else n) / 960.0
            return 0.1 + n / 1700.0
        except Exception:
            return 0.5

    def emit(self, eng, fn, reads=(), writes=(), signal=True, dma=False):
        self.ninstr += 1
        if eng == "pe" and self.open_pe is not None:
            g = self.open_pe.id
            fwd = False
            for r in reads:
                t = self.lastw.get(r)
                if t is not None and t > g:
                    fwd = True
            for w in writes:
                t = self.lastw.get(w)
                if t is not None and t > g:
                    fwd = True
                for t in self.readers.get(w, ()):
                    if t > g:
                        fwd = True
            if fwd:
                self.open_pe = None
        if eng == "pe" and self.open_pe is not None:
            node = self.open_pe
        else:
            node = Node(len(self.nodes), eng, dma, True)
            self.nodes.append(node)
        node.fns.append(fn)
        node.dur += self._cost(eng, fn, dma)
        nid = node.id
        for r in reads:
            t = self.lastw.get(r)
            if t is not None and t != nid:
                node.deps.add(t)
        for w in writes:
            t = self.lastw.get(w)
            if t is not None and t != nid:
                node.deps.add(t)
            for t in self.readers.get(w, ()):
                if t != nid:
                    node.deps.add(t)
        for w in writes:
            self.lastw[w] = nid
            self.readers[w] = []
        for r in reads:
            lst = self.readers.setdefault(r, [])
            if not lst or lst[-1] != nid:
                lst.append(nid)
        if eng == "pe":
            self.open_pe = None if signal else node
        return nid

    def wait_all(self, eng, toks):
        node = Node(len(self.nodes), eng, False, False)
        self.nodes.append(node)
        node.deps = set(toks)
        node.dur = 0.05
        return node.id

    def finalize(self):
        assert self.open_pe is None
        nodes = self.nodes
        for n in nodes:
            assert all(d < n.id for d in n.deps), "forward dependency"
            n.ndep = len(n.deps)
            for d in n.deps:
                nodes[d].succ.append(n.id)
        queues = {e: [n.id for n in nodes if n.eng == e] for e in self.ENGS}
        heads = {e: 0 for e in self.ENGS}
        free = {e: 0.0 for e in self.ENGS}
        done = [False] * len(nodes)
        order = []
        remaining = len(nodes)
        W, LAT = self.WINDOW, self.LAT
        while remaining:
            best = None
            for e in self.ENGS:
                q = queues[e]
                i = heads[e]
                seen = 0
                fe = free[e]
                while i < len(q) and seen < W:
                    nid = q[i]
                    i += 1
                    if done[nid]:
                        continue
                    seen += 1
                    n = nodes[nid]
                    if n.ndep:
                        continue
                    st = n.ready if n.ready > fe else fe
                    if best is None or st < best[0] - 1e-9 or (st < best[0] + 0.05 and nid < best[1]):
                        best = (st, nid, e)
                    if n.ready <= fe:
                        break
            assert best is not None, "scheduler deadlock"
            st, nid, e = best
            n = nodes[nid]
            n.finish = st + n.dur
            free[e] = n.finish if not n.dma else st + 0.15
            done[nid] = True
            order.append(nid)
            remaining -= 1
            q = queues[e]
            while heads[e] < len(q) and done[q[heads[e]]]:
                heads[e] += 1
            for sid in n.succ:
                sn = nodes[sid]
                sn.ndep -= 1
                r = n.finish + (LAT if sn.eng != e else 0.06)
                if r > sn.ready:
                    sn.ready = r
        self.est_us = max(n.finish for n in nodes)
        cnt = {e: 0 for e in self.ENGS}
        clock = {e: {} for e in self.ENGS}
        rr = 0
        for nid in order:
            n = nodes[nid]
            eng = n.eng
            clk = clock[eng]
            waits = {}
            for d in n.deps:
                t = nodes[d].tok
                if eng == "pe" and t[0] == "pe":
                    continue
                if clk.get(t[0], 0) < t[1]:
                    if waits.get(t[0], 0) < t[1]:
                        waits[t[0]] = t[1]
            for d in n.deps:
                dn = nodes[d]
                if eng == "pe" and dn.tok[0] == "pe":
                    continue
                for k, v in dn.vc.items():
                    if clk.get(k, 0) < v:
                        clk[k] = v
                if clk.get(dn.tok[0], 0) < dn.tok[1]:
                    clk[dn.tok[0]] = dn.tok[1]
            inc = None
            if n.dma:
                lst = self.dma_sems["sp"]
                ent = lst[rr]
                rr = (rr + 1) % len(lst)
                name, cur = ent[0], ent[1]
                if cur > 0 and clk.get(name, 0) < cur:
                    waits[name] = max(waits.get(name, 0), cur)
                    clk[name] = cur
                ent[1] = cur + 16
                n.tok = (name, cur + 16)
                inc = (name, 16)
            elif n.fns:
                cnt[eng] += 1
                n.tok = (eng, cnt[eng])
                inc = (eng, 1)
            else:
                n.tok = (eng, cnt[eng])
            n.vc = dict(clk)
            self.prog[eng].append((tuple(waits.items()), n.fns, inc))

    def replay(self, eng, e, sems):
        for waits, fns, inc in self.prog[eng]:
            for name, val in waits:
                e.wait_ge(sems[name], val)
            ins = None
            for fn in fns:
                ins = getattr(e, fn[0])(*fn[1], **fn[2])
            if inc is not None and ins is not None:
                ins.then_inc(sems[inc[0]], inc[1])


class Pool:
    def __init__(self, name, aps):
        self.name = name
        self.aps = aps
        self.held = [False] * len(aps)
        self.rr = 0

    def get(self):
        n = len(self.aps)
        for k in range(n):
            i = (self.rr + k) % n
            if not self.held[i]:
                self.held[i] = True
                self.rr = (i + 1) % n
                return i
        raise RuntimeError("pool %s exhausted" % self.name)

    def get_pair(self):
        n = len(self.aps)
        for k in range(n):
            i = (self.rr + k) % n
            if i + 1 < n and not self.held[i] and not self.held[i + 1]:
                self.held[i] = self.held[i + 1] = True
                self.rr = (i + 2) % n
                return i, i + 1
        raise RuntimeError("pool %s: no free pair" % self.name)

    def free(self, i):
        assert self.held[i]
        self.held[i] = False

    def key(self, i):
        return (self.name, i)


CF_IDENT = 0
CF_MASKA = 128
CF_MASKB = 256
CF_HMA = 512
CF_HMB = 514
CF_ROPE = 518
CF_EPS = 522
CF_N = 523

CB_IDENT = 0
CB_ONES = 128
CB_BLK64 = 256
CB_CMASK = 384
CB_AMASK = 512
CB_SCANM = 1024
CB_N = 1536


def _consts():
    cf = np.zeros((128, CF_N), np.float32)
    p = np.arange(128)
    cf[:, CF_IDENT:CF_IDENT + 128] = np.eye(128, dtype=np.float32)
    cf[:, CF_MASKA:CF_MASKA + 128] = (p[:, None] // 64 == np.arange(128)[None, :] // 64)
    cf[:, CF_MASKB:CF_MASKB + 256] = (p[:, None] // 32 == np.arange(256)[None, :] // 64)
    for h in range(2):
        cf[:, CF_HMA + h] = (p // 64 == h)
    for h in range(4):
        cf[:, CF_HMB + h] = (p // 32 == h)
    dd = p % 64
    j = dd % 8
    invf = np.power(np.float32(500000.0), -np.arange(0, 16, 2, dtype=np.float32) / np.float32(16)).astype(np.float32)
    rot = dd < 16
    pi = np.float32(np.pi)
    cf[:, CF_ROPE + 0] = np.where(rot, invf[j], 0.0)
    cf[:, CF_ROPE + 1] = np.float32(np.pi / 2)
    cf[:, CF_ROPE + 2] = np.where(rot, invf[j], 0.0)
    cf[:, CF_ROPE + 3] = np.where(dd < 8, pi, 0.0)
    cf[:, CF_EPS] = EPS
    cb = np.zeros((128, CB_N), np.float32)
    cb[:, CB_IDENT:CB_IDENT + 128] = np.eye(128)
    cb[:, CB_ONES:CB_ONES + 128] = 1.0
    cb[:, CB_BLK64:CB_BLK64 + 128] = (p[:, None] // 64 == np.arange(128)[None, :] // 64)
    s_ = p[:, None]
    t_ = np.arange(128)[None, :]
    cb[:, CB_CMASK:CB_CMASK + 128] = (s_ // 64 == t_ // 64) & (s_ <= t_)
    am = np.zeros((128, 2, 2, 128), np.float32)
    am[:, :, 0, :] = (s_ >= t_)[:, None, :]
    am[:, :, 1, :] = (s_ <= t_)[:, None, :]
    cb[:, CB_AMASK:CB_AMASK + 512] = am.reshape(128, 512)
    cb[:, CB_SCANM:CB_SCANM + 512] = (np.arange(512)[None, :] % 64 != 0)
    return cf, cb.astype(ml_dtypes.bfloat16)


PV_FFN1 = 0
PV_MIX = 8
PV_CROSS = 16
PV_MEMN = 24
PV_FFN2 = 32
PV_LBLOG = 40
PV_HGN = 42
PV_GLN = 44
PV_GLB = 46
PV_CVB = 47
PV_CVG = 49
PV_CVBB = 51
PV_CVW = 53
PV_L = 115
PV_FINAL = DEPTH * PV_L
PV_N = PV_FINAL + 8


def _fm(v, ncol):
    return np.ascontiguousarray(np.asarray(v, np.float32).reshape(ncol, 128).T)


def _pack_params(inp):
    pv = np.zeros((128, PV_N), np.float32)
    for l in range(DEPTH):
        b = l * PV_L
        pv[:, b + PV_FFN1:b + PV_FFN1 + 8] = _fm(inp["ffn1_norm"][l], 8)
        pv[:, b + PV_MIX:b + PV_MIX + 8] = _fm(inp["mix_norm"][l], 8)
        pv[:, b + PV_CROSS:b + PV_CROSS + 8] = _fm(inp["cross_norm"][l], 8)
        pv[:, b + PV_MEMN:b + PV_MEMN + 8] = _fm(inp["mem_norm"][l], 8)
        pv[:, b + PV_FFN2:b + PV_FFN2 + 8] = _fm(inp["ffn2_norm"][l], 8)
        pv[:, b + PV_LBLOG:b + PV_LBLOG + 2] = _fm(inp["hgrn_lb_logits"][l], 2)
        pv[:, b + PV_HGN:b + PV_HGN + 2] = _fm(inp["hgrn_out_norm"][l], 2)
        pv[:, b + PV_GLN:b + PV_GLN + 2] = _fm(inp["gla_out_norm"][l], 2)
        pv[:, b + PV_GLB:b + PV_GLB + 1] = _fm(inp["gla_gate_b"][l], 1)
        pv[:, b + PV_CVB:b + PV_CVB + 2] = _fm(inp["conv_b"][l], 2)
        pv[:, b + PV_CVG:b + PV_CVG + 2] = _fm(inp["conv_ln_g"][l], 2)
        pv[:, b + PV_CVBB:b + PV_CVBB + 2] = _fm(inp["conv_ln_b"][l], 2)
        cw = np.asarray(inp["conv_w"][l], np.float32)
        for cc in range(2):
            pv[:, b + PV_CVW + cc * 31:b + PV_CVW + cc * 31 + 31] = cw[:, cc * 128:(cc + 1) * 128].T
    pv[:, PV_FINAL:PV_FINAL + 8] = _fm(inp["final_norm"], 8)
    return pv


WNAMES = ["ffn1_w_up", "ffn1_w_down", "w_in", "w_out", "cross_wq", "cross_wkv", "cross_wo",
          "ffn2_w_up", "ffn2_w_down"]
WSHAPES = {"ffn1_w_up": [DEPTH, D, 2 * DFF], "ffn1_w_down": [DEPTH, DFF, D], "w_in": [DEPTH, D, INW],
           "w_out": [DEPTH, D, D], "cross_wq": [DEPTH, D, D], "cross_wkv": [DEPTH, D, 2 * D],
           "cross_wo": [DEPTH, D, D], "ffn2_w_up": [DEPTH, D, 2 * DFF], "ffn2_w_down": [DEPTH, DFF, D]}

NBB = 10
NTP = 7
NTB = 10
NWS = 2
NWB = 5

DP_G = 0
DP_L = 48
DP_OML = 40
DP_LBF = 42
DP_HGN = 44
DP_GLN = 46
DP_FINAL = DEPTH * DP_L
DP_N = DP_FINAL + 8


class Builder:
    def __init__(self, nseq=SEQ_PER_CORE, depth=DEPTH, stop=None, dbg=None, mixsel="abcd"):
        self.nseq, self.depth, self.stop, self.dbgspec, self.mixsel = nseq, depth, stop, dbg, mixsel
        self.em = Em()
        self.nc = bass.Bass("TRN2", target_bir_lowering=False)
        self.out_toks = []

    def E(self, eng, op, *args, reads=(), writes=(), signal=True, dma=False, **kw):
        return self.em.emit(eng, (op, args, kw), reads, writes, signal=signal, dma=dma)

    def bbk(self, i, a=0, b=S):
        return [("BB", i, t) for t in range(a // TT, (b - 1) // TT + 1)]

    def bbf(self, i):
        return self.BB[:, i:i + 2, :].rearrange("p a b -> p (a b)").bitcast(F32)

    def bbfk(self, i):
        return self.bbk(i) + self.bbk(i + 1)

    def xk(self, c, tt):
        return ("X", c, tt)

    def hk(self, c, tt):
        return ("H", c, tt)

    def hks(self, tt):
        return [("H", c, tt) for c in range(8)]

    def load_slab(self, src_ap, kc, cols):
        assert kc * cols <= 1024
        n = kc * cols
        i = self.ws_rr
        self.ws_rr = (i + 1) % NWS
        j = self.wbp.get()
        ws = self.WS[:, i, 0:n].rearrange("p (k c) -> p k c", k=kc)
        self.E("sp", "dma_start", out=ws, in_=src_ap, writes=[("WS", i)], dma=True)
        self.E("pool", "tensor_copy", self.WB[:, j, 0:n], self.WS[:, i, 0:n], reads=[("WS", i)], writes=[("WB", j)])
        return j, self.WB[:, j, 0:n].rearrange("p (k c) -> p k c", k=kc), ("WB", j)

    def wcols(self, name, l, c0, ncols, r0=0, nrows=D):
        w = self.W[name]
        return w[l, r0:r0 + nrows, c0:c0 + ncols].rearrange("(k p) c -> p k c", p=128)

    def ps_get(self):
        i = self.psp.get()
        return i, self.PS[i], ("PS", i)

    def tp_get(self):
        i = self.tpp.get()
        return i, self.TP[:, i, :], ("TP", i)

    def tb_get(self):
        i = self.tbp.get()
        return i, self.TB[:, i, :], ("TB", i)

    def rstd(self, ps, pk, n, ncols=TT):
        ri, rap, rk = self.tp_get()
        self.E("act", "activation", rap[:, 0:ncols], ps[:, 0:ncols], AF.Ln, bias=self.CF[:, CF_EPS:CF_EPS + 1], scale=1.0 / n,
               reads=[pk, "CF"], writes=[rk])
        self.E("act", "activation", rap[:, 0:ncols], rap[:, 0:ncols], AF.Exp, scale=-0.5, reads=[rk], writes=[rk])
        return ri, rap, rk

    def proj_fm(self, w, wk, tt, m=128):
        sl = slice(tt * TT, (tt + 1) * TT)
        pi, ps, pk = self.ps_get()
        for k in range(8):
            self.E("pe", "matmul", ps[0:m, :], w[:, k, 0:m], self.H[:, k, sl], start=(k == 0), stop=(k == 7),
                   reads=[wk, self.hk(k, tt)], writes=[pk], signal=(k == 7))
        return pi, ps, pk

    def build(self):
        nc = self.nc
        ns = self.nseq
        self.x_d = nc.dram_tensor("x", [ns, S, D], F32, kind="ExternalInput").ap()
        self.mem_d = nc.dram_tensor("mem", [ns, MEM, D], F32, kind="ExternalInput").ap()
        self.pos_d = nc.dram_tensor("pos", [ns, S], I32, kind="ExternalInput").ap()
        self.W = {n: nc.dram_tensor(n, WSHAPES[n], F32, kind="ExternalInput").ap() for n in WNAMES}
        self.gw_d = nc.dram_tensor("gla_gate_w", [DEPTH, 16, 128], F32, kind="ExternalInput").ap()
        self.pv_d = nc.dram_tensor("pv", [128, PV_N], F32, kind="ExternalInput").ap()
        self.cf_d = nc.dram_tensor("cf", [128, CF_N], F32, kind="ExternalInput").ap()
        self.cb_d = nc.dram_tensor("cb", [128, CB_N], BF16, kind="ExternalInput").ap()
        self.y_d = nc.dram_tensor("y", [ns, S, D], F32, kind="ExternalOutput").ap()
        with ExitStack() as st:
            def sb(name, shape, dt):
                return st.enter_context(nc.sbuf_tensor(name, shape, dt))
            self.X = sb("X", [128, 8, S], F32)
            self.H = sb("H", [128, 8, S], BF16)
            self.BB = sb("BB", [128, NBB, S], BF16)
            self.WS = sb("WS", [128, NWS, 1024], F32)
            self.WB = sb("WB", [128, NWB, 1024], BF16)
            self.TP = sb("TP", [128, NTP, TT], F32)
            self.TB = sb("TB", [128, NTB, TT], BF16)
            self.ROPE = sb("ROPE", [128, 2, S], BF16)
            self.KX = sb("KX", [128, 8, MEM], BF16)
            self.VX = sb("VX", [128, 2, D], BF16)
            self.CF = sb("CF", [128, CF_N], F32)
            self.CB = sb("CB", [128, CB_N], BF16)
            self.PV = sb("PV", [128, PV_N], F32)
            self.DP = sb("DP", [128, DP_N], F32)
            self.GWS = sb("GWS", [16, DEPTH, 128], F32)
            self.GW = sb("GW", [16, DEPTH, 128], BF16)
            self.SS = sb("SS", [128, 256], F32)
            self.D32 = sb("D32", [128, 2, 256], F32)
            self.D16 = sb("D16", [128, 4, 256], BF16)
            self.EB = sb("EB", [128, 64], F32)
            self.SM = sb("SM", [128, 64], F32)
            self.PS = [st.enter_context(nc.psum_tensor("ps%d" % i, [128, TT], F32)) for i in range(8)]
            self.psp = Pool("PS", self.PS)
            self.tpp = Pool("TP", list(range(NTP)))
            self.tbp = Pool("TB", list(range(NTB)))
            self.wbp = Pool("WB", list(range(NWB)))
            self.bbp = Pool("BB", list(range(NBB)))
            self.ws_rr = 0
            sems = {n: st.enter_context(nc.semaphore(n)) for n in self.em.sem_names()}
            self.program()
            self.em.finalize()
            block = st.enter_context(nc.Block())
            em = self.em

            @block.sync
            def _(e):
                em.replay("sp", e, sems)

            @block.gpsimd
            def _(e):
                em.replay("pool", e, sems)

            @block.tensor
            def _(e):
                em.replay("pe", e, sems)

            @block.vector
            def _(e):
                em.replay("dve", e, sems)

            @block.scalar
            def _(e):
                em.replay("act", e, sems)
        return nc

    def program(self):
        E = self.E
        CF, CB, PV, DP = self.CF, self.CB, self.PV, self.DP
        E("sp", "dma_start", out=CF[:, :], in_=self.cf_d[:, :], writes=["CF"], dma=True)
        E("sp", "dma_start", out=CB[:, :], in_=self.cb_d[:, :], writes=["CB"], dma=True)
        E("sp", "dma_start", out=PV[:, :], in_=self.pv_d[:, :], writes=["PV"], dma=True)
        E("sp", "dma_start", out=self.GWS[:, :, :], in_=self.gw_d.rearrange("l r c -> r l c"), writes=["GWS"], dma=True)
        E("pool", "tensor_copy", self.GW[:, :, :], self.GWS[:, :, :], reads=["GWS"], writes=["GW"])
        for l in range(self.depth):
            pb, db = l * PV_L, l * DP_L
            E("dve", "tensor_copy", DP[:, db:db + 40], PV[:, pb:pb + 40], reads=["PV"], writes=["DP"])
            E("dve", "tensor_copy", DP[:, db + DP_HGN:db + DP_HGN + 4], PV[:, pb + PV_HGN:pb + PV_HGN + 4], reads=["PV"], writes=["DP"])
            if l == 0:
                E("dve", "memset", DP[:, db + DP_OML:db + DP_OML + 2], 1.0, writes=["DP"])
                E("dve", "memset", DP[:, db + DP_LBF:db + DP_LBF + 2], 1e-20, writes=["DP"])
            else:
                E("dve", "tensor_tensor", self.SM[:, 0:2], PV[:, pb + PV_LBLOG:pb + PV_LBLOG + 2], PV[:, PV_LBLOG:PV_LBLOG + 2], ALU.subtract,
                  reads=["PV"], writes=["SM"])
                E("act", "activation", self.SM[:, 2:4], self.SM[:, 0:2], AF.Sigmoid, reads=["SM"], writes=["SM"])
                E("dve", "tensor_scalar", DP[:, db + DP_OML:db + DP_OML + 2], self.SM[:, 2:4], -1.0, 1.0, ALU.mult, ALU.add,
                  reads=["SM"], writes=["DP"])
                E("dve", "tensor_scalar", DP[:, db + DP_LBF:db + DP_LBF + 2], self.SM[:, 2:4], 1e-20, None, ALU.max,
                  reads=["SM"], writes=["DP"])
        E("dve", "tensor_copy", DP[:, DP_FINAL:DP_FINAL + 8], PV[:, PV_FINAL:PV_FINAL + 8], reads=["PV"], writes=["DP"])

        for s in range(self.nseq):
            self.load_x(s)
            if self.stop == "load":
                self.final(s, norm=False)
                continue
            self.rope_tables(s)
            for l in range(self.depth):
                last = (l == self.depth - 1)
                self.ffn(l, "ffn1", PV_FFN1)
                if last and self.stop == "ffn1":
                    break
                self.mixer(l, s)
                if last and self.stop == "mixer":
                    break
                self.cross(l, s)
                if last and self.stop == "cross":
                    break
                self.ffn(l, "ffn2", PV_FFN2)
            self.final(s)
        self.em.wait_all("sp", self.out_toks)

    def load_x(self, s):
        E = self.E
        ident = self.CF[:, CF_IDENT:CF_IDENT + 128]
        for tb in range(S // 128):
            halves = []
            for hf in range(2):
                i, ap, key = self.tp_get()
                E("sp", "dma_start", out=ap, in_=self.x_d[s, tb * 128:(tb + 1) * 128, hf * 512:(hf + 1) * 512], writes=[key], dma=True)
                halves.append((i, ap, key))
            for hf in range(2):
                i, ap, key = halves[hf]
                pi, ps, pk = self.ps_get()
                for q in range(4):
                    E("pe", "transpose", ps[:, q * 128:(q + 1) * 128], ap[:, q * 128:(q + 1) * 128], ident,
                      reads=[key, "CF"], writes=[pk], signal=(q == 3))
                c0 = hf * 4
                tt = tb // 4
                dst = self.X[:, c0:c0 + 4, tb * 128:(tb + 1) * 128]
                src = ps.rearrange("p (c t) -> p c t", c=4)
                wr = [self.xk(c, tt) for c in range(c0, c0 + 4)]
                if hf == 0:
                    E("act", "activation", dst, src, AF.Copy, reads=[pk], writes=wr)
                else:
                    E("dve", "tensor_copy", dst, src, reads=[pk], writes=wr)
                self.psp.free(pi)
                self.tpp.free(i)

    def rmsnorm_x(self, gcol):
        E = self.E
        ones = self.CB[:, CB_ONES:CB_ONES + 128]
        for tt in range(NT):
            sl = slice(tt * TT, (tt + 1) * TT)
            pi, ps, pk = self.ps_get()
            for c in range(8):
                bi, bap, bk = self.tb_get()
                E("act", "activation", bap, self.X[:, c, sl], AF.Square, reads=[self.xk(c, tt)], writes=[bk])
                E("pe", "matmul", ps[:, :], ones, bap, start=(c == 0), stop=(c == 7), reads=[bk, "CB"], writes=[pk], signal=(c == 7))
                self.tbp.free(bi)
            ri, rap, rk = self.rstd(ps, pk, D)
            self.psp.free(pi)
            for c in range(8):
                E("dve", "scalar_tensor_tensor", self.H[:, c, sl], self.X[:, c, sl], self.DP[:, gcol + c:gcol + c + 1], rap, ALU.mult, ALU.mult,
                  reads=[self.xk(c, tt), rk, "DP"], writes=[self.hk(c, tt)])
            self.tpp.free(ri)

    def out_proj(self, wname, l, r0, bufs, scale=1.0):
        E = self.E
        n = len(bufs)
        for dc in range(8):
            di, dw, dk = self.load_slab(self.wcols(wname, l, dc * 128, 128, r0=r0, nrows=n * 128), n, 128)
            for tt in range(NT):
                sl = slice(tt * TT, (tt + 1) * TT)
                pa, psa, pka = self.ps_get()
                for k in range(n):
                    E("pe", "matmul", psa[:, :], dw[:, k, :], self.BB[:, bufs[k], sl], start=(k == 0), stop=(k == n - 1),
                      reads=[dk, ("BB", bufs[k], tt)], writes=[pka], signal=(k == n - 1))
                E("dve", "scalar_tensor_tensor", self.X[:, dc, sl], psa[:, :], float(scale), self.X[:, dc, sl], ALU.mult, ALU.add,
                  reads=[pka, self.xk(dc, tt)], writes=[self.xk(dc, tt)])
                self.psp.free(pa)
            self.wbp.free(di)

    def ffn(self, l, pre, pvcol):
        E = self.E
        self.rmsnorm_x(l * DP_L + pvcol)
        wup, wdn = pre + "_w_up", pre + "_w_down"
        groups = [(0, 8), (8, 16), (16, 22)]
        for (fa, fb) in groups:
            n = fb - fa
            bufs = [self.bbp.get() for _ in range(n)]
            for j in range(fa, fb):
                gi, gw, gk = self.load_slab(self.wcols(wup, l, j * 128, 128), 8, 128)
                ui, uw, uk = self.load_slab(self.wcols(wup, l, DFF + j * 128, 128), 8, 128)
                bbi = bufs[j - fa]
                for tt in range(NT):
                    sl = slice(tt * TT, (tt + 1) * TT)
                    pa, psa, pka = self.proj_fm(gw, gk, tt)
                    pb, psb, pkb = self.proj_fm(uw, uk, tt)
                    ti, tap, tk = self.tp_get()
                    E("act", "activation", tap, psa[:, :], AF.Silu, reads=[pka], writes=[tk])
                    E("dve", "tensor_tensor", self.BB[:, bbi, sl], psb[:, :], tap, ALU.mult, reads=[tk, pkb], writes=[("BB", bbi, tt)])
                    self.psp.free(pa)
                    self.psp.free(pb)
                    self.tpp.free(ti)
                self.wbp.free(gi)
                self.wbp.free(ui)
            self.out_proj(wdn, l, fa * 128, bufs, scale=0.5)
            for b in bufs:
                self.bbp.free(b)

    def final(self, s, norm=True):
        E = self.E
        identf = self.CF[:, CF_IDENT:CF_IDENT + 128]
        ones = self.CB[:, CB_ONES:CB_ONES + 128]
        gcol = DP_FINAL
        for tt in range(NT):
            sl = slice(tt * TT, (tt + 1) * TT)
            if norm:
                pi, ps, pk = self.ps_get()
                for c in range(8):
                    bi, bap, bk = self.tb_get()
                    E("act", "activation", bap, self.X[:, c, sl], AF.Square, reads=[self.xk(c, tt)], writes=[bk])
                    E("pe", "matmul", ps[:, :], ones, bap, start=(c == 0), stop=(c == 7), reads=[bk, "CB"], writes=[pk], signal=(c == 7))
                    self.tbp.free(bi)
                ri, rap, rk = self.rstd(ps, pk, D)
                self.psp.free(pi)
                for c in range(8):
                    E("dve", "scalar_tensor_tensor", self.X[:, c, sl], self.X[:, c, sl], self.DP[:, gcol + c:gcol + c + 1], rap, ALU.mult, ALU.mult,
                      reads=[self.xk(c, tt), rk, "DP"], writes=[self.xk(c, tt)])
                self.tpp.free(ri)
            for q in range(4):
                tb = tt * 4 + q
                for hf in range(2):
                    pi, ps, pk = self.ps_get()
                    for c4 in range(4):
                        c = hf * 4 + c4
                        E("pe", "transpose", ps[:, c4 * 128:(c4 + 1) * 128], self.X[:, c, tb * 128:(tb + 1) * 128], identf,
                          reads=[self.xk(c, tt), "CF"], writes=[pk], signal=(c4 == 3))
                    oi, oap, ok = self.tp_get()
                    if hf == 0:
                        E("act", "activation", oap, ps[:, :], AF.Copy, reads=[pk], writes=[ok])
                    else:
                        E("dve", "tensor_copy", oap, ps[:, :], reads=[pk], writes=[ok])
                    self.psp.free(pi)
                    t = E("sp", "dma_start", out=self.y_d[s, tb * 128:(tb + 1) * 128, hf * 512:(hf + 1) * 512], in_=oap, reads=[ok], dma=True)
                    self.out_toks.append(t)
                    self.tpp.free(oi)

    def rope_tables(self, s):
        E = self.E
        b = list(self.bbp.get_pair()) + list(self.bbp.get_pair()) + list(self.bbp.get_pair())
        posi = self.BB[:, b[0]:b[0] + 2, :].rearrange("p a b -> p (a b)").bitcast(I32)
        kint = self.BB[:, b[4]:b[4] + 2, :].rearrange("p a b -> p (a b)").bitcast(I32)
        posf = self.bbf(b[2])
        u = self.bbf(b[0])
        kf = self.bbf(b[4])
        k01, k23, k45 = self.bbfk(b[0]), self.bbfk(b[2]), self.bbfk(b[4])
        assert b[1] == b[0] + 1 and b[3] == b[2] + 1 and b[5] == b[4] + 1
        E("sp", "dma_start", out=posi, in_=self.pos_d[s:s + 1, :].to_broadcast([128, S]), writes=k01, dma=True)
        E("dve", "tensor_copy", posf, posi, reads=k01, writes=k23)
        twopi = float(2 * np.pi)
        for j in range(2):
            invc = self.CF[:, CF_ROPE + 2 * j:CF_ROPE + 2 * j + 1]
            phic = self.CF[:, CF_ROPE + 2 * j + 1:CF_ROPE + 2 * j + 2]
            E("dve", "tensor_scalar", u, posf, invc, phic, ALU.mult, ALU.add, reads=k23 + ["CF"], writes=k01)
            E("dve", "tensor_scalar", kint, u, 1.0 / twopi, None, ALU.mult, reads=k01, writes=k45)
            E("dve", "tensor_copy", kf, kint, reads=k45, writes=k45)
            E("dve", "scalar_tensor_tensor", u, kf, -twopi, u, ALU.mult, ALU.add, reads=k45 + k01, writes=k01)
            E("dve", "tensor_scalar", u, u, float(np.pi), float(-np.pi), ALU.min, ALU.max, reads=k01, writes=k01)
            E("act", "activation", self.ROPE[:, j, :], u, AF.Sin, reads=k01, writes=[("ROPE", j)])
        for x in b:
            self.bbp.free(x)

    def mixer(self, l, s):
        self.rmsnorm_x(l * DP_L + PV_MIX)
        self.cross_kv(l, s)
        if "a" in self.mixsel:
            gens = [self.gla_group(l, "A", 0, 0), self.gla_group(l, "A", 1, 1)]
            while gens:
                for gen in list(gens):
                    try:
                        next(gen)
                    except StopIteration:
                        gens.remove(gen)
        if "b" in self.mixsel:
            for _ in self.gla_group(l, "B", 0, 0):
                pass
        if "c" in self.mixsel:
            self.attn_group(l, 0)
            self.attn_group(l, 1)
        if "d" in self.mixsel:
            self.conv_group(l)

    def gla_group(self, l, kind, cc, slot=0):
        E = self.E
        pb, db = l * PV_L, l * DP_L
        CF, CB, DP, PV = self.CF, self.CB, self.DP, self.PV
        if kind == "A":
            nh, nv = 2, 128
            qc0, zc0, vc0, gc0 = 0 + cc * 128, 256 + cc * 128, 512 + cc * 128, 768 + cc * 128
            wrow = cc * 128
            gain0 = db + DP_HGN + cc
            hmcol = CF_HMA
            smask = CF[:, CF_MASKA:CF_MASKA + 128]
            sc_e = 1.0
            qscale = 1.0
        else:
            nh, nv = 4, 256
            qc0, kc0, vc0, lrc0, gc0 = 1024, 1152, 1280, 1536, 1552
            wrow = 256
            gain0 = db + DP_GLN
            hmcol = CF_HMB
            smask = CF[:, CF_MASKB:CF_MASKB + 256]
            sc_e = 1.0 / 16.0
            qscale = float(32 ** -0.5)
        noc = nv // 128
        QT, KT, KTT = self.bbp.get(), self.bbp.get(), self.bbp.get()
        VT = [self.bbp.get() for _ in range(noc)]
        OA = [self.bbp.get() for _ in range(noc)]
        ktt = self.BB[:, KTT, :].rearrange("p (b c) -> p b c", c=128)
        vkeys_all = [k for v in VT for k in self.bbk(v)]
        scanm = CB[:, CB_SCANM:CB_SCANM + TT]
        identb = CB[:, CB_IDENT:CB_IDENT + 128]

        wq_i, wq, wqk = self.load_slab(self.wcols("w_in", l, qc0, 128), 8, 128)
        if kind == "A":
            wz_i, wz, wzk = self.load_slab(self.wcols("w_in", l, zc0, 128), 8, 128)
        else:
            wk_i, wk, wkk = self.load_slab(self.wcols("w_in", l, kc0, 128), 8, 128)
            wl_i, wl, wlk = self.load_slab(self.wcols("w_in", l, lrc0, 16), 8, 16)
        wv = [self.load_slab(self.wcols("w_in", l, vc0 + a * 128, 128), 8, 128) for a in range(noc)]
        for tt in range(NT):
            sl = slice(tt * TT, (tt + 1) * TT)
            t0i, t0, t0k = self.tp_get()
            if kind == "A":
                t1i, t1, t1k = self.tp_get()
                pi, ps, pk = self.proj_fm(wz, wzk, tt)
                E("act", "activation", t0, ps[:, :], AF.Sigmoid, reads=[pk], writes=[t0k])
                E("act", "activation", t1, ps[:, :], AF.Sigmoid, scale=-1.0, reads=[pk], writes=[t1k])
                self.psp.free(pi)
                E("dve", "tensor_scalar", t0, t0, DP[:, db + DP_OML + cc:db + DP_OML + cc + 1], DP[:, db + DP_LBF + cc:db + DP_LBF + cc + 1],
                  ALU.mult, ALU.add, reads=[t0k, "DP"], writes=[t0k])
                E("act", "activation", t0, t0, AF.Ln, reads=[t0k], writes=[t0k])
            else:
                pi, ps, pk = self.proj_fm(wl, wlk, tt, m=16)
                li, lap, lk = self.tb_get()
                E("act", "activation", lap[0:16, :], ps[0:16, :], AF.Copy, reads=[pk], writes=[lk])
                self.psp.free(pi)
                pi, ps, pk = self.ps_get()
                E("pe", "matmul", ps[:, :], self.GW[:, l, :], lap[0:16, :], start=True, stop=True, reads=["GW", lk], writes=[pk])
                self.tbp.free(li)
                E("act", "activation", t0, ps[:, :], AF.Sigmoid, bias=PV[:, pb + PV_GLB:pb + PV_GLB + 1], reads=[pk, "PV"], writes=[t0k])
                self.psp.free(pi)
                E("act", "activation", t0, t0, AF.Ln, reads=[t0k], writes=[t0k])
            t2i, t2, t2k = self.tp_get()
            t3i, t3, t3k = self.tp_get()
            E("dve", "tensor_tensor_scan", t2, scanm, t0, 0.0, ALU.mult, ALU.add, reads=[t0k, "CB"], writes=[t2k])
            b3 = t2.rearrange("p (a b) -> p a b", b=64)
            E("act", "activation", self.EB[:, slot * 32 + tt * 8:slot * 32 + (tt + 1) * 8], t2[:, 63:TT:64], AF.Exp, scale=sc_e, reads=[t2k], writes=[("EB", slot, tt)])
            E("dve", "tensor_tensor", t3.rearrange("p (a b) -> p a b", b=64), b3, b3[:, :, 63:64].to_broadcast([128, 8, 64]), ALU.subtract,
              reads=[t2k], writes=[t3k])
            E("act", "activation", t0, t3, AF.Exp, scale=sc_e, reads=[t3k], writes=[t0k])
            E("act", "activation", t2, t3, AF.Exp, scale=-sc_e, reads=[t3k], writes=[t2k])
            self.tpp.free(t3i)
            pi, ps, pk = self.proj_fm(wq, wqk, tt)
            if qscale == 1.0:
                E("dve", "tensor_tensor", self.BB[:, QT, sl], ps[:, :], t0, ALU.mult, reads=[pk, t0k], writes=[("BB", QT, tt)])
            else:
                E("dve", "scalar_tensor_tensor", self.BB[:, QT, sl], ps[:, :], qscale, t0, ALU.mult, ALU.mult, reads=[pk, t0k], writes=[("BB", QT, tt)])
            self.psp.free(pi)
            if kind == "A":
                E("dve", "scalar_tensor_tensor", self.BB[:, KT, sl], t1, DP[:, db + DP_OML + cc:db + DP_OML + cc + 1], t2, ALU.mult, ALU.mult,
                  reads=[t1k, t2k, "DP"], writes=[("BB", KT, tt)])
                self.tpp.free(t1i)
            else:
                pi, ps, pk = self.proj_fm(wk, wkk, tt)
                E("dve", "tensor_tensor", self.BB[:, KT, sl], ps[:, :], t2, ALU.mult, reads=[pk, t2k], writes=[("BB", KT, tt)])
                self.psp.free(pi)
            self.tpp.free(t0i)
            self.tpp.free(t2i)
            pi, ps, pk = self.ps_get()
            psb = ps[:, 0:256].bitcast(BF16)
            for q4 in range(4):
                E("pe", "transpose", psb[:, q4 * 128:(q4 + 1) * 128], self.BB[:, KT, tt * TT + q4 * 128:tt * TT + (q4 + 1) * 128], identb,
                  reads=[("BB", KT, tt), "CB"], writes=[pk], signal=(q4 == 3))
            E("act", "activation", ktt[:, tt * 4:(tt + 1) * 4, :], psb.rearrange("p (b c) -> p b c", c=128), AF.Copy,
              reads=[pk], writes=[("BB", KTT, tt)])
            self.psp.free(pi)
            for a in range(noc):
                pi, ps, pk = self.ps_get()
                for q4 in range(4):
                    tb = tt * 4 + q4
                    for k in range(8):
                        E("pe", "matmul", ps[:, q4 * 128:(q4 + 1) * 128], self.H[:, k, tb * 128:(tb + 1) * 128], wv[a][1][:, k, :],
                          start=(k == 0), stop=(k == 7), reads=[wv[a][2], self.hk(k, tt)], writes=[pk], signal=(k == 7))
                dstv = self.BB[:, VT[a], :].rearrange("p (b c) -> p b c", c=128)[:, tt * 4:(tt + 1) * 4, :]
                E("act", "activation", dstv, ps.rearrange("p (b c) -> p b c", c=128), AF.Copy, reads=[pk], writes=[("BB", VT[a], tt)])
                self.psp.free(pi)
        self.wbp.free(wq_i)
        if kind == "A":
            self.wbp.free(wz_i)
        else:
            self.wbp.free(wk_i)
            self.wbp.free(wl_i)
        for a in range(noc):
            self.wbp.free(wv[a][0])

        wg = [self.load_slab(self.wcols("w_in", l, gc0 + a * 128, 128), 8, 128) for a in range(noc)]
        blk64 = CB[:, CB_BLK64:CB_BLK64 + 128]
        cmask = CB[:, CB_CMASK:CB_CMASK + 128]
        hm = CF[:, hmcol:hmcol + nh]
        so = slot * 128
        slots = [slot] if nv == 128 else [0, 1]
        SS = self.SS[:, so:so + nv]
        EB = self.EB[:, slot * 32:(slot + 1) * 32]
        D32 = [self.D32[:, i, so:so + nv] for i in range(2)]
        D16 = [self.D16[:, i, so:so + nv] for i in range(4)]
        ssk = [("SS", x) for x in slots]

        def d32k(i):
            return [("D32", i, x) for x in slots]

        def d16k(i):
            return [("D16", i, x) for x in slots]
        for i in range(2):
            E("dve", "memset", D32[i], 0.0, writes=d32k(i))
        for i in range(4):
            E("dve", "memset", D16[i], 0.0, writes=d16k(i))
        yield

        def vtap(blk, p0, p1, c0, c1):
            a = c0 // 128
            assert (c1 - 1) // 128 == a
            return self.BB[:, VT[a], :].rearrange("p (b c) -> p b c", c=128)[p0:p1, blk, c0 - a * 128:c1 - a * 128]

        state = {}

        def front(tt, blk):
            b = tt * 4 + blk
            g0 = tt * TT + blk * 128
            kmi, kmap, kmk = self.tb_get()
            km = kmap[:, 0:nh * 128].rearrange("p (h t) -> p h t", h=nh)
            E("pool", "tensor_tensor", km, self.BB[:, KT, g0:g0 + 128].unsqueeze(1).to_broadcast([128, nh, 128]),
              hm.unsqueeze(2).to_broadcast([128, nh, 128]), ALU.mult, reads=[("BB", KT, tt), "CF"], writes=[kmk])
            si, sps, sk = self.ps_get()
            for h in range(nh):
                E("pe", "matmul", sps[:, h * 128:(h + 1) * 128], km[:, h, :], self.BB[:, QT, g0:g0 + 128], start=True, stop=True,
                  reads=[kmk, ("BB", QT, tt)], writes=[sk], signal=(h == nh - 1))
            self.tbp.free(kmi)
            tpsl = []
            for half in range(2):
                pr = half * 64
                ti, tps, tk = self.ps_get()
                for a in range(noc):
                    E("pe", "matmul", tps[:, a * 128:(a + 1) * 128], ktt[pr:pr + 64, b, :], vtap(b, pr, pr + 64, a * 128, (a + 1) * 128),
                      start=True, stop=True, reads=[("BB", KTT, tt), ("BB", VT[a], tt)], writes=[tk], signal=(a == noc - 1))
                tpsl.append((ti, tps, tk))
            for half in range(2):
                c = 2 * b + half
                ti, tps, tk = tpsl[half]
                if c > 0:
                    E("dve", "scalar_tensor_tensor", D32[c % 2], SS, EB[:, c:c + 1], smask, ALU.mult, ALU.mult,
                      reads=ssk + [("EB", slot, c // 8), "CF"], writes=d32k(c % 2))
                    E("act", "activation", D16[c % 4], D32[c % 2], AF.Copy, reads=d32k(c % 2), writes=d16k(c % 4))
                    E("dve", "tensor_tensor", SS, D32[c % 2], tps[:, 0:nv], ALU.add, reads=d32k(c % 2) + [tk], writes=ssk)
                else:
                    E("dve", "tensor_copy", SS, tps[:, 0:nv], reads=[tk], writes=ssk)
                self.psp.free(ti)
            sci, scap, sck = self.tb_get()
            sc = scap[:, 0:nh * 128].rearrange("p (h t) -> p h t", h=nh)
            E("dve", "tensor_tensor", sc, sps[:, 0:nh * 128].rearrange("p (h t) -> p h t", h=nh),
              cmask.unsqueeze(1).to_broadcast([128, nh, 128]), ALU.mult, reads=[sk, "CB"], writes=[sck])
            self.psp.free(si)
            state[b] = (sci, sc, sck)

        def back(tt, blk, ops):
            b = tt * 4 + blk
            c0 = blk * 128
            sci, sc, sck = state.pop(b)
            for h in range(nh):
                oc, po = h // 2, (h % 2) * 64
                E("pe", "matmul", ops[oc][1][po:po + 64, c0:c0 + 128], vtap(b, 0, 128, h * 64, (h + 1) * 64), sc[:, h, :],
                  start=False, stop=False, skip_group_check=True,
                  reads=[sck, ("BB", VT[(h * 64) // 128], tt)], writes=[ops[oc][2]], signal=(h == nh - 1))
            self.tbp.free(sci)
            for half in range(2):
                c = 2 * b + half
                tc = c0 + half * 64
                gtc = tt * TT + tc
                if c > 0:
                    for oc in range(noc):
                        E("pe", "matmul", ops[oc][1][:, tc:tc + 64], D16[c % 4][:, oc * 128:(oc + 1) * 128], self.BB[:, QT, gtc:gtc + 64],
                          start=False, stop=False, skip_group_check=True,
                          reads=d16k(c % 4) + [("BB", QT, tt)], writes=[ops[oc][2]])

        for tt in range(NT):
            sl = slice(tt * TT, (tt + 1) * TT)
            ops = [self.ps_get() for _ in range(noc)]
            for (oi, op, ok) in ops:
                E("dve", "memset", op[:, :], 0.0, writes=[ok])
            front(tt, 0)
            yield
            for blk in range(1, 4):
                front(tt, blk)
                yield
                back(tt, blk - 1, ops)
                yield
            back(tt, 3, ops)
            yield
            for oc in range(noc):
                oi, op, ok = ops[oc]
                o32i, o32, o32k = self.tp_get()
                qi, qap, qk = self.tb_get()
                E("act", "activation", o32, op[:, :], AF.Copy, reads=[ok], writes=[o32k])
                E("act", "activation", qap, op[:, :], AF.Square, reads=[ok], writes=[qk])
                self.psp.free(oi)
                ni, nps, nk = self.ps_get()
                E("pe", "matmul", nps[:, :], blk64, qap, start=True, stop=True, reads=[qk, "CB"], writes=[nk])
                self.tbp.free(qi)
                ri, rap, rk = self.rstd(nps, nk, 64)
                self.psp.free(ni)
                E("dve", "scalar_tensor_tensor", o32, o32, DP[:, gain0 + oc:gain0 + oc + 1], rap, ALU.mult, ALU.mult,
                  reads=[o32k, rk, "DP"], writes=[o32k])
                self.tpp.free(ri)
                gi, gps, gk = self.proj_fm(wg[oc][1], wg[oc][2], tt)
                sgi, sg, sgk = self.tp_get()
                E("act", "activation", sg, gps[:, :], AF.Silu, reads=[gk], writes=[sgk])
                self.psp.free(gi)
                E("dve", "tensor_tensor", self.BB[:, OA[oc], sl], o32, sg, ALU.mult, reads=[o32k, sgk], writes=[("BB", OA[oc], tt)])
                self.tpp.free(o32i)
                self.tpp.free(sgi)
            yield
        for a in range(noc):
            self.wbp.free(wg[a][0])
        self.bbp.free(QT)
        self.bbp.free(KT)
        self.bbp.free(KTT)
        for v in VT:
            self.bbp.free(v)
        self.out_proj("w_out", l, wrow, OA)
        for o in OA:
            self.bbp.free(o)

    def attn_group(self, l, cc):
        E = self.E
        CF, CB = self.CF, self.CB
        qc0, kc0, vc0 = 1808 + cc * 128, 2064 + cc * 128, 2320 + cc * 128
        NUMb, n2 = self.bbp.get_pair()
        DENb, d2 = self.bbp.get_pair()
        QR = self.bbp.get()
        KM = [self.bbp.get(), self.bbp.get()]
        VT = self.bbp.get()
        assert n2 == NUMb + 1 and d2 == DENb + 1
        NUM, DEN = self.bbf(NUMb), self.bbf(DENb)
        numk, denk = self.bbfk(NUMb), self.bbfk(DENb)
        def swapped(src_ap, src_key):
            j = self.wbp.get()
            dst = self.WB[:, j, 0:1024].rearrange("p (k c) -> p k c", k=8)
            E("pool", "tensor_copy", dst, src_ap, reads=[src_key], writes=[("WB", j)])
            s4 = src_ap.rearrange("p k (h d) -> p k h d", h=2)
            d4 = dst.rearrange("p k (h d) -> p k h d", h=2)
            E("pool", "tensor_copy", d4[:, :, :, 0:8], s4[:, :, :, 8:16], reads=[src_key], writes=[("WB", j)])
            E("pool", "tensor_copy", d4[:, :, :, 8:16], s4[:, :, :, 0:8], reads=[src_key], writes=[("WB", j)])
            return j, dst, ("WB", j)
        hm = CF[:, CF_HMA:CF_HMA + 2]
        for which, c0 in (("q", qc0), ("k", kc0)):
            wi, w, wk = self.load_slab(self.wcols("w_in", l, c0, 128), 8, 128)
            si, sw, swk = swapped(w, wk)
            for tt in range(NT):
                sl = slice(tt * TT, (tt + 1) * TT)
                p1i, p1, p1k = self.proj_fm(w, wk, tt)
                p2i, p2, p2k = self.proj_fm(sw, swk, tt)
                t0i, t0, t0k = self.tp_get()
                t1i, t1, t1k = self.tp_get()
                E("dve", "tensor_tensor", t0, p1[:, :], self.ROPE[:, 0, sl], ALU.mult, reads=[p1k, ("ROPE", 0)], writes=[t0k])
                E("dve", "tensor_tensor", t1, p2[:, :], self.ROPE[:, 1, sl], ALU.mult, reads=[p2k, ("ROPE", 1)], writes=[t1k])
                self.psp.free(p1i)
                self.psp.free(p2i)
                if which == "q":
                    E("dve", "tensor_tensor", self.BB[:, QR, sl], t0, t1, ALU.add, reads=[t0k, t1k], writes=[("BB", QR, tt)])
                else:
                    E("dve", "tensor_tensor", t0, t0, t1, ALU.add, reads=[t0k, t1k], writes=[t0k])
                    for h in range(2):
                        E("dve", "tensor_scalar", self.BB[:, KM[h], sl], t0, hm[:, h:h + 1], None, ALU.mult, reads=[t0k, "CF"],
                          writes=[("BB", KM[h], tt)])
                self.tpp.free(t0i)
                self.tpp.free(t1i)
            self.wbp.free(wi)
            self.wbp.free(si)
        wvi, wv, wvk = self.load_slab(self.wcols("w_in", l, vc0, 128), 8, 128)
        ones64 = CB[:, CB_ONES:CB_ONES + 64]
        amask = CB[:, CB_AMASK:CB_AMASK + 512].rearrange("p (h k q) -> p h k q", h=2, k=2)
        vt = self.BB[:, VT, :].rearrange("p (b c) -> p b c", c=128)
        allq = self.bbk(QR)
        allk = [self.bbk(KM[0]), self.bbk(KM[1])]
        allv = self.bbk(VT)
        allh = [self.hk(k, t) for k in range(8) for t in range(NT)]
        for pat, d in enumerate((1, 4, 16)):
            nbs = 16 // d

            def tok(r, n):
                st0 = r + d * 128 * n
                return slice(st0, st0 + d * 127 + 1, d) if d > 1 else slice(st0, st0 + 128)
            for b4 in range(4):
                pi, ps, pk = self.ps_get()
                for q4 in range(4):
                    bid = b4 * 4 + q4
                    r, n = bid // nbs, bid % nbs
                    for k in range(8):
                        E("pe", "matmul", ps[:, q4 * 128:(q4 + 1) * 128], self.H[:, k, tok(r, n)], wv[:, k, :], start=(k == 0), stop=(k == 7),
                          reads=[wvk] + [self.hk(k, t) for t in range(NT)], writes=[pk], signal=(k == 7))
                E("act", "activation", vt[:, b4 * 4:(b4 + 1) * 4, :], ps.rearrange("p (b c) -> p b c", c=128), AF.Copy,
                  reads=[pk], writes=[("BB", VT, b4)])
                self.psp.free(pi)
            pend = {}
            banks = {}

            def front(bid):
                r, n = bid // nbs, bid % nbs
                kbs = [1] if n == 0 else [0, 1]
                si, sps, sk = self.ps_get()
                s4 = sps.rearrange("p (h k q) -> p h k q", h=2, k=2)
                for h in range(2):
                    for kb in kbs:
                        kn = n - 1 + kb
                        E("pe", "matmul", s4[:, h, kb, :], self.BB[:, KM[h], tok(r, kn)], self.BB[:, QR, tok(r, n)], start=True, stop=True,
                          reads=allk[h] + allq, writes=[sk], signal=(h == 1 and kb == 1))
                pi, pap, pk_ = self.tb_get()
                p4 = pap.rearrange("p (h k q) -> p h k q", h=2, k=2)
                k0 = kbs[0]
                E("act", "activation", p4[:, :, k0:2, :], s4[:, :, k0:2, :], AF.Exp, scale=0.125, reads=[sk], writes=[pk_])
                self.psp.free(si)
                E("dve", "tensor_tensor", p4[:, :, k0:2, :], p4[:, :, k0:2, :], amask[:, :, k0:2, :], ALU.mult, reads=[pk_, "CB"], writes=[pk_])
                pend[bid] = (pi, p4, pk_, kbs)

            def back(bid):
                g4, q4 = bid // 4, bid % 4
                if q4 == 0:
                    banks[g4] = (self.ps_get(), self.ps_get())
                (ni, nps, nk), (di, dps, dk) = banks[g4]
                pi, p4, pk_, kbs = pend.pop(bid)
                for h in range(2):
                    po = h * 64
                    for idx, kb in enumerate(kbs):
                        kbid = bid - 1 + kb
                        E("pe", "matmul", nps[po:po + 64, q4 * 128:(q4 + 1) * 128], vt[:, kbid, h * 64:(h + 1) * 64], p4[:, h, kb, :],
                          start=(idx == 0), stop=(idx == len(kbs) - 1), reads=[pk_] + allv, writes=[nk], signal=False)
                    for idx, kb in enumerate(kbs):
                        E("pe", "matmul", dps[po:po + 64, q4 * 128:(q4 + 1) * 128], ones64, p4[:, h, kb, :],
                          start=(idx == 0), stop=(idx == len(kbs) - 1), reads=[pk_, "CB"], writes=[dk],
                          signal=(h == 1 and idx == len(kbs) - 1))
                self.tbp.free(pi)
                if q4 < 3:
                    return
                if d == 1:
                    sl = slice(g4 * TT, (g4 + 1) * TT)
                    E("act", "activation", NUM[:, sl], nps[:, :], AF.Copy, reads=[nk], writes=numk)
                    E("dve", "tensor_copy", DEN[:, sl], dps[:, :], reads=[dk], writes=denk)
                else:
                    if d == 4:
                        nv_ = NUM[:, g4:S:4]
                        dv_ = DEN[:, g4:S:4]
                        pn, pd = nps[:, :], dps[:, :]
                    else:
                        nv_ = NUM.rearrange("p (i r) -> p r i", r=16)[:, g4 * 4:(g4 + 1) * 4, :]
                        dv_ = DEN.rearrange("p (i r) -> p r i", r=16)[:, g4 * 4:(g4 + 1) * 4, :]
                        pn, pd = nps.rearrange("p (r i) -> p r i", r=4), dps.rearrange("p (r i) -> p r i", r=4)
                    E("dve", "tensor_tensor", nv_, nv_, pn, ALU.add, reads=[nk] + numk, writes=numk)
                    E("dve", "tensor_tensor", dv_, dv_, pd, ALU.add, reads=[dk] + denk, writes=denk)
                self.psp.free(ni)
                self.psp.free(di)
                del banks[g4]

            front(0)
            for bid in range(1, 16):
                front(bid)
                back(bid - 1)
            back(15)
        self.wbp.free(wvi)
        for tt in range(NT):
            sl = slice(tt * TT, (tt + 1) * TT)
            E("act", "activation", DEN[:, sl], DEN[:, sl], AF.Ln, reads=denk, writes=denk)
            E("act", "activation", DEN[:, sl], DEN[:, sl], AF.Exp, scale=-1.0, reads=denk, writes=denk)
            E("dve", "tensor_tensor", self.BB[:, QR, sl], NUM[:, sl], DEN[:, sl], ALU.mult, reads=numk + denk, writes=[("BB", QR, tt)])
        for x in (KM[0], KM[1], VT, NUMb, n2, DENb, d2):
            self.bbp.free(x)
        self.out_proj("w_out", l, 512 + cc * 128, [QR])
        self.bbp.free(QR)

    def conv_group(self, l):
        E = self.E
        pb = l * PV_L
        CF, CB, PV = self.CF, self.CB, self.PV
        DGb = [self.bbp.get_pair(), self.bbp.get_pair()]
        UB = [self.bbp.get(), self.bbp.get()]
        OD = [self.bbp.get(), self.bbp.get()]
        identb = CB[:, CB_IDENT:CB_IDENT + 128]
        DG, dgk = [], []
        for cc in range(2):
            dg = self.BB[:, DGb[cc][0]:DGb[cc][0] + 2, :].rearrange("p a b -> p (a b)")[:, 0:31 * 128].rearrange("p (j c) -> p j c", c=128)
            keys = self.bbk(DGb[cc][0]) + self.bbk(DGb[cc][1])
            wc = pb + PV_CVW + cc * 31
            E("dve", "tensor_tensor", dg, identb.unsqueeze(1).to_broadcast([128, 31, 128]),
              PV[:, wc:wc + 31].unsqueeze(2).to_broadcast([128, 31, 128]), ALU.mult, reads=["CB", "PV"], writes=keys)
            DG.append(dg)
            dgk.append(keys)
        for cc in range(2):
            wa_i, wa, wak = self.load_slab(self.wcols("w_in", l, 2576 + cc * 128, 128), 8, 128)
            wg_i, wg, wgk = self.load_slab(self.wcols("w_in", l, 2832 + cc * 128, 128), 8, 128)
            for tt in range(NT):
                sl = slice(tt * TT, (tt + 1) * TT)
                pgi, pg, pgk = self.proj_fm(wg, wgk, tt)
                pai, pa, pak = self.proj_fm(wa, wak, tt)
                ti, t, tk = self.tp_get()
                E("act", "activation", t, pg[:, :], AF.Sigmoid, reads=[pgk], writes=[tk])
                E("dve", "tensor_tensor", self.BB[:, UB[cc], sl], pa[:, :], t, ALU.mult, reads=[pak, tk], writes=[("BB", UB[cc], tt)])
                self.psp.free(pgi)
                self.psp.free(pai)
                self.tpp.free(ti)
            self.wbp.free(wa_i)
            self.wbp.free(wg_i)
        ones = CB[:, CB_ONES:CB_ONES + 128]
        for tt in range(NT):
            sl = slice(tt * TT, (tt + 1) * TT)
            t0 = tt * TT
            s1i, s1, s1k = self.ps_get()
            s2i, s2, s2k = self.ps_get()
            ys = []
            for cc in range(2):
                yi, yps, ypk = self.ps_get()
                order = [30] + list(range(30))
                for idx, j in enumerate(order):
                    sh = 30 - j
                    lo = max(0, sh - t0)
                    rd = [("BB", UB[cc], tt)] + ([("BB", UB[cc], tt - 1)] if tt > 0 else [])
                    E("pe", "matmul", yps[:, lo:TT], DG[cc][:, j, :], self.BB[:, UB[cc], t0 + lo - sh:t0 + TT - sh],
                      start=(idx == 0), stop=(idx == 30), reads=dgk[cc] + rd, writes=[ypk], signal=(idx == 30))
                bcol = PV[:, pb + PV_CVB + cc:pb + PV_CVB + cc + 1]
                ai, a, ak = self.tb_get()
                bi, b, bk = self.tb_get()
                y32i, y32, y32k = self.tp_get()
                E("dve", "tensor_scalar", y32, yps[:, :], bcol, None, ALU.add, reads=[ypk, "PV"], writes=[y32k])
                self.psp.free(yi)
                E("act", "activation", a, y32, AF.Copy, reads=[y32k], writes=[ak])
                E("act", "activation", b, y32, AF.Square, reads=[y32k], writes=[bk])
                E("pe", "matmul", s1[:, :], ones, a, start=(cc == 0), stop=(cc == 1), reads=[ak, "CB"], writes=[s1k])
                E("pe", "matmul", s2[:, :], ones, b, start=(cc == 0), stop=(cc == 1), reads=[bk, "CB"], writes=[s2k])
                self.tbp.free(ai)
                self.tbp.free(bi)
                ys.append((y32i, y32, y32k))
            mi, m, mk = self.tp_get()
            vi, v, vk = self.tp_get()
            E("dve", "tensor_scalar", m, s1[:, :], 1.0 / 256, None, ALU.mult, reads=[s1k], writes=[mk])
            E("dve", "tensor_tensor", v, m, m, ALU.mult, reads=[mk], writes=[vk])
            E("dve", "scalar_tensor_tensor", v, s2[:, :], 1.0 / 256, v, ALU.mult, ALU.subtract, reads=[s2k, vk], writes=[vk])
            self.psp.free(s1i)
            self.psp.free(s2i)
            E("act", "activation", v, v, AF.Ln, bias=CF[:, CF_EPS:CF_EPS + 1], reads=[vk, "CF"], writes=[vk])
            E("act", "activation", v, v, AF.Exp, scale=-0.5, reads=[vk], writes=[vk])
            for cc in range(2):
                y32i, y32, y32k = ys[cc]
                E("dve", "tensor_tensor", y32, y32, m, ALU.subtract, reads=[y32k, mk], writes=[y32k])
                E("dve", "tensor_tensor", y32, y32, v, ALU.mult, reads=[y32k, vk], writes=[y32k])
                E("act", "activation", self.BB[:, OD[cc], sl], y32, AF.Silu, bias=PV[:, pb + PV_CVBB + cc:pb + PV_CVBB + cc + 1],
                  scale=PV[:, pb + PV_CVG + cc:pb + PV_CVG + cc + 1], reads=[y32k, "PV"], writes=[("BB", OD[cc], tt)])
                self.tpp.free(y32i)
            self.tpp.free(mi)
            self.tpp.free(vi)
        for p in DGb:
            self.bbp.free(p[0])
            self.bbp.free(p[1])
        for x in UB:
            self.bbp.free(x)
        self.out_proj("w_out", l, 768, OD)
        for x in OD:
            self.bbp.free(x)

    def cross_kv(self, l, s):
        E = self.E
        pb, db = l * PV_L, l * DP_L
        CF, CB, DP = self.CF, self.CB, self.DP
        identf = CF[:, CF_IDENT:CF_IDENT + 128]
        ones = CB[:, CB_ONES:CB_ONES + 128]
        m0, m1 = self.bbp.get_pair()
        mh = self.bbp.get()
        assert m1 == m0 + 1
        MT = self.bbf(m0).rearrange("p (c t) -> p c t", c=8)
        mtk = self.bbfk(m0)
        MH = self.BB[:, mh, :].rearrange("p (c t) -> p c t", c=8)
        mhk = self.bbk(mh)
        for mb in range(2):
            for hf in range(2):
                i, ap, key = self.tp_get()
                E("sp", "dma_start", out=ap, in_=self.mem_d[s, mb * 128:(mb + 1) * 128, hf * 512:(hf + 1) * 512], writes=[key], dma=True)
                pi, ps, pk = self.ps_get()
                for q in range(4):
                    E("pe", "transpose", ps[:, q * 128:(q + 1) * 128], ap[:, q * 128:(q + 1) * 128], identf,
                      reads=[key, "CF"], writes=[pk], signal=(q == 3))
                E("act", "activation", MT[:, hf * 4:(hf + 1) * 4, mb * 128:(mb + 1) * 128], ps.rearrange("p (c t) -> p c t", c=4), AF.Copy,
                  reads=[pk], writes=mtk)
                self.psp.free(pi)
                self.tpp.free(i)
        pi, ps, pk = self.ps_get()
        for c in range(8):
            bi, bap, bk = self.tb_get()
            E("act", "activation", bap[:, 0:MEM], MT[:, c, :], AF.Square, reads=mtk, writes=[bk])
            E("pe", "matmul", ps[:, 0:MEM], ones, bap[:, 0:MEM], start=(c == 0), stop=(c == 7), reads=[bk, "CB"], writes=[pk], signal=(c == 7))
            self.tbp.free(bi)
        ri, rap, rk = self.rstd(ps, pk, D, ncols=MEM)
        self.psp.free(pi)
        for c in range(8):
            E("dve", "scalar_tensor_tensor", MH[:, c, :], MT[:, c, :], DP[:, db + PV_MEMN + c:db + PV_MEMN + c + 1], rap[:, 0:MEM], ALU.mult, ALU.mult,
              reads=mtk + [rk, "DP"], writes=mhk)
        self.tpp.free(ri)
        for c in range(8):
            wi, w, wk = self.load_slab(self.wcols("cross_wkv", l, c * 128, 128), 8, 128)
            pi, ps, pk = self.ps_get()
            for k in range(8):
                E("pe", "matmul", ps[:, 0:MEM], w[:, k, :], MH[:, k, :], start=(k == 0), stop=(k == 7), reads=[wk] + mhk, writes=[pk], signal=(k == 7))
            E("act", "activation", self.KX[:, c, :], ps[:, 0:MEM], AF.Copy, reads=[pk], writes=[("KX", c)])
            self.psp.free(pi)
            self.wbp.free(wi)
        for vc in range(8):
            wi, w, wk = self.load_slab(self.wcols("cross_wkv", l, D + vc * 128, 128), 8, 128)
            pi, ps, pk = self.ps_get()
            for mb in range(2):
                for k in range(8):
                    E("pe", "matmul", ps[:, mb * 128:(mb + 1) * 128], MH[:, k, mb * 128:(mb + 1) * 128], w[:, k, :], start=(k == 0), stop=(k == 7),
                      reads=[wk] + mhk, writes=[pk], signal=(k == 7))
            E("dve", "tensor_copy", self.VX[:, :, vc * 128:(vc + 1) * 128], ps[:, 0:256].rearrange("p (m c) -> p m c", m=2), reads=[pk], writes=[("VX", vc)])
            self.psp.free(pi)
            self.wbp.free(wi)
        for x in (m0, m1, mh):
            self.bbp.free(x)

    def cross(self, l, s):
        E = self.E
        pb, db = l * PV_L, l * DP_L
        CF, CB, DP = self.CF, self.CB, self.DP
        self.rmsnorm_x(db + PV_CROSS)
        ones = CB[:, CB_ONES:CB_ONES + 128]
        for hp in range(2):
            QX = [self.bbp.get() for _ in range(4)]
            OX = [self.bbp.get() for _ in range(4)]
            for qi_ in range(4):
                qc = hp * 4 + qi_
                wi, w, wk = self.load_slab(self.wcols("cross_wq", l, qc * 128, 128), 8, 128)
                for tt in range(NT):
                    sl = slice(tt * TT, (tt + 1) * TT)
                    pi, ps, pk = self.proj_fm(w, wk, tt)
                    E("act", "activation", self.BB[:, QX[qi_], sl], ps[:, :], AF.Copy, reads=[pk], writes=[("BB", QX[qi_], tt)])
                    self.psp.free(pi)
                self.wbp.free(wi)
            for hh in range(2):
                h = hp * 2 + hh
                for tt in range(NT):
                    sl = slice(tt * TT, (tt + 1) * TT)
                    P = []
                    for mb in range(2):
                        si, sps, sk = self.ps_get()
                        for dc in range(2):
                            E("pe", "matmul", sps[:, :], self.KX[:, 2 * h + dc, mb * 128:(mb + 1) * 128], self.BB[:, QX[hh * 2 + dc], sl],
                              start=(dc == 0), stop=(dc == 1), reads=[("KX", 2 * h + dc), ("BB", QX[hh * 2 + dc], tt)], writes=[sk], signal=(dc == 1))
                        pi, pap, pk_ = self.tb_get()
                        E("act", "activation", pap, sps[:, :], AF.Exp, scale=1.0 / 16, reads=[sk], writes=[pk_])
                        self.psp.free(si)
                        P.append((pi, pap, pk_))
                    di, dps, dk = self.ps_get()
                    for mb in range(2):
                        E("pe", "matmul", dps[:, :], ones, P[mb][1], start=(mb == 0), stop=(mb == 1), reads=[P[mb][2], "CB"], writes=[dk], signal=(mb == 1))
                    ri, rap, rk = self.tp_get()
                    E("act", "activation", rap, dps[:, :], AF.Ln, reads=[dk], writes=[rk])
                    E("act", "activation", rap, rap, AF.Exp, scale=-1.0, reads=[rk], writes=[rk])
                    self.psp.free(di)
                    for vc in range(2):
                        oi, ops_, ok = self.ps_get()
                        col = h * 256 + vc * 128
                        for mb in range(2):
                            E("pe", "matmul", ops_[:, :], self.VX[:, mb, col:col + 128], P[mb][1], start=(mb == 0), stop=(mb == 1),
                              reads=[P[mb][2], ("VX", col // 128)], writes=[ok], signal=(mb == 1))
                        E("dve", "tensor_tensor", self.BB[:, OX[hh * 2 + vc], sl], ops_[:, :], rap, ALU.mult, reads=[ok, rk],
                          writes=[("BB", OX[hh * 2 + vc], tt)])
                        self.psp.free(oi)
                    self.tpp.free(ri)
                    for mb in range(2):
                        self.tbp.free(P[mb][0])
            for x in QX:
                self.bbp.free(x)
            self.out_proj("cross_wo", l, hp * 512, OX)
            for x in OX:
                self.bbp.free(x)


_CACHE = {}


def _get_nc(nseq=SEQ_PER_CORE, depth=DEPTH, stop=None, dbg=None, mixsel="abcd"):
    key = (nseq, depth, stop, dbg, mixsel)
    if key not in _CACHE:
        b = Builder(nseq, depth, stop, dbg, mixsel)
        _CACHE[key] = (b.build(), b)
    return _CACHE[key]


def kernel(**inputs):
    nc, _ = _get_nc()
    cf, cb = _consts()
    pv = _pack_params(inputs)
    x = np.ascontiguousarray(np.asarray(inputs["x"], np.float32))
    mem = np.ascontiguousarray(np.asarray(inputs["mem"], np.float32))
    pos = np.ascontiguousarray(np.asarray(inputs["positions"], np.int32))
    shared = {n: np.ascontiguousarray(np.asarray(inputs[n], np.float32)) for n in WNAMES}
    shared["gla_gate_w"] = np.ascontiguousarray(np.asarray(inputs["gla_gate_w"], np.float32))
    shared["pv"] = pv
    shared["cf"] = cf
    shared["cb"] = cb
    in_maps = []
    for c in range(NCORES):
        m = dict(shared)
        sl = slice(c * SEQ_PER_CORE, (c + 1) * SEQ_PER_CORE)
        m["x"] = x[sl]
        m["mem"] = mem[sl]
        m["pos"] = pos[sl]
        in_maps.append(m)
    res = run_bass_kernel_spmd(nc, in_maps, core_ids=list(range(NCORES)))
    return np.concatenate([r["y"] for r in res.results], axis=0)
```

```python
import numpy as np
import ml_dtypes
from contextlib import ExitStack
import concourse.bass as bass
import concourse.mybir as mybir
from concourse.bass_utils import run_bass_kernel_spmd

F32 = mybir.dt.float32
BF16 = mybir.dt.bfloat16
I32 = mybir.dt.int32
AF = mybir.ActivationFunctionType
ALU = mybir.AluOpType

D = 1024
S = 2048
NB = 16
DEPTH = 2
MEM = 256
DFF = 2816
INW = 3088
EPS = 1e-6
NT = 4
TT = 512
NCORES = 8
SEQ_PER_CORE = NB // NCORES


class Node:
    __slots__ = ("id", "eng", "fns", "deps", "succ", "dur", "dma", "signal", "ndep", "ready", "finish", "tok", "vc")

    def __init__(self, id, eng, dma, signal):
        self.id, self.eng, self.dma, self.signal = id, eng, dma, signal
        self.fns = []
        self.deps = set()
        self.succ = []
        self.dur = 0.0
        self.ready = 0.0
        self.finish = None
        self.tok = None
        self.vc = None


def _nelem(ap):
    n = 1
    for d in ap.shape[1:]:
        n *= d
    return n


class Em:
    ENGS = ("pe", "act", "dve", "pool", "sp")
    WINDOW = 64
    LAT = 1.3

    def __init__(self, n_dma_sems=12):
        self.nodes = []
        self.lastw = {}
        self.readers = {}
        self.open_pe = None
        self.dma_sems = {"sp": [["dma_sp_%d" % i, 0, None] for i in range(n_dma_sems)]}
        self.ninstr = 0
        self.prog = {e: [] for e in self.ENGS}

    def sem_names(self):
        return list(self.ENGS) + [s[0] for s in self.dma_sems["sp"]]

    @staticmethod
    def _cost(eng, fn, dma):
        op, args, kw = fn
        try:
            if dma:
                return 2.0 + _nelem(kw["out"]) * 128 * 4 / 150e3
            n = _nelem(args[0])
            if eng == "pe":
                return 0.035 + n / 2800.0
            if eng == "act":
                return 0.2 + n / 1200.0
            if eng == "dve":
                return 0.1 + (2 * n if op == "tensor_tensor_scan" else n) / 960.0
            return 0.1 + n / 1700.0
        except Exception:
            return 0.5

    def emit(self, eng, fn, reads=(), writes=(), signal=True, dma=False):
        self.ninstr += 1
        if eng == "pe" and self.open_pe is not None:
            g = self.open_pe.id
            fwd = False
            for r in reads:
                t = self.lastw.get(r)
                if t is not None and t > g:
                    fwd = True
            for w in writes:
                t = self.lastw.get(w)
                if t is not None and t > g:
                    fwd = True
                for t in self.readers.get(w, ()):
                    if t > g:
                        fwd = True
            if fwd:
                self.open_pe = None
        if eng == "pe" and self.open_pe is not None:
            node = self.open_pe
        else:
            node = Node(len(self.nodes), eng, dma, True)
            self.nodes.append(node)
        node.fns.append(fn)
        node.dur += self._cost(eng, fn, dma)
        nid = node.id
        for r in reads:
            t = self.lastw.get(r)
            if t is not None and t != nid:
                node.deps.add(t)
        for w in writes:
            t = self.lastw.get(w)
            if t is not None and t != nid:
                node.deps.add(t)
            for t in self.readers.get(w, ()):
                if t != nid:
                    node.deps.add(t)
        for w in writes:
            self.lastw[w] = nid
            self.readers[w] = []
        for r in reads:
            lst = self.readers.setdefault(r, [])
            if not lst or lst[-1] != nid:
                lst.append(nid)
        if eng == "pe":
            self.open_pe = None if signal else node
        return nid

    def wait_all(self, eng, toks):
        node = Node(len(self.nodes), eng, False, False)
        self.nodes.append(node)
        node.deps = set(toks)
        node.dur = 0.05
        return node.id

    def finalize(self):
        assert self.open_pe is None
        nodes = self.nodes
        for n in nodes:
            assert all(d < n.id for d in n.deps), "forward dependency"
            n.ndep = len(n.deps)
            for d in n.deps:
                nodes[d].succ.append(n.id)
        queues = {e: [n.id for n in nodes if n.eng == e] for e in self.ENGS}
        heads = {e: 0 for e in self.ENGS}
        free = {e: 0.0 for e in self.ENGS}
        done = [False] * len(nodes)
        order = []
        remaining = len(nodes)
        W, LAT = self.WINDOW, self.LAT
        bl = [0.0] * len(nodes)
        for n in reversed(nodes):
            m = 0.0
            for sid in n.succ:
                v = bl[sid] + (LAT if nodes[sid].eng != n.eng else 0.06)
                if v > m:
                    m = v
            bl[n.id] = n.dur + m
        while remaining:
            best = None
            for e in self.ENGS:
                q = queues[e]
                i = heads[e]
                seen = 0
                fe = free[e]
                while i < len(q) and seen < W:
                    nid = q[i]
                    i += 1
                    if done[nid]:
                        continue
                    seen += 1
                    n = nodes[nid]
                    if n.ndep:
                        continue
                    st = n.ready if n.ready > fe else fe
                    key = (round(st / 0.3), -bl[nid], nid)
                    if best is None or key < best[0]:
                        best = (key, st, nid, e)
            assert best is not None, "scheduler deadlock"
            _, st, nid, e = best
            n = nodes[nid]
            n.finish = st + n.dur
            free[e] = n.finish if not n.dma else st + 0.15
            done[nid] = True
            order.append(nid)
            remaining -= 1
            q = queues[e]
            while heads[e] < len(q) and done[q[heads[e]]]:
                heads[e] += 1
            for sid in n.succ:
                sn = nodes[sid]
                sn.ndep -= 1
                r = n.finish + (LAT if sn.eng != e else 0.06)
                if r > sn.ready:
                    sn.ready = r
        self.est_us = max(n.finish for n in nodes)
        cnt = {e: 0 for e in self.ENGS}
        clock = {e: {} for e in self.ENGS}
        rr = 0
        for nid in order:
            n = nodes[nid]
            eng = n.eng
            clk = clock[eng]
            waits = {}
            for d in n.deps:
                t = nodes[d].tok
                if eng == "pe" and t[0] == "pe":
                    continue
                if clk.get(t[0], 0) < t[1]:
                    if waits.get(t[0], 0) < t[1]:
                        waits[t[0]] = t[1]
            for d in n.deps:
                dn = nodes[d]
                if eng == "pe" and dn.tok[0] == "pe":
                    continue
                for k, v in dn.vc.items():
                    if clk.get(k, 0) < v:
                        clk[k] = v
                if clk.get(dn.tok[0], 0) < dn.tok[1]:
                    clk[dn.tok[0]] = dn.tok[1]
            inc = None
            if n.dma:
                lst = self.dma_sems["sp"]
                ent = lst[rr]
                rr = (rr + 1) % len(lst)
                name, cur = ent[0], ent[1]
                if cur > 0 and clk.get(name, 0) < cur:
                    waits[name] = max(waits.get(name, 0), cur)
                    clk[name] = cur
                ent[1] = cur + 16
                n.tok = (name, cur + 16)
                inc = (name, 16)
            elif n.fns:
                cnt[eng] += 1
                n.tok = (eng, cnt[eng])
                inc = (eng, 1)
            else:
                n.tok = (eng, cnt[eng])
            n.vc = dict(clk)
            self.prog[eng].append((tuple(waits.items()), n.fns, inc))

    def replay(self, eng, e, sems):
        for waits, fns, inc in self.prog[eng]:
            for name, val in waits:
                e.wait_ge(sems[name], val)
            ins = None
            for fn in fns:
                ins = getattr(e, fn[0])(*fn[1], **fn[2])
            if inc is not None and ins is not None:
                ins.then_inc(sems[inc[0]], inc[1])


class Pool:
    def __init__(self, name, aps):
        self.name = name
        self.aps = aps
        self.held = [False] * len(aps)
        self.rr = 0

    def get(self):
        n = len(self.aps)
        for k in range(n):
            i = (self.rr + k) % n
            if not self.held[i]:
                self.held[i] = True
                self.rr = (i + 1) % n
                return i
        raise RuntimeError("pool %s exhausted" % self.name)

    def get_pair(self):
        n = len(self.aps)
        for k in range(n):
            i = (self.rr + k) % n
            if i + 1 < n and not self.held[i] and not self.held[i + 1]:
                self.held[i] = self.held[i + 1] = True
                self.rr = (i + 2) % n
                return i, i + 1
        raise RuntimeError("pool %s: no free pair" % self.name)

    def free(self, i):
        assert self.held[i]
        self.held[i] = False

    def key(self, i):
        return (self.name, i)


CF_IDENT = 0
CF_MASKA = 128
CF_MASKB = 256
CF_HMA = 512
CF_HMB = 514
CF_ROPE = 518
CF_EPS = 522
CF_N = 523

CB_IDENT = 0
CB_ONES = 128
CB_BLK64 = 256
CB_CMASK = 384
CB_AMASK = 512
CB_SCANM = 1024
CB_N = 1536


def _consts():
    cf = np.zeros((128, CF_N), np.float32)
    p = np.arange(128)
    cf[:, CF_IDENT:CF_IDENT + 128] = np.eye(128, dtype=np.float32)
    cf[:, CF_MASKA:CF_MASKA + 128] = (p[:, None] // 64 == np.arange(128)[None, :] // 64)
    cf[:, CF_MASKB:CF_MASKB + 256] = (p[:, None] // 32 == np.arange(256)[None, :] // 64)
    for h in range(2):
        cf[:, CF_HMA + h] = (p // 64 == h)
    for h in range(4):
        cf[:, CF_HMB + h] = (p // 32 == h)
    dd = p % 64
    j = dd % 8
    invf = np.power(np.float32(500000.0), -np.arange(0, 16, 2, dtype=np.float32) / np.float32(16)).astype(np.float32)
    rot = dd < 16
    pi = np.float32(np.pi)
    cf[:, CF_ROPE + 0] = np.where(rot, invf[j], 0.0)
    cf[:, CF_ROPE + 1] = np.float32(np.pi / 2)
    cf[:, CF_ROPE + 2] = np.where(rot, invf[j], 0.0)
    cf[:, CF_ROPE + 3] = np.where(dd < 8, pi, 0.0)
    cf[:, CF_EPS] = EPS
    cb = np.zeros((128, CB_N), np.float32)
    cb[:, CB_IDENT:CB_IDENT + 128] = np.eye(128)
    cb[:, CB_ONES:CB_ONES + 128] = 1.0
    cb[:, CB_BLK64:CB_BLK64 + 128] = (p[:, None] // 64 == np.arange(128)[None, :] // 64)
    s_ = p[:, None]
    t_ = np.arange(128)[None, :]
    cb[:, CB_CMASK:CB_CMASK + 128] = (s_ // 64 == t_ // 64) & (s_ <= t_)
    am = np.zeros((128, 2, 2, 128), np.float32)
    am[:, :, 0, :] = (s_ >= t_)[:, None, :]
    am[:, :, 1, :] = (s_ <= t_)[:, None, :]
    cb[:, CB_AMASK:CB_AMASK + 512] = am.reshape(128, 512)
    cb[:, CB_SCANM:CB_SCANM + 512] = (np.arange(512)[None, :] % 64 != 0)
    return cf, cb.astype(ml_dtypes.bfloat16)


PV_FFN1 = 0
PV_MIX = 8
PV_CROSS = 16
PV_MEMN = 24
PV_FFN2 = 32
PV_LBLOG = 40
PV_HGN = 42
PV_GLN = 44
PV_GLB = 46
PV_CVB = 47
PV_CVG = 49
PV_CVBB = 51
PV_CVW = 53
PV_L = 115
PV_FINAL = DEPTH * PV_L
PV_N = PV_FINAL + 8


def _fm(v, ncol):
    return np.ascontiguousarray(np.asarray(v, np.float32).reshape(ncol, 128).T)


def _pack_params(inp):
    pv = np.zeros((128, PV_N), np.float32)
    for l in range(DEPTH):
        b = l * PV_L
        pv[:, b + PV_FFN1:b + PV_FFN1 + 8] = _fm(inp["ffn1_norm"][l], 8)
        pv[:, b + PV_MIX:b + PV_MIX + 8] = _fm(inp["mix_norm"][l], 8)
        pv[:, b + PV_CROSS:b + PV_CROSS + 8] = _fm(inp["cross_norm"][l], 8)
        pv[:, b + PV_MEMN:b + PV_MEMN + 8] = _fm(inp["mem_norm"][l], 8)
        pv[:, b + PV_FFN2:b + PV_FFN2 + 8] = _fm(inp["ffn2_norm"][l], 8)
        pv[:, b + PV_LBLOG:b + PV_LBLOG + 2] = _fm(inp["hgrn_lb_logits"][l], 2)
        pv[:, b + PV_HGN:b + PV_HGN + 2] = _fm(inp["hgrn_out_norm"][l], 2)
        pv[:, b + PV_GLN:b + PV_GLN + 2] = _fm(inp["gla_out_norm"][l], 2)
        pv[:, b + PV_GLB:b + PV_GLB + 1] = _fm(inp["gla_gate_b"][l], 1)
        pv[:, b + PV_CVB:b + PV_CVB + 2] = _fm(inp["conv_b"][l], 2)
        pv[:, b + PV_CVG:b + PV_CVG + 2] = _fm(inp["conv_ln_g"][l], 2)
        pv[:, b + PV_CVBB:b + PV_CVBB + 2] = _fm(inp["conv_ln_b"][l], 2)
        cw = np.asarray(inp["conv_w"][l], np.float32)
        for cc in range(2):
            pv[:, b + PV_CVW + cc * 31:b + PV_CVW + cc * 31 + 31] = cw[:, cc * 128:(cc + 1) * 128].T
    pv[:, PV_FINAL:PV_FINAL + 8] = _fm(inp["final_norm"], 8)
    return pv


WNAMES = ["ffn1_w_up", "ffn1_w_down", "w_in", "w_out", "cross_wq", "cross_wkv", "cross_wo",
          "ffn2_w_up", "ffn2_w_down"]
WSHAPES = {"ffn1_w_up": [DEPTH, D, 2 * DFF], "ffn1_w_down": [DEPTH, DFF, D], "w_in": [DEPTH, D, INW],
           "w_out": [DEPTH, D, D], "cross_wq": [DEPTH, D, D], "cross_wkv": [DEPTH, D, 2 * D],
           "cross_wo": [DEPTH, D, D], "ffn2_w_up": [DEPTH, D, 2 * DFF], "ffn2_w_down": [DEPTH, DFF, D]}

NBB = 10
NTP = 6
NTB = 8
NWS = 3
NWB = 5

DP_G = 0
DP_L = 48
DP_OML = 40
DP_LBF = 42
DP_HGN = 44
DP_GLN = 46
DP_FINAL = DEPTH * DP_L
DP_N = DP_FINAL + 8


class Builder:
    def __init__(self, nseq=SEQ_PER_CORE, depth=DEPTH, stop=None, dbg=None, mixsel="abcd"):
        self.nseq, self.depth, self.stop, self.dbgspec, self.mixsel = nseq, depth, stop, dbg, mixsel
        self.em = Em()
        self.nc = bass.Bass("TRN2", target_bir_lowering=False)
        self.out_toks = []

    def E(self, eng, op, *args, reads=(), writes=(), signal=True, dma=False, **kw):
        return self.em.emit(eng, (op, args, kw), reads, writes, signal=signal, dma=dma)

    def bbk(self, i, a=0, b=S):
        return [("BB", i, t) for t in range(a // TT, (b - 1) // TT + 1)]

    def bbf(self, i):
        return self.BB[:, i:i + 2, :].rearrange("p a b -> p (a b)").bitcast(F32)

    def bbfk(self, i):
        return self.bbk(i) + self.bbk(i + 1)

    def xk(self, c, tt):
        return ("X", c, tt)

    def hk(self, c, tt):
        return ("H", c, tt)

    def hks(self, tt):
        return [("H", c, tt) for c in range(8)]

    def load_slab(self, src_ap, kc, cols):
        assert kc * cols <= 1024
        n = kc * cols
        i = self.ws_rr
        self.ws_rr = (i + 1) % NWS
        j = self.wbp.get()
        ws = self.WS[:, i, 0:n].rearrange("p (k c) -> p k c", k=kc)
        self.E("sp", "dma_start", out=ws, in_=src_ap, writes=[("WS", i)], dma=True)
        self.E("pool", "tensor_copy", self.WB[:, j, 0:n], self.WS[:, i, 0:n], reads=[("WS", i)], writes=[("WB", j)])
        return j, self.WB[:, j, 0:n].rearrange("p (k c) -> p k c", k=kc), ("WB", j)

    def wcols(self, name, l, c0, ncols, r0=0, nrows=D):
        w = self.W[name]
        return w[l, r0:r0 + nrows, c0:c0 + ncols].rearrange("(k p) c -> p k c", p=128)

    def ps_get(self):
        i = self.psp.get()
        return i, self.PS[i], ("PS", i)

    def tp_get(self):
        i = self.tpp.get()
        return i, self.TP[:, i, :], ("TP", i)

    def tb_get(self):
        i = self.tbp.get()
        return i, self.TB[:, i, :], ("TB", i)

    def rstd(self, ps, pk, n, ncols=TT):
        ri, rap, rk = self.tp_get()
        self.E("act", "activation", rap[:, 0:ncols], ps[:, 0:ncols], AF.Ln, bias=self.CF[:, CF_EPS:CF_EPS + 1], scale=1.0 / n,
               reads=[pk, "CF"], writes=[rk])
        self.E("act", "activation", rap[:, 0:ncols], rap[:, 0:ncols], AF.Exp, scale=-0.5, reads=[rk], writes=[rk])
        return ri, rap, rk

    def proj_fm(self, w, wk, tt, m=128):
        sl = slice(tt * TT, (tt + 1) * TT)
        pi, ps, pk = self.ps_get()
        for k in range(8):
            self.E("pe", "matmul", ps[0:m, :], w[:, k, 0:m], self.H[:, k, sl], start=(k == 0), stop=(k == 7),
                   reads=[wk, self.hk(k, tt)], writes=[pk], signal=(k == 7))
        return pi, ps, pk

    def build(self):
        nc = self.nc
        ns = self.nseq
        self.x_d = nc.dram_tensor("x", [ns, S, D], F32, kind="ExternalInput").ap()
        self.mem_d = nc.dram_tensor("mem", [ns, MEM, D], F32, kind="ExternalInput").ap()
        self.pos_d = nc.dram_tensor("pos", [ns, S], I32, kind="ExternalInput").ap()
        self.W = {n: nc.dram_tensor(n, WSHAPES[n], F32, kind="ExternalInput").ap() for n in WNAMES}
        self.gw_d = nc.dram_tensor("gla_gate_w", [DEPTH, 16, 128], F32, kind="ExternalInput").ap()
        self.pv_d = nc.dram_tensor("pv", [128, PV_N], F32, kind="ExternalInput").ap()
        self.cf_d = nc.dram_tensor("cf", [128, CF_N], F32, kind="ExternalInput").ap()
        self.cb_d = nc.dram_tensor("cb", [128, CB_N], BF16, kind="ExternalInput").ap()
        self.y_d = nc.dram_tensor("y", [ns, S, D], F32, kind="ExternalOutput").ap()
        with ExitStack() as st:
            def sb(name, shape, dt):
                return st.enter_context(nc.sbuf_tensor(name, shape, dt))
            self.X = sb("X", [128, 8, S], F32)
            self.H = sb("H", [128, 8, S], BF16)
            self.BB = sb("BB", [128, NBB, S], BF16)
            self.WS = sb("WS", [128, NWS, 1024], F32)
            self.WB = sb("WB", [128, NWB, 1024], BF16)
            self.TP = sb("TP", [128, NTP, TT], F32)
            self.TB = sb("TB", [128, NTB, TT], BF16)
            self.ROPE = sb("ROPE", [128, 2, S], BF16)
            self.KX = sb("KX", [128, 8, MEM], BF16)
            self.VX = sb("VX", [128, 2, D], BF16)
            self.CF = sb("CF", [128, CF_N], F32)
            self.CB = sb("CB", [128, CB_N], BF16)
            self.PV = sb("PV", [128, PV_N], F32)
            self.DP = sb("DP", [128, DP_N], F32)
            self.GWS = sb("GWS", [16, DEPTH, 128], F32)
            self.GW = sb("GW", [16, DEPTH, 128], BF16)
            self.SS = sb("SS", [128, 256], F32)
            self.D32 = sb("D32", [128, 2, 256], F32)
            self.D16 = sb("D16", [128, 4, 256], BF16)
            self.EB = sb("EB", [128, 64], F32)
            self.SM = sb("SM", [128, 64], F32)
            self.PS = [st.enter_context(nc.psum_tensor("ps%d" % i, [128, TT], F32)) for i in range(8)]
            self.psp = Pool("PS", self.PS)
            self.tpp = Pool("TP", list(range(NTP)))
            self.tbp = Pool("TB", list(range(NTB)))
            self.wbp = Pool("WB", list(range(NWB)))
            self.bbp = Pool("BB", list(range(NBB)))
            self.ws_rr = 0
            sems = {n: st.enter_context(nc.semaphore(n)) for n in self.em.sem_names()}
            self.program()
            self.em.finalize()
            block = st.enter_context(nc.Block())
            em = self.em

            @block.sync
            def _(e):
                em.replay("sp", e, sems)

            @block.gpsimd
            def _(e):
                em.replay("pool", e, sems)

            @block.tensor
            def _(e):
                em.replay("pe", e, sems)

            @block.vector
            def _(e):
                em.replay("dve", e, sems)

            @block.scalar
            def _(e):
                em.replay("act", e, sems)
        return nc

    def program(self):
        E = self.E
        CF, CB, PV, DP = self.CF, self.CB, self.PV, self.DP
        E("sp", "dma_start", out=CF[:, :], in_=self.cf_d[:, :], writes=["CF"], dma=True)
        E("sp", "dma_start", out=CB[:, :], in_=self.cb_d[:, :], writes=["CB"], dma=True)
        E("sp", "dma_start", out=PV[:, :], in_=self.pv_d[:, :], writes=["PV"], dma=True)
        E("sp", "dma_start", out=self.GWS[:, :, :], in_=self.gw_d.rearrange("l r c -> r l c"), writes=["GWS"], dma=True)
        E("pool", "tensor_copy", self.GW[:, :, :], self.GWS[:, :, :], reads=["GWS"], writes=["GW"])
        for l in range(self.depth):
            pb, db = l * PV_L, l * DP_L
            E("dve", "tensor_copy", DP[:, db:db + 40], PV[:, pb:pb + 40], reads=["PV"], writes=["DP"])
            E("dve", "tensor_copy", DP[:, db + DP_HGN:db + DP_HGN + 4], PV[:, pb + PV_HGN:pb + PV_HGN + 4], reads=["PV"], writes=["DP"])
            if l == 0:
                E("dve", "memset", DP[:, db + DP_OML:db + DP_OML + 2], 1.0, writes=["DP"])
                E("dve", "memset", DP[:, db + DP_LBF:db + DP_LBF + 2], 1e-20, writes=["DP"])
            else:
                E("dve", "tensor_tensor", self.SM[:, 0:2], PV[:, pb + PV_LBLOG:pb + PV_LBLOG + 2], PV[:, PV_LBLOG:PV_LBLOG + 2], ALU.subtract,
                  reads=["PV"], writes=["SM"])
                E("act", "activation", self.SM[:, 2:4], self.SM[:, 0:2], AF.Sigmoid, reads=["SM"], writes=["SM"])
                E("dve", "tensor_scalar", DP[:, db + DP_OML:db + DP_OML + 2], self.SM[:, 2:4], -1.0, 1.0, ALU.mult, ALU.add,
                  reads=["SM"], writes=["DP"])
                E("dve", "tensor_scalar", DP[:, db + DP_LBF:db + DP_LBF + 2], self.SM[:, 2:4], 1e-20, None, ALU.max,
                  reads=["SM"], writes=["DP"])
        E("dve", "tensor_copy", DP[:, DP_FINAL:DP_FINAL + 8], PV[:, PV_FINAL:PV_FINAL + 8], reads=["PV"], writes=["DP"])

        for s in range(self.nseq):
            self.load_x(s)
            if self.stop == "load":
                self.final(s, norm=False)
                continue
            self.rope_tables(s)
            for l in range(self.depth):
                last = (l == self.depth - 1)
                self.ffn(l, "ffn1", PV_FFN1)
                if last and self.stop == "ffn1":
                    break
                self.mixer(l, s)
                if last and self.stop == "mixer":
                    break
                self.cross(l, s)
                if last and self.stop == "cross":
                    break
                self.ffn(l, "ffn2", PV_FFN2)
            self.final(s)
        self.em.wait_all("sp", self.out_toks)

    def load_x(self, s):
        E = self.E
        ident = self.CF[:, CF_IDENT:CF_IDENT + 128]
        for tb in range(S // 128):
            halves = []
            for hf in range(2):
                i, ap, key = self.tp_get()
                E("sp", "dma_start", out=ap, in_=self.x_d[s, tb * 128:(tb + 1) * 128, hf * 512:(hf + 1) * 512], writes=[key], dma=True)
                halves.append((i, ap, key))
            for hf in range(2):
                i, ap, key = halves[hf]
                pi, ps, pk = self.ps_get()
                for q in range(4):
                    E("pe", "transpose", ps[:, q * 128:(q + 1) * 128], ap[:, q * 128:(q + 1) * 128], ident,
                      reads=[key, "CF"], writes=[pk], signal=(q == 3))
                c0 = hf * 4
                tt = tb // 4
                dst = self.X[:, c0:c0 + 4, tb * 128:(tb + 1) * 128]
                src = ps.rearrange("p (c t) -> p c t", c=4)
                wr = [self.xk(c, tt) for c in range(c0, c0 + 4)]
                if hf == 0:
                    E("act", "activation", dst, src, AF.Copy, reads=[pk], writes=wr)
                else:
                    E("dve", "tensor_copy", dst, src, reads=[pk], writes=wr)
                self.psp.free(pi)
                self.tpp.free(i)

    def rmsnorm_x(self, gcol):
        E = self.E
        ones = self.CB[:, CB_ONES:CB_ONES + 128]
        for tt in range(NT):
            sl = slice(tt * TT, (tt + 1) * TT)
            pi, ps, pk = self.ps_get()
            for c in range(8):
                bi, bap, bk = self.tb_get()
                E("act", "activation", bap, self.X[:, c, sl], AF.Square, reads=[self.xk(c, tt)], writes=[bk])
                E("pe", "matmul", ps[:, :], ones, bap, start=(c == 0), stop=(c == 7), reads=[bk, "CB"], writes=[pk], signal=(c == 7))
                self.tbp.free(bi)
            ri, rap, rk = self.rstd(ps, pk, D)
            self.psp.free(pi)
            for c in range(8):
                E("dve", "scalar_tensor_tensor", self.H[:, c, sl], self.X[:, c, sl], self.DP[:, gcol + c:gcol + c + 1], rap, ALU.mult, ALU.mult,
                  reads=[self.xk(c, tt), rk, "DP"], writes=[self.hk(c, tt)])
            self.tpp.free(ri)

    def out_proj(self, wname, l, r0, bufs, scale=1.0):
        E = self.E
        n = len(bufs)
        for dc in range(8):
            di, dw, dk = self.load_slab(self.wcols(wname, l, dc * 128, 128, r0=r0, nrows=n * 128), n, 128)
            for tt in range(NT):
                sl = slice(tt * TT, (tt + 1) * TT)
                pa, psa, pka = self.ps_get()
                for k in range(n):
                    E("pe", "matmul", psa[:, :], dw[:, k, :], self.BB[:, bufs[k], sl], start=(k == 0), stop=(k == n - 1),
                      reads=[dk, ("BB", bufs[k], tt)], writes=[pka], signal=(k == n - 1))
                E("dve", "scalar_tensor_tensor", self.X[:, dc, sl], psa[:, :], float(scale), self.X[:, dc, sl], ALU.mult, ALU.add,
                  reads=[pka, self.xk(dc, tt)], writes=[self.xk(dc, tt)])
                self.psp.free(pa)
            self.wbp.free(di)

    def ffn(self, l, pre, pvcol):
        E = self.E
        self.rmsnorm_x(l * DP_L + pvcol)
        wup, wdn = pre + "_w_up", pre + "_w_down"
        groups = [(0, 8), (8, 16), (16, 22)]
        for (fa, fb) in groups:
            n = fb - fa
            bufs = [self.bbp.get() for _ in range(n)]
            for j in range(fa, fb):
                gi, gw, gk = self.load_slab(self.wcols(wup, l, j * 128, 128), 8, 128)
                ui, uw, uk = self.load_slab(self.wcols(wup, l, DFF + j * 128, 128), 8, 128)
                bbi = bufs[j - fa]
                for tt in range(NT):
                    sl = slice(tt * TT, (tt + 1) * TT)
                    pa, psa, pka = self.proj_fm(gw, gk, tt)
                    pb, psb, pkb = self.proj_fm(uw, uk, tt)
                    ti, tap, tk = self.tp_get()
                    E("act", "activation", tap, psa[:, :], AF.Silu, reads=[pka], writes=[tk])
                    E("dve", "tensor_tensor", self.BB[:, bbi, sl], psb[:, :], tap, ALU.mult, reads=[tk, pkb], writes=[("BB", bbi, tt)])
                    self.psp.free(pa)
                    self.psp.free(pb)
                    self.tpp.free(ti)
                self.wbp.free(gi)
                self.wbp.free(ui)
            self.out_proj(wdn, l, fa * 128, bufs, scale=0.5)
            for b in bufs:
                self.bbp.free(b)

    def final(self, s, norm=True):
        E = self.E
        identf = self.CF[:, CF_IDENT:CF_IDENT + 128]
        ones = self.CB[:, CB_ONES:CB_ONES + 128]
        gcol = DP_FINAL
        for tt in range(NT):
            sl = slice(tt * TT, (tt + 1) * TT)
            if norm:
                pi, ps, pk = self.ps_get()
                for c in range(8):
                    bi, bap, bk = self.tb_get()
                    E("act", "activation", bap, self.X[:, c, sl], AF.Square, reads=[self.xk(c, tt)], writes=[bk])
                    E("pe", "matmul", ps[:, :], ones, bap, start=(c == 0), stop=(c == 7), reads=[bk, "CB"], writes=[pk], signal=(c == 7))
                    self.tbp.free(bi)
                ri, rap, rk = self.rstd(ps, pk, D)
                self.psp.free(pi)
                for c in range(8):
                    E("dve", "scalar_tensor_tensor", self.X[:, c, sl], self.X[:, c, sl], self.DP[:, gcol + c:gcol + c + 1], rap, ALU.mult, ALU.mult,
                      reads=[self.xk(c, tt), rk, "DP"], writes=[self.xk(c, tt)])
                self.tpp.free(ri)
            for q in range(4):
                tb = tt * 4 + q
                for hf in range(2):
                    pi, ps, pk = self.ps_get()
                    for c4 in range(4):
                        c = hf * 4 + c4
                        E("pe", "transpose", ps[:, c4 * 128:(c4 + 1) * 128], self.X[:, c, tb * 128:(tb + 1) * 128], identf,
                          reads=[self.xk(c, tt), "CF"], writes=[pk], signal=(c4 == 3))
                    oi, oap, ok = self.tp_get()
                    if hf == 0:
                        E("act", "activation", oap, ps[:, :], AF.Copy, reads=[pk], writes=[ok])
                    else:
                        E("dve", "tensor_copy", oap, ps[:, :], reads=[pk], writes=[ok])
                    self.psp.free(pi)
                    t = E("sp", "dma_start", out=self.y_d[s, tb * 128:(tb + 1) * 128, hf * 512:(hf + 1) * 512], in_=oap, reads=[ok], dma=True)
                    self.out_toks.append(t)
                    self.tpp.free(oi)

    def rope_tables(self, s):
        E = self.E
        b = list(self.bbp.get_pair()) + list(self.bbp.get_pair()) + list(self.bbp.get_pair())
        posi = self.BB[:, b[0]:b[0] + 2, :].rearrange("p a b -> p (a b)").bitcast(I32)
        kint = self.BB[:, b[4]:b[4] + 2, :].rearrange("p a b -> p (a b)").bitcast(I32)
        posf = self.bbf(b[2])
        u = self.bbf(b[0])
        kf = self.bbf(b[4])
        k01, k23, k45 = self.bbfk(b[0]), self.bbfk(b[2]), self.bbfk(b[4])
        assert b[1] == b[0] + 1 and b[3] == b[2] + 1 and b[5] == b[4] + 1
        E("sp", "dma_start", out=posi, in_=self.pos_d[s:s + 1, :].to_broadcast([128, S]), writes=k01, dma=True)
        E("dve", "tensor_copy", posf, posi, reads=k01, writes=k23)
        twopi = float(2 * np.pi)
        for j in range(2):
            invc = self.CF[:, CF_ROPE + 2 * j:CF_ROPE + 2 * j + 1]
            phic = self.CF[:, CF_ROPE + 2 * j + 1:CF_ROPE + 2 * j + 2]
            E("dve", "tensor_scalar", u, posf, invc, phic, ALU.mult, ALU.add, reads=k23 + ["CF"], writes=k01)
            E("dve", "tensor_scalar", kint, u, 1.0 / twopi, None, ALU.mult, reads=k01, writes=k45)
            E("dve", "tensor_copy", kf, kint, reads=k45, writes=k45)
            E("dve", "scalar_tensor_tensor", u, kf, -twopi, u, ALU.mult, ALU.add, reads=k45 + k01, writes=k01)
            E("dve", "tensor_scalar", u, u, float(np.pi), float(-np.pi), ALU.min, ALU.max, reads=k01, writes=k01)
            E("act", "activation", self.ROPE[:, j, :], u, AF.Sin, reads=k01, writes=[("ROPE", j)])
        for x in b:
            self.bbp.free(x)

    def mixer(self, l, s):
        self.rmsnorm_x(l * DP_L + PV_MIX)
        self.cross_kv(l, s)
        if "a" in self.mixsel:
            gens = [self.gla_group(l, "A", 0, 0), self.gla_group(l, "A", 1, 1)]
            while gens:
                for gen in list(gens):
                    try:
                        next(gen)
                    except StopIteration:
                        gens.remove(gen)
        if "b" in self.mixsel:
            for _ in self.gla_group(l, "B", 0, 0):
                pass
        if "c" in self.mixsel:
            self.attn_group(l, 0)
            self.attn_group(l, 1)
        if "d" in self.mixsel:
            self.conv_group(l)

    def gla_group(self, l, kind, cc, slot=0):
        E = self.E
        pb, db = l * PV_L, l * DP_L
        CF, CB, DP, PV = self.CF, self.CB, self.DP, self.PV
        if kind == "A":
            nh, nv = 2, 128
            qc0, zc0, vc0, gc0 = 0 + cc * 128, 256 + cc * 128, 512 + cc * 128, 768 + cc * 128
            wrow = cc * 128
            gain0 = db + DP_HGN + cc
            hmcol = CF_HMA
            smask = CF[:, CF_MASKA:CF_MASKA + 128]
            sc_e = 1.0
            qscale = 1.0
        else:
            nh, nv = 4, 256
            qc0, kc0, vc0, lrc0, gc0 = 1024, 1152, 1280, 1536, 1552
            wrow = 256
            gain0 = db + DP_GLN
            hmcol = CF_HMB
            smask = CF[:, CF_MASKB:CF_MASKB + 256]
            sc_e = 1.0 / 16.0
            qscale = float(32 ** -0.5)
        noc = nv // 128
        QT, KT, KTT = self.bbp.get(), self.bbp.get(), self.bbp.get()
        VT = [self.bbp.get() for _ in range(noc)]
        OA = [self.bbp.get() for _ in range(noc)]
        ktt = self.BB[:, KTT, :].rearrange("p (b c) -> p b c", c=128)
        vkeys_all = [k for v in VT for k in self.bbk(v)]
        scanm = CB[:, CB_SCANM:CB_SCANM + TT]
        identb = CB[:, CB_IDENT:CB_IDENT + 128]

        wq_i, wq, wqk = self.load_slab(self.wcols("w_in", l, qc0, 128), 8, 128)
        if kind == "A":
            wz_i, wz, wzk = self.load_slab(self.wcols("w_in", l, zc0, 128), 8, 128)
        else:
            wk_i, wk, wkk = self.load_slab(self.wcols("w_in", l, kc0, 128), 8, 128)
            wl_i, wl, wlk = self.load_slab(self.wcols("w_in", l, lrc0, 16), 8, 16)
        wv = [self.load_slab(self.wcols("w_in", l, vc0 + a * 128, 128), 8, 128) for a in range(noc)]
        for tt in range(NT):
            sl = slice(tt * TT, (tt + 1) * TT)
            t0i, t0, t0k = self.tp_get()
            if kind == "A":
                t1i, t1, t1k = self.tp_get()
                pi, ps, pk = self.proj_fm(wz, wzk, tt)
                E("act", "activation", t0, ps[:, :], AF.Sigmoid, reads=[pk], writes=[t0k])
                E("act", "activation", t1, ps[:, :], AF.Sigmoid, scale=-1.0, reads=[pk], writes=[t1k])
                self.psp.free(pi)
                E("dve", "tensor_scalar", t0, t0, DP[:, db + DP_OML + cc:db + DP_OML + cc + 1], DP[:, db + DP_LBF + cc:db + DP_LBF + cc + 1],
                  ALU.mult, ALU.add, reads=[t0k, "DP"], writes=[t0k])
                E("act", "activation", t0, t0, AF.Ln, reads=[t0k], writes=[t0k])
            else:
                pi, ps, pk = self.proj_fm(wl, wlk, tt, m=16)
                li, lap, lk = self.tb_get()
                E("act", "activation", lap[0:16, :], ps[0:16, :], AF.Copy, reads=[pk], writes=[lk])
                self.psp.free(pi)
                pi, ps, pk = self.ps_get()
                E("pe", "matmul", ps[:, :], self.GW[:, l, :], lap[0:16, :], start=True, stop=True, reads=["GW", lk], writes=[pk])
                self.tbp.free(li)
                E("act", "activation", t0, ps[:, :], AF.Sigmoid, bias=PV[:, pb + PV_GLB:pb + PV_GLB + 1], reads=[pk, "PV"], writes=[t0k])
                self.psp.free(pi)
                E("act", "activation", t0, t0, AF.Ln, reads=[t0k], writes=[t0k])
            t2i, t2, t2k = self.tp_get()
            t3i, t3, t3k = self.tp_get()
            E("dve", "tensor_tensor_scan", t2, scanm, t0, 0.0, ALU.mult, ALU.add, reads=[t0k, "CB"], writes=[t2k])
            b3 = t2.rearrange("p (a b) -> p a b", b=64)
            E("act", "activation", self.EB[:, slot * 32 + tt * 8:slot * 32 + (tt + 1) * 8], t2[:, 63:TT:64], AF.Exp, scale=sc_e, reads=[t2k], writes=[("EB", slot, tt)])
            E("dve", "tensor_tensor", t3.rearrange("p (a b) -> p a b", b=64), b3, b3[:, :, 63:64].to_broadcast([128, 8, 64]), ALU.subtract,
              reads=[t2k], writes=[t3k])
            E("act", "activation", t0, t3, AF.Exp, scale=sc_e, reads=[t3k], writes=[t0k])
            E("act", "activation", t2, t3, AF.Exp, scale=-sc_e, reads=[t3k], writes=[t2k])
            self.tpp.free(t3i)
            pi, ps, pk = self.proj_fm(wq, wqk, tt)
            if qscale == 1.0:
                E("dve", "tensor_tensor", self.BB[:, QT, sl], ps[:, :], t0, ALU.mult, reads=[pk, t0k], writes=[("BB", QT, tt)])
            else:
                E("dve", "scalar_tensor_tensor", self.BB[:, QT, sl], ps[:, :], qscale, t0, ALU.mult, ALU.mult, reads=[pk, t0k], writes=[("BB", QT, tt)])
            self.psp.free(pi)
            if kind == "A":
                E("dve", "scalar_tensor_tensor", self.BB[:, KT, sl], t1, DP[:, db + DP_OML + cc:db + DP_OML + cc + 1], t2, ALU.mult, ALU.mult,
                  reads=[t1k, t2k, "DP"], writes=[("BB", KT, tt)])
                self.tpp.free(t1i)
            else:
                pi, ps, pk = self.proj_fm(wk, wkk, tt)
                E("dve", "tensor_tensor", self.BB[:, KT, sl], ps[:, :], t2, ALU.mult, reads=[pk, t2k], writes=[("BB", KT, tt)])
                self.psp.free(pi)
            self.tpp.free(t0i)
            self.tpp.free(t2i)
            pi, ps, pk = self.ps_get()
            psb = ps[:, 0:256].bitcast(BF16)
            for q4 in range(4):
                E("pe", "transpose", psb[:, q4 * 128:(q4 + 1) * 128], self.BB[:, KT, tt * TT + q4 * 128:tt * TT + (q4 + 1) * 128], identb,
                  reads=[("BB", KT, tt), "CB"], writes=[pk], signal=(q4 == 3))
            E("act", "activation", ktt[:, tt * 4:(tt + 1) * 4, :], psb.rearrange("p (b c) -> p b c", c=128), AF.Copy,
              reads=[pk], writes=[("BB", KTT, tt)])
            self.psp.free(pi)
            for a in range(noc):
                pi, ps, pk = self.ps_get()
                for q4 in range(4):
                    tb = tt * 4 + q4
                    for k in range(8):
                        E("pe", "matmul", ps[:, q4 * 128:(q4 + 1) * 128], self.H[:, k, tb * 128:(tb + 1) * 128], wv[a][1][:, k, :],
                          start=(k == 0), stop=(k == 7), reads=[wv[a][2], self.hk(k, tt)], writes=[pk], signal=(k == 7))
                dstv = self.BB[:, VT[a], :].rearrange("p (b c) -> p b c", c=128)[:, tt * 4:(tt + 1) * 4, :]
                E("act", "activation", dstv, ps.rearrange("p (b c) -> p b c", c=128), AF.Copy, reads=[pk], writes=[("BB", VT[a], tt)])
                self.psp.free(pi)
        self.wbp.free(wq_i)
        if kind == "A":
            self.wbp.free(wz_i)
        else:
            self.wbp.free(wk_i)
            self.wbp.free(wl_i)
        for a in range(noc):
            self.wbp.free(wv[a][0])

        wg = [self.load_slab(self.wcols("w_in", l, gc0 + a * 128, 128), 8, 128) for a in range(noc)]
        blk64 = CB[:, CB_BLK64:CB_BLK64 + 128]
        cmask = CB[:, CB_CMASK:CB_CMASK + 128]
        hm = CF[:, hmcol:hmcol + nh]
        so = slot * 128
        slots = [slot] if nv == 128 else [0, 1]
        SS = self.SS[:, so:so + nv]
        EB = self.EB[:, slot * 32:(slot + 1) * 32]
        D32 = [self.D32[:, i, so:so + nv] for i in range(2)]
        D16 = [self.D16[:, i, so:so + nv] for i in range(4)]
        ssk = [("SS", x) for x in slots]

        def d32k(i):
            return [("D32", i, x) for x in slots]

        def d16k(i):
            return [("D16", i, x) for x in slots]
        for i in range(2):
            E("dve", "memset", D32[i], 0.0, writes=d32k(i))
        for i in range(4):
            E("dve", "memset", D16[i], 0.0, writes=d16k(i))
        yield

        def vtap(blk, p0, p1, c0, c1):
            a = c0 // 128
            assert (c1 - 1) // 128 == a
            return self.BB[:, VT[a], :].rearrange("p (b c) -> p b c", c=128)[p0:p1, blk, c0 - a * 128:c1 - a * 128]

        state = {}

        def front(tt, blk):
            b = tt * 4 + blk
            g0 = tt * TT + blk * 128
            kmi, kmap, kmk = self.tb_get()
            km = kmap[:, 0:nh * 128].rearrange("p (h t) -> p h t", h=nh)
            E("pool", "tensor_tensor", km, self.BB[:, KT, g0:g0 + 128].unsqueeze(1).to_broadcast([128, nh, 128]),
              hm.unsqueeze(2).to_broadcast([128, nh, 128]), ALU.mult, reads=[("BB", KT, tt), "CF"], writes=[kmk])
            si, sps, sk = self.ps_get()
            for h in range(nh):
                E("pe", "matmul", sps[:, h * 128:(h + 1) * 128], km[:, h, :], self.BB[:, QT, g0:g0 + 128], start=True, stop=True,
                  reads=[kmk, ("BB", QT, tt)], writes=[sk], signal=(h == nh - 1))
            self.tbp.free(kmi)
            tpsl = []
            for half in range(2):
                pr = half * 64
                ti, tps, tk = self.ps_get()
                for a in range(noc):
                    E("pe", "matmul", tps[:, a * 128:(a + 1) * 128], ktt[pr:pr + 64, b, :], vtap(b, pr, pr + 64, a * 128, (a + 1) * 128),
                      start=True, stop=True, reads=[("BB", KTT, tt), ("BB", VT[a], tt)], writes=[tk], signal=(a == noc - 1))
                tpsl.append((ti, tps, tk))
            for half in range(2):
                c = 2 * b + half
                ti, tps, tk = tpsl[half]
                if c > 0:
                    E("dve", "scalar_tensor_tensor", D32[c % 2], SS, EB[:, c:c + 1], smask, ALU.mult, ALU.mult,
                      reads=ssk + [("EB", slot, c // 8), "CF"], writes=d32k(c % 2))
                    E("act", "activation", D16[c % 4], D32[c % 2], AF.Copy, reads=d32k(c % 2), writes=d16k(c % 4))
                    E("dve", "tensor_tensor", SS, D32[c % 2], tps[:, 0:nv], ALU.add, reads=d32k(c % 2) + [tk], writes=ssk)
                else:
                    E("dve", "tensor_copy", SS, tps[:, 0:nv], reads=[tk], writes=ssk)
                self.psp.free(ti)
            sci, scap, sck = self.tb_get()
            sc = scap[:, 0:nh * 128].rearrange("p (h t) -> p h t", h=nh)
            E("dve", "tensor_tensor", sc, sps[:, 0:nh * 128].rearrange("p (h t) -> p h t", h=nh),
              cmask.unsqueeze(1).to_broadcast([128, nh, 128]), ALU.mult, reads=[sk, "CB"], writes=[sck])
            self.psp.free(si)
            state[b] = (sci, sc, sck)

        def back(tt, blk, ops):
            b = tt * 4 + blk
            c0 = blk * 128
            sci, sc, sck = state.pop(b)
            for h in range(nh):
                oc, po = h // 2, (h % 2) * 64
                E("pe", "matmul", ops[oc][1][po:po + 64, c0:c0 + 128], vtap(b, 0, 128, h * 64, (h + 1) * 64), sc[:, h, :],
                  start=False, stop=False, skip_group_check=True,
                  reads=[sck, ("BB", VT[(h * 64) // 128], tt)], writes=[ops[oc][2]], signal=(h == nh - 1))
            self.tbp.free(sci)
            for half in range(2):
                c = 2 * b + half
                tc = c0 + half * 64
                gtc = tt * TT + tc
                if c > 0:
                    for oc in range(noc):
                        E("pe", "matmul", ops[oc][1][:, tc:tc + 64], D16[c % 4][:, oc * 128:(oc + 1) * 128], self.BB[:, QT, gtc:gtc + 64],
                          start=False, stop=False, skip_group_check=True,
                          reads=d16k(c % 4) + [("BB", QT, tt)], writes=[ops[oc][2]])

        for tt in range(NT):
            sl = slice(tt * TT, (tt + 1) * TT)
            ops = [self.ps_get() for _ in range(noc)]
            for (oi, op, ok) in ops:
                E("dve", "memset", op[:, :], 0.0, writes=[ok])
            front(tt, 0)
            yield
            for blk in range(1, 4):
                front(tt, blk)
                yield
                back(tt, blk - 1, ops)
                yield
            back(tt, 3, ops)
            yield
            for oc in range(noc):
                oi, op, ok = ops[oc]
                o32i, o32, o32k = self.tp_get()
                qi, qap, qk = self.tb_get()
                E("act", "activation", o32, op[:, :], AF.Copy, reads=[ok], writes=[o32k])
                E("act", "activation", qap, op[:, :], AF.Square, reads=[ok], writes=[qk])
                self.psp.free(oi)
                ni, nps, nk = self.ps_get()
                E("pe", "matmul", nps[:, :], blk64, qap, start=True, stop=True, reads=[qk, "CB"], writes=[nk])
                self.tbp.free(qi)
                ri, rap, rk = self.rstd(nps, nk, 64)
                self.psp.free(ni)
                E("dve", "scalar_tensor_tensor", o32, o32, DP[:, gain0 + oc:gain0 + oc + 1], rap, ALU.mult, ALU.mult,
                  reads=[o32k, rk, "DP"], writes=[o32k])
                self.tpp.free(ri)
                gi, gps, gk = self.proj_fm(wg[oc][1], wg[oc][2], tt)
                sgi, sg, sgk = self.tp_get()
                E("act", "activation", sg, gps[:, :], AF.Silu, reads=[gk], writes=[sgk])
                self.psp.free(gi)
                E("dve", "tensor_tensor", self.BB[:, OA[oc], sl], o32, sg, ALU.mult, reads=[o32k, sgk], writes=[("BB", OA[oc], tt)])
                self.tpp.free(o32i)
                self.tpp.free(sgi)
            yield
        for a in range(noc):
            self.wbp.free(wg[a][0])
        self.bbp.free(QT)
        self.bbp.free(KT)
        self.bbp.free(KTT)
        for v in VT:
            self.bbp.free(v)
        self.out_proj("w_out", l, wrow, OA)
        for o in OA:
            self.bbp.free(o)

    def attn_group(self, l, cc):
        E = self.E
        CF, CB = self.CF, self.CB
        qc0, kc0, vc0 = 1808 + cc * 128, 2064 + cc * 128, 2320 + cc * 128
        NUMb, n2 = self.bbp.get_pair()
        DENb, d2 = self.bbp.get_pair()
        QR = self.bbp.get()
        KM = [self.bbp.get(), self.bbp.get()]
        VT = self.bbp.get()
        assert n2 == NUMb + 1 and d2 == DENb + 1
        NUM, DEN = self.bbf(NUMb), self.bbf(DENb)
        numk, denk = self.bbfk(NUMb), self.bbfk(DENb)
        def swapped(src_ap, src_key):
            j = self.wbp.get()
            dst = self.WB[:, j, 0:1024].rearrange("p (k c) -> p k c", k=8)
            E("pool", "tensor_copy", dst, src_ap, reads=[src_key], writes=[("WB", j)])
            s4 = src_ap.rearrange("p k (h d) -> p k h d", h=2)
            d4 = dst.rearrange("p k (h d) -> p k h d", h=2)
            E("pool", "tensor_copy", d4[:, :, :, 0:8], s4[:, :, :, 8:16], reads=[src_key], writes=[("WB", j)])
            E("pool", "tensor_copy", d4[:, :, :, 8:16], s4[:, :, :, 0:8], reads=[src_key], writes=[("WB", j)])
            return j, dst, ("WB", j)
        hm = CF[:, CF_HMA:CF_HMA + 2]
        for which, c0 in (("q", qc0), ("k", kc0)):
            wi, w, wk = self.load_slab(self.wcols("w_in", l, c0, 128), 8, 128)
            si, sw, swk = swapped(w, wk)
            for tt in range(NT):
                sl = slice(tt * TT, (tt + 1) * TT)
                p1i, p1, p1k = self.proj_fm(w, wk, tt)
                p2i, p2, p2k = self.proj_fm(sw, swk, tt)
                t0i, t0, t0k = self.tp_get()
                t1i, t1, t1k = self.tp_get()
                E("dve", "tensor_tensor", t0, p1[:, :], self.ROPE[:, 0, sl], ALU.mult, reads=[p1k, ("ROPE", 0)], writes=[t0k])
                E("dve", "tensor_tensor", t1, p2[:, :], self.ROPE[:, 1, sl], ALU.mult, reads=[p2k, ("ROPE", 1)], writes=[t1k])
                self.psp.free(p1i)
                self.psp.free(p2i)
                if which == "q":
                    E("dve", "tensor_tensor", self.BB[:, QR, sl], t0, t1, ALU.add, reads=[t0k, t1k], writes=[("BB", QR, tt)])
                else:
                    E("dve", "tensor_tensor", t0, t0, t1, ALU.add, reads=[t0k, t1k], writes=[t0k])
                    for h in range(2):
                        E("dve", "tensor_scalar", self.BB[:, KM[h], sl], t0, hm[:, h:h + 1], None, ALU.mult, reads=[t0k, "CF"],
                          writes=[("BB", KM[h], tt)])
                self.tpp.free(t0i)
                self.tpp.free(t1i)
            self.wbp.free(wi)
            self.wbp.free(si)
        wvi, wv, wvk = self.load_slab(self.wcols("w_in", l, vc0, 128), 8, 128)
        ones64 = CB[:, CB_ONES:CB_ONES + 64]
        amask = CB[:, CB_AMASK:CB_AMASK + 512].rearrange("p (h k q) -> p h k q", h=2, k=2)
        vt = self.BB[:, VT, :].rearrange("p (b c) -> p b c", c=128)
        allq = self.bbk(QR)
        allk = [self.bbk(KM[0]), self.bbk(KM[1])]
        allv = self.bbk(VT)
        allh = [self.hk(k, t) for k in range(8) for t in range(NT)]
        for pat, d in enumerate((1, 4, 16)):
            nbs = 16 // d

            def tok(r, n):
                st0 = r + d * 128 * n
                return slice(st0, st0 + d * 127 + 1, d) if d > 1 else slice(st0, st0 + 128)
            for b4 in range(4):
                pi, ps, pk = self.ps_get()
                for q4 in range(4):
                    bid = b4 * 4 + q4
                    r, n = bid // nbs, bid % nbs
                    for k in range(8):
                        E("pe", "matmul", ps[:, q4 * 128:(q4 + 1) * 128], self.H[:, k, tok(r, n)], wv[:, k, :], start=(k == 0), stop=(k == 7),
                          reads=[wvk] + [self.hk(k, t) for t in range(NT)], writes=[pk], signal=(k == 7))
                E("act", "activation", vt[:, b4 * 4:(b4 + 1) * 4, :], ps.rearrange("p (b c) -> p b c", c=128), AF.Copy,
                  reads=[pk], writes=[("BB", VT, b4)])
                self.psp.free(pi)
            pend = {}
            banks = {}

            def front(bid):
                r, n = bid // nbs, bid % nbs
                kbs = [1] if n == 0 else [0, 1]
                si, sps, sk = self.ps_get()
                s4 = sps.rearrange("p (h k q) -> p h k q", h=2, k=2)
                for h in range(2):
                    for kb in kbs:
                        kn = n - 1 + kb
                        E("pe", "matmul", s4[:, h, kb, :], self.BB[:, KM[h], tok(r, kn)], self.BB[:, QR, tok(r, n)], start=True, stop=True,
                          reads=allk[h] + allq, writes=[sk], signal=(h == 1 and kb == 1))
                pi, pap, pk_ = self.tb_get()
                p4 = pap.rearrange("p (h k q) -> p h k q", h=2, k=2)
                k0 = kbs[0]
                E("act", "activation", p4[:, :, k0:2, :], s4[:, :, k0:2, :], AF.Exp, scale=0.125, reads=[sk], writes=[pk_])
                self.psp.free(si)
                E("dve", "tensor_tensor", p4[:, :, k0:2, :], p4[:, :, k0:2, :], amask[:, :, k0:2, :], ALU.mult, reads=[pk_, "CB"], writes=[pk_])
                pend[bid] = (pi, p4, pk_, kbs)

            def back(bid):
                g4, q4 = bid // 4, bid % 4
                if q4 == 0:
                    banks[g4] = (self.ps_get(), self.ps_get())
                (ni, nps, nk), (di, dps, dk) = banks[g4]
                pi, p4, pk_, kbs = pend.pop(bid)
                for h in range(2):
                    po = h * 64
                    for idx, kb in enumerate(kbs):
                        kbid = bid - 1 + kb
                        E("pe", "matmul", nps[po:po + 64, q4 * 128:(q4 + 1) * 128], vt[:, kbid, h * 64:(h + 1) * 64], p4[:, h, kb, :],
                          start=(idx == 0), stop=(idx == len(kbs) - 1), reads=[pk_] + allv, writes=[nk], signal=False)
                    for idx, kb in enumerate(kbs):
                        E("pe", "matmul", dps[po:po + 64, q4 * 128:(q4 + 1) * 128], ones64, p4[:, h, kb, :],
                          start=(idx == 0), stop=(idx == len(kbs) - 1), reads=[pk_, "CB"], writes=[dk],
                          signal=(h == 1 and idx == len(kbs) - 1))
                self.tbp.free(pi)
                if q4 < 3:
                    return
                if d == 1:
                    sl = slice(g4 * TT, (g4 + 1) * TT)
                    E("act", "activation", NUM[:, sl], nps[:, :], AF.Copy, reads=[nk], writes=numk)
                    E("dve", "tensor_copy", DEN[:, sl], dps[:, :], reads=[dk], writes=denk)
                else:
                    if d == 4:
                        nv_ = NUM[:, g4:S:4]
                        dv_ = DEN[:, g4:S:4]
                        pn, pd = nps[:, :], dps[:, :]
                    else:
                        nv_ = NUM.rearrange("p (i r) -> p r i", r=16)[:, g4 * 4:(g4 + 1) * 4, :]
                        dv_ = DEN.rearrange("p (i r) -> p r i", r=16)[:, g4 * 4:(g4 + 1) * 4, :]
                        pn, pd = nps.rearrange("p (r i) -> p r i", r=4), dps.rearrange("p (r i) -> p r i", r=4)
                    E("dve", "tensor_tensor", nv_, nv_, pn, ALU.add, reads=[nk] + numk, writes=numk)
                    E("dve", "tensor_tensor", dv_, dv_, pd, ALU.add, reads=[dk] + denk, writes=denk)
                self.psp.free(ni)
                self.psp.free(di)
                del banks[g4]

            front(0)
            for bid in range(1, 16):
                front(bid)
                back(bid - 1)
            back(15)
        self.wbp.free(wvi)
        for tt in range(NT):
            sl = slice(tt * TT, (tt + 1) * TT)
            E("act", "activation", DEN[:, sl], DEN[:, sl], AF.Ln, reads=denk, writes=denk)
            E("act", "activation", DEN[:, sl], DEN[:, sl], AF.Exp, scale=-1.0, reads=denk, writes=denk)
            E("dve", "tensor_tensor", self.BB[:, QR, sl], NUM[:, sl], DEN[:, sl], ALU.mult, reads=numk + denk, writes=[("BB", QR, tt)])
        for x in (KM[0], KM[1], VT, NUMb, n2, DENb, d2):
            self.bbp.free(x)
        self.out_proj("w_out", l, 512 + cc * 128, [QR])
        self.bbp.free(QR)

    def conv_group(self, l):
        E = self.E
        pb = l * PV_L
        CF, CB, PV = self.CF, self.CB, self.PV
        DGb = [self.bbp.get_pair(), self.bbp.get_pair()]
        UB = [self.bbp.get(), self.bbp.get()]
        OD = [self.bbp.get(), self.bbp.get()]
        identb = CB[:, CB_IDENT:CB_IDENT + 128]
        DG, dgk = [], []
        for cc in range(2):
            dg = self.BB[:, DGb[cc][0]:DGb[cc][0] + 2, :].rearrange("p a b -> p (a b)")[:, 0:31 * 128].rearrange("p (j c) -> p j c", c=128)
            keys = self.bbk(DGb[cc][0]) + self.bbk(DGb[cc][1])
            wc = pb + PV_CVW + cc * 31
            E("dve", "tensor_tensor", dg, identb.unsqueeze(1).to_broadcast([128, 31, 128]),
              PV[:, wc:wc + 31].unsqueeze(2).to_broadcast([128, 31, 128]), ALU.mult, reads=["CB", "PV"], writes=keys)
            DG.append(dg)
            dgk.append(keys)
        for cc in range(2):
            wa_i, wa, wak = self.load_slab(self.wcols("w_in", l, 2576 + cc * 128, 128), 8, 128)
            wg_i, wg, wgk = self.load_slab(self.wcols("w_in", l, 2832 + cc * 128, 128), 8, 128)
            for tt in range(NT):
                sl = slice(tt * TT, (tt + 1) * TT)
                pgi, pg, pgk = self.proj_fm(wg, wgk, tt)
                pai, pa, pak = self.proj_fm(wa, wak, tt)
                ti, t, tk = self.tp_get()
                E("act", "activation", t, pg[:, :], AF.Sigmoid, reads=[pgk], writes=[tk])
                E("dve", "tensor_tensor", self.BB[:, UB[cc], sl], pa[:, :], t, ALU.mult, reads=[pak, tk], writes=[("BB", UB[cc], tt)])
                self.psp.free(pgi)
                self.psp.free(pai)
                self.tpp.free(ti)
            self.wbp.free(wa_i)
            self.wbp.free(wg_i)
        ones = CB[:, CB_ONES:CB_ONES + 128]
        for tt in range(NT):
            sl = slice(tt * TT, (tt + 1) * TT)
            t0 = tt * TT
            s1i, s1, s1k = self.ps_get()
            s2i, s2, s2k = self.ps_get()
            ys = []
            for cc in range(2):
                yi, yps, ypk = self.ps_get()
                order = [30] + list(range(30))
                for idx, j in enumerate(order):
                    sh = 30 - j
                    lo = max(0, sh - t0)
                    rd = [("BB", UB[cc], tt)] + ([("BB", UB[cc], tt - 1)] if tt > 0 else [])
                    E("pe", "matmul", yps[:, lo:TT], DG[cc][:, j, :], self.BB[:, UB[cc], t0 + lo - sh:t0 + TT - sh],
                      start=(idx == 0), stop=(idx == 30), reads=dgk[cc] + rd, writes=[ypk], signal=(idx == 30))
                bcol = PV[:, pb + PV_CVB + cc:pb + PV_CVB + cc + 1]
                ai, a, ak = self.tb_get()
                bi, b, bk = self.tb_get()
                y32i, y32, y32k = self.tp_get()
                E("dve", "tensor_scalar", y32, yps[:, :], bcol, None, ALU.add, reads=[ypk, "PV"], writes=[y32k])
                self.psp.free(yi)
                E("act", "activation", a, y32, AF.Copy, reads=[y32k], writes=[ak])
                E("act", "activation", b, y32, AF.Square, reads=[y32k], writes=[bk])
                E("pe", "matmul", s1[:, :], ones, a, start=(cc == 0), stop=(cc == 1), reads=[ak, "CB"], writes=[s1k])
                E("pe", "matmul", s2[:, :], ones, b, start=(cc == 0), stop=(cc == 1), reads=[bk, "CB"], writes=[s2k])
                self.tbp.free(ai)
                self.tbp.free(bi)
                ys.append((y32i, y32, y32k))
            mi, m, mk = self.tp_get()
            vi, v, vk = self.tp_get()
            E("dve", "tensor_scalar", m, s1[:, :], 1.0 / 256, None, ALU.mult, reads=[s1k], writes=[mk])
            E("dve", "tensor_tensor", v, m, m, ALU.mult, reads=[mk], writes=[vk])
            E("dve", "scalar_tensor_tensor", v, s2[:, :], 1.0 / 256, v, ALU.mult, ALU.subtract, reads=[s2k, vk], writes=[vk])
            self.psp.free(s1i)
            self.psp.free(s2i)
            E("act", "activation", v, v, AF.Ln, bias=CF[:, CF_EPS:CF_EPS + 1], reads=[vk, "CF"], writes=[vk])
            E("act", "activation", v, v, AF.Exp, scale=-0.5, reads=[vk], writes=[vk])
            for cc in range(2):
                y32i, y32, y32k = ys[cc]
                E("dve", "tensor_tensor", y32, y32, m, ALU.subtract, reads=[y32k, mk], writes=[y32k])
                E("dve", "tensor_tensor", y32, y32, v, ALU.mult, reads=[y32k, vk], writes=[y32k])
                E("act", "activation", self.BB[:, OD[cc], sl], y32, AF.Silu, bias=PV[:, pb + PV_CVBB + cc:pb + PV_CVBB + cc + 1],
                  scale=PV[:, pb + PV_CVG + cc:pb + PV_CVG + cc + 1], reads=[y32k, "PV"], writes=[("BB", OD[cc], tt)])
                self.tpp.free(y32i)
            self.tpp.free(mi)
            self.tpp.free(vi)
        for p in DGb:
            self.bbp.free(p[0])
            self.bbp.free(p[1])
        for x in UB:
            self.bbp.free(x)
        self.out_proj("w_out", l, 768, OD)
        for x in OD:
            self.bbp.free(x)

    def cross_kv(self, l, s):
        E = self.E
        pb, db = l * PV_L, l * DP_L
        CF, CB, DP = self.CF, self.CB, self.DP
        identf = CF[:, CF_IDENT:CF_IDENT + 128]
        ones = CB[:, CB_ONES:CB_ONES + 128]
        m0, m1 = self.bbp.get_pair()
        mh = self.bbp.get()
        assert m1 == m0 + 1
        MT = self.bbf(m0).rearrange("p (c t) -> p c t", c=8)
        mtk = self.bbfk(m0)
        MH = self.BB[:, mh, :].rearrange("p (c t) -> p c t", c=8)
        mhk = self.bbk(mh)
        for mb in range(2):
            for hf in range(2):
                i, ap, key = self.tp_get()
                E("sp", "dma_start", out=ap, in_=self.mem_d[s, mb * 128:(mb + 1) * 128, hf * 512:(hf + 1) * 512], writes=[key], dma=True)
                pi, ps, pk = self.ps_get()
                for q in range(4):
                    E("pe", "transpose", ps[:, q * 128:(q + 1) * 128], ap[:, q * 128:(q + 1) * 128], identf,
                      reads=[key, "CF"], writes=[pk], signal=(q == 3))
                E("act", "activation", MT[:, hf * 4:(hf + 1) * 4, mb * 128:(mb + 1) * 128], ps.rearrange("p (c t) -> p c t", c=4), AF.Copy,
                  reads=[pk], writes=mtk)
                self.psp.free(pi)
                self.tpp.free(i)
        pi, ps, pk = self.ps_get()
        for c in range(8):
            bi, bap, bk = self.tb_get()
            E("act", "activation", bap[:, 0:MEM], MT[:, c, :], AF.Square, reads=mtk, writes=[bk])
            E("pe", "matmul", ps[:, 0:MEM], ones, bap[:, 0:MEM], start=(c == 0), stop=(c == 7), reads=[bk, "CB"], writes=[pk], signal=(c == 7))
            self.tbp.free(bi)
        ri, rap, rk = self.rstd(ps, pk, D, ncols=MEM)
        self.psp.free(pi)
        for c in range(8):
            E("dve", "scalar_tensor_tensor", MH[:, c, :], MT[:, c, :], DP[:, db + PV_MEMN + c:db + PV_MEMN + c + 1], rap[:, 0:MEM], ALU.mult, ALU.mult,
              reads=mtk + [rk, "DP"], writes=mhk)
        self.tpp.free(ri)
        for c in range(8):
            wi, w, wk = self.load_slab(self.wcols("cross_wkv", l, c * 128, 128), 8, 128)
            pi, ps, pk = self.ps_get()
            for k in range(8):
                E("pe", "matmul", ps[:, 0:MEM], w[:, k, :], MH[:, k, :], start=(k == 0), stop=(k == 7), reads=[wk] + mhk, writes=[pk], signal=(k == 7))
            E("act", "activation", self.KX[:, c, :], ps[:, 0:MEM], AF.Copy, reads=[pk], writes=[("KX", c)])
            self.psp.free(pi)
            self.wbp.free(wi)
        for vc in range(8):
            wi, w, wk = self.load_slab(self.wcols("cross_wkv", l, D + vc * 128, 128), 8, 128)
            pi, ps, pk = self.ps_get()
            for mb in range(2):
                for k in range(8):
                    E("pe", "matmul", ps[:, mb * 128:(mb + 1) * 128], MH[:, k, mb * 128:(mb + 1) * 128], w[:, k, :], start=(k == 0), stop=(k == 7),
                      reads=[wk] + mhk, writes=[pk], signal=(k == 7))
            E("dve", "tensor_copy", self.VX[:, :, vc * 128:(vc + 1) * 128], ps[:, 0:256].rearrange("p (m c) -> p m c", m=2), reads=[pk], writes=[("VX", vc)])
            self.psp.free(pi)
            self.wbp.free(wi)
        for x in (m0, m1, mh):
            self.bbp.free(x)

    def cross(self, l, s):
        E = self.E
        pb, db = l * PV_L, l * DP_L
        CF, CB, DP = self.CF, self.CB, self.DP
        self.rmsnorm_x(db + PV_CROSS)
        ones = CB[:, CB_ONES:CB_ONES + 128]
        for hp in range(2):
            QX = [self.bbp.get() for _ in range(4)]
            OX = [self.bbp.get() for _ in range(4)]
            for qi_ in range(4):
                qc = hp * 4 + qi_
                wi, w, wk = self.load_slab(self.wcols("cross_wq", l, qc * 128, 128), 8, 128)
                for tt in range(NT):
                    sl = slice(tt * TT, (tt + 1) * TT)
                    pi, ps, pk = self.proj_fm(w, wk, tt)
                    E("act", "activation", self.BB[:, QX[qi_], sl], ps[:, :], AF.Copy, reads=[pk], writes=[("BB", QX[qi_], tt)])
                    self.psp.free(pi)
                self.wbp.free(wi)
            for hh in range(2):
                h = hp * 2 + hh
                for tt in range(NT):
                    sl = slice(tt * TT, (tt + 1) * TT)
                    P = []
                    for mb in range(2):
                        si, sps, sk = self.ps_get()
                        for dc in range(2):
                            E("pe", "matmul", sps[:, :], self.KX[:, 2 * h + dc, mb * 128:(mb + 1) * 128], self.BB[:, QX[hh * 2 + dc], sl],
                              start=(dc == 0), stop=(dc == 1), reads=[("KX", 2 * h + dc), ("BB", QX[hh * 2 + dc], tt)], writes=[sk], signal=(dc == 1))
                        pi, pap, pk_ = self.tb_get()
                        E("act", "activation", pap, sps[:, :], AF.Exp, scale=1.0 / 16, reads=[sk], writes=[pk_])
                        self.psp.free(si)
                        P.append((pi, pap, pk_))
                    di, dps, dk = self.ps_get()
                    for mb in range(2):
                        E("pe", "matmul", dps[:, :], ones, P[mb][1], start=(mb == 0), stop=(mb == 1), reads=[P[mb][2], "CB"], writes=[dk], signal=(mb == 1))
                    ri, rap, rk = self.tp_get()
                    E("act", "activation", rap, dps[:, :], AF.Ln, reads=[dk], writes=[rk])
                    E("act", "activation", rap, rap, AF.Exp, scale=-1.0, reads=[rk], writes=[rk])
                    self.psp.free(di)
                    for vc in range(2):
                        oi, ops_, ok = self.ps_get()
                        col = h * 256 + vc * 128
                        for mb in range(2):
                            E("pe", "matmul", ops_[:, :], self.VX[:, mb, col:col + 128], P[mb][1], start=(mb == 0), stop=(mb == 1),
                              reads=[P[mb][2], ("VX", col // 128)], writes=[ok], signal=(mb == 1))
                        E("dve", "tensor_tensor", self.BB[:, OX[hh * 2 + vc], sl], ops_[:, :], rap, ALU.mult, reads=[ok, rk],
                          writes=[("BB", OX[hh * 2 + vc], tt)])
                        self.psp.free(oi)
                    self.tpp.free(ri)
                    for mb in range(2):
                        self.tbp.free(P[mb][0])
            for x in QX:
                self.bbp.free(x)
            self.out_proj("cross_wo", l, hp * 512, OX)
            for x in OX:
                self.bbp.free(x)


_CACHE = {}


def _get_nc(nseq=SEQ_PER_CORE, depth=DEPTH, stop=None, dbg=None, mixsel="abcd"):
    key = (nseq, depth, stop, dbg, mixsel)
    if key not in _CACHE:
        b = Builder(nseq, depth, stop, dbg, mixsel)
        _CACHE[key] = (b.build(), b)
    return _CACHE[key]


def kernel(**inputs):
    nc, _ = _get_nc()
    cf, cb = _consts()
    pv = _pack_params(inputs)
    x = np.ascontiguousarray(np.asarray(inputs["x"], np.float32))
    mem = np.ascontiguousarray(np.asarray(inputs["mem"], np.float32))
    pos = np.ascontiguousarray(np.asarray(inputs["positions"], np.int32))
    shared = {n: np.ascontiguousarray(np.asarray(inputs[n], np.float32)) for n in WNAMES}
    shared["gla_gate_w"] = np.ascontiguousarray(np.asarray(inputs["gla_gate_w"], np.float32))
    shared["pv"] = pv
    shared["cf"] = cf
    shared["cb"] = cb
    in_maps = []
    for c in range(NCORES):
        m = dict(shared)
        sl = slice(c * SEQ_PER_CORE, (c + 1) * SEQ_PER_CORE)
        m["x"] = x[sl]
        m["mem"] = mem[sl]
        m["pos"] = pos[sl]
        in_maps.append(m)
    res = run_bass_kernel_spmd(nc, in_maps, core_ids=list(range(NCORES)))
    return np.concatenate([r["y"] for r in res.results], axis=0)
```

```python
import numpy as np
import ml_dtypes
from contextlib import ExitStack
import concourse.bass as bass
import concourse.mybir as mybir
from concourse.bass_utils import run_bass_kernel_spmd

F32 = mybir.dt.float32
BF16 = mybir.dt.bfloat16
I32 = mybir.dt.int32
AF = mybir.ActivationFunctionType
ALU = mybir.AluOpType

D = 1024
S = 2048
NB = 16
DEPTH = 2
MEM = 256
DFF = 2816
INW = 3088
EPS = 1e-6
NT = 4
TT = 512
NCORES = 8
SEQ_PER_CORE = NB // NCORES


class Node:
    __slots__ = ("id", "eng", "fns", "deps", "succ", "dur", "dma", "signal", "ndep", "ready", "finish", "tok", "vc")

    def __init__(self, id, eng, dma, signal):
        self.id, self.eng, self.dma, self.signal = id, eng, dma, signal
        self.fns = []
        self.deps = set()
        self.succ = []
        self.dur = 0.0
        self.ready = 0.0
        self.finish = None
        self.tok = None
        self.vc = None


def _nelem(ap):
    n = 1
    for d in ap.shape[1:]:
        n *= d
    return n


class Em:
    ENGS = ("pe", "act", "dve", "pool", "sp")
    WINDOW = 64
    LAT = 1.3

    def __init__(self, n_dma_sems=12):
        self.nodes = []
        self.lastw = {}
        self.readers = {}
        self.open_pe = None
        self.dma_sems = {"sp": [["dma_sp_%d" % i, 0, None] for i in range(n_dma_sems)]}
        self.ninstr = 0
        self.prog = {e: [] for e in self.ENGS}

    def sem_names(self):
        return list(self.ENGS) + [s[0] for s in self.dma_sems["sp"]]

    @staticmethod
    def _cost(eng, fn, dma):
        op, args, kw = fn
        try:
            if dma:
                return 2.0 + _nelem(kw["out"]) * 128 * 4 / 150e3
            n = _nelem(args[0])
            if eng == "pe":
                return 0.035 + n / 2800.0
            if eng == "act":
                return 0.2 + n / 1200.0
            if eng == "dve":
                return 0.1 + (2 * n if op == "tensor_tensor_scan" else n) / 960.0
            return 0.1 + n / 1700.0
        except Exception:
            return 0.5

    def emit(self, eng, fn, reads=(), writes=(), signal=True, dma=False):
        self.ninstr += 1
        if eng == "pe" and self.open_pe is not None:
            g = self.open_pe.id
            fwd = False
            for r in reads:
                t = self.lastw.get(r)
                if t is not None and t > g:
                    fwd = True
            for w in writes:
                t = self.lastw.get(w)
                if t is not None and t > g:
                    fwd = True
                for t in self.readers.get(w, ()):
                    if t > g:
                        fwd = True
            if fwd:
                self.open_pe = None
        if eng == "pe" and self.open_pe is not None:
            node = self.open_pe
        else:
            node = Node(len(self.nodes), eng, dma, True)
            self.nodes.append(node)
        node.fns.append(fn)
        node.dur += self._cost(eng, fn, dma)
        nid = node.id
        for r in reads:
            t = self.lastw.get(r)
            if t is not None and t != nid:
                node.deps.add(t)
        for w in writes:
            t = self.lastw.get(w)
            if t is not None and t != nid:
                node.deps.add(t)
            for t in self.readers.get(w, ()):
                if t != nid:
                    node.deps.add(t)
        for w in writes:
            self.lastw[w] = nid
            self.readers[w] = []
        for r in reads:
            lst = self.readers.setdefault(r, [])
            if not lst or lst[-1] != nid:
                lst.append(nid)
        if eng == "pe":
            self.open_pe = None if signal else node
        return nid

    def wait_all(self, eng, toks):
        node = Node(len(self.nodes), eng, False, False)
        self.nodes.append(node)
        node.deps = set(toks)
        node.dur = 0.05
        return node.id

    def finalize(self):
        assert self.open_pe is None
        nodes = self.nodes
        for n in nodes:
            assert all(d < n.id for d in n.deps), "forward dependency"
            n.ndep = len(n.deps)
            for d in n.deps:
                nodes[d].succ.append(n.id)
        queues = {e: [n.id for n in nodes if n.eng == e] for e in self.ENGS}
        heads = {e: 0 for e in self.ENGS}
        free = {e: 0.0 for e in self.ENGS}
        done = [False] * len(nodes)
        order = []
        remaining = len(nodes)
        W, LAT = self.WINDOW, self.LAT
        bl = [0.0] * len(nodes)
        for n in reversed(nodes):
            m = 0.0
            for sid in n.succ:
                v = bl[sid] + (LAT if nodes[sid].eng != n.eng else 0.06)
                if v > m:
                    m = v
            bl[n.id] = n.dur + m
        while remaining:
            best = None
            for e in self.ENGS:
                q = queues[e]
                i = heads[e]
                seen = 0
                fe = free[e]
                while i < len(q) and seen < W:
                    nid = q[i]
                    i += 1
                    if done[nid]:
                        continue
                    seen += 1
                    n = nodes[nid]
                    if n.ndep:
                        continue
                    st = n.ready if n.ready > fe else fe
                    key = (round(st / 0.3), -bl[nid], nid)
                    if best is None or key < best[0]:
                        best = (key, st, nid, e)
            assert best is not None, "scheduler deadlock"
            _, st, nid, e = best
            n = nodes[nid]
            n.finish = st + n.dur
            free[e] = n.finish if not n.dma else st + 0.15
            done[nid] = True
            order.append(nid)
            remaining -= 1
            q = queues[e]
            while heads[e] < len(q) and done[q[heads[e]]]:
                heads[e] += 1
            for sid in n.succ:
                sn = nodes[sid]
                sn.ndep -= 1
                r = n.finish + (LAT if sn.eng != e else 0.06)
                if r > sn.ready:
                    sn.ready = r
        self.est_us = max(n.finish for n in nodes)
        cnt = {e: 0 for e in self.ENGS}
        clock = {e: {} for e in self.ENGS}
        rr = 0
        for nid in order:
            n = nodes[nid]
            eng = n.eng
            clk = clock[eng]
            waits = {}
            for d in n.deps:
                t = nodes[d].tok
                if eng == "pe" and t[0] == "pe":
                    continue
                if clk.get(t[0], 0) < t[1]:
                    if waits.get(t[0], 0) < t[1]:
                        waits[t[0]] = t[1]
            for d in n.deps:
                dn = nodes[d]
                if eng == "pe" and dn.tok[0] == "pe":
                    continue
                for k, v in dn.vc.items():
                    if clk.get(k, 0) < v:
                        clk[k] = v
                if clk.get(dn.tok[0], 0) < dn.tok[1]:
                    clk[dn.tok[0]] = dn.tok[1]
            inc = None
            if n.dma:
                lst = self.dma_sems["sp"]
                ent = lst[rr]
                rr = (rr + 1) % len(lst)
                name, cur = ent[0], ent[1]
                if cur > 0 and clk.get(name, 0) < cur:
                    waits[name] = max(waits.get(name, 0), cur)
                    clk[name] = cur
                ent[1] = cur + 16
                n.tok = (name, cur + 16)
                inc = (name, 16)
            elif n.fns:
                cnt[eng] += 1
                n.tok = (eng, cnt[eng])
                inc = (eng, 1)
            else:
                n.tok = (eng, cnt[eng])
            n.vc = dict(clk)
            self.prog[eng].append((tuple(waits.items()), n.fns, inc))

    def replay(self, eng, e, sems):
        for waits, fns, inc in self.prog[eng]:
            for name, val in waits:
                e.wait_ge(sems[name], val)
            ins = None
            for fn in fns:
                ins = getattr(e, fn[0])(*fn[1], **fn[2])
            if inc is not None and ins is not None:
                ins.then_inc(sems[inc[0]], inc[1])


class Pool:
    def __init__(self, name, aps):
        self.name = name
        self.aps = aps
        self.held = [False] * len(aps)
        self.rr = 0

    def get(self):
        n = len(self.aps)
        for k in range(n):
            i = (self.rr + k) % n
            if not self.held[i]:
                self.held[i] = True
                self.rr = (i + 1) % n
                return i
        raise RuntimeError("pool %s exhausted" % self.name)

    def get_pair(self):
        n = len(self.aps)
        for k in range(n):
            i = (self.rr + k) % n
            if i + 1 < n and not self.held[i] and not self.held[i + 1]:
                self.held[i] = self.held[i + 1] = True
                self.rr = (i + 2) % n
                return i, i + 1
        raise RuntimeError("pool %s: no free pair" % self.name)

    def free(self, i):
        assert self.held[i]
        self.held[i] = False

    def key(self, i):
        return (self.name, i)


CF_IDENT = 0
CF_MASKA = 128
CF_MASKB = 256
CF_HMA = 512
CF_HMB = 514
CF_ROPE = 518
CF_EPS = 522
CF_N = 523

CB_IDENT = 0
CB_ONES = 128
CB_BLK64 = 256
CB_CMASK = 384
CB_AMASK = 512
CB_SCANM = 1024
CB_N = 1536


def _consts():
    cf = np.zeros((128, CF_N), np.float32)
    p = np.arange(128)
    cf[:, CF_IDENT:CF_IDENT + 128] = np.eye(128, dtype=np.float32)
    cf[:, CF_MASKA:CF_MASKA + 128] = (p[:, None] // 64 == np.arange(128)[None, :] // 64)
    cf[:, CF_MASKB:CF_MASKB + 256] = (p[:, None] // 32 == np.arange(256)[None, :] // 64)
    for h in range(2):
        cf[:, CF_HMA + h] = (p // 64 == h)
    for h in range(4):
        cf[:, CF_HMB + h] = (p // 32 == h)
    dd = p % 64
    j = dd % 8
    invf = np.power(np.float32(500000.0), -np.arange(0, 16, 2, dtype=np.float32) / np.float32(16)).astype(np.float32)
    rot = dd < 16
    pi = np.float32(np.pi)
    cf[:, CF_ROPE + 0] = np.where(rot, invf[j], 0.0)
    cf[:, CF_ROPE + 1] = np.float32(np.pi / 2)
    cf[:, CF_ROPE + 2] = np.where(rot, invf[j], 0.0)
    cf[:, CF_ROPE + 3] = np.where(dd < 8, pi, 0.0)
    cf[:, CF_EPS] = EPS
    cb = np.zeros((128, CB_N), np.float32)
    cb[:, CB_IDENT:CB_IDENT + 128] = np.eye(128)
    cb[:, CB_ONES:CB_ONES + 128] = 1.0
    cb[:, CB_BLK64:CB_BLK64 + 128] = (p[:, None] // 64 == np.arange(128)[None, :] // 64)
    s_ = p[:, None]
    t_ = np.arange(128)[None, :]
    cb[:, CB_CMASK:CB_CMASK + 128] = (s_ // 64 == t_ // 64) & (s_ <= t_)
    am = np.zeros((128, 2, 2, 128), np.float32)
    am[:, :, 0, :] = (s_ >= t_)[:, None, :]
    am[:, :, 1, :] = (s_ <= t_)[:, None, :]
    cb[:, CB_AMASK:CB_AMASK + 512] = am.reshape(128, 512)
    cb[:, CB_SCANM:CB_SCANM + 512] = (np.arange(512)[None, :] % 64 != 0)
    return cf, cb.astype(ml_dtypes.bfloat16)


PV_FFN1 = 0
PV_MIX = 8
PV_CROSS = 16
PV_MEMN = 24
PV_FFN2 = 32
PV_LBLOG = 40
PV_HGN = 42
PV_GLN = 44
PV_GLB = 46
PV_CVB = 47
PV_CVG = 49
PV_CVBB = 51
PV_CVW = 53
PV_L = 115
PV_FINAL = DEPTH * PV_L
PV_N = PV_FINAL + 8


def _fm(v, ncol):
    return np.ascontiguousarray(np.asarray(v, np.float32).reshape(ncol, 128).T)


def _pack_params(inp):
    pv = np.zeros((128, PV_N), np.float32)
    for l in range(DEPTH):
        b = l * PV_L
        pv[:, b + PV_FFN1:b + PV_FFN1 + 8] = _fm(inp["ffn1_norm"][l], 8)
        pv[:, b + PV_MIX:b + PV_MIX + 8] = _fm(inp["mix_norm"][l], 8)
        pv[:, b + PV_CROSS:b + PV_CROSS + 8] = _fm(inp["cross_norm"][l], 8)
        pv[:, b + PV_MEMN:b + PV_MEMN + 8] = _fm(inp["mem_norm"][l], 8)
        pv[:, b + PV_FFN2:b + PV_FFN2 + 8] = _fm(inp["ffn2_norm"][l], 8)
        pv[:, b + PV_LBLOG:b + PV_LBLOG + 2] = _fm(inp["hgrn_lb_logits"][l], 2)
        pv[:, b + PV_HGN:b + PV_HGN + 2] = _fm(inp["hgrn_out_norm"][l], 2)
        pv[:, b + PV_GLN:b + PV_GLN + 2] = _fm(inp["gla_out_norm"][l], 2)
        pv[:, b + PV_GLB:b + PV_GLB + 1] = _fm(inp["gla_gate_b"][l], 1)
        pv[:, b + PV_CVB:b + PV_CVB + 2] = _fm(inp["conv_b"][l], 2)
        pv[:, b + PV_CVG:b + PV_CVG + 2] = _fm(inp["conv_ln_g"][l], 2)
        pv[:, b + PV_CVBB:b + PV_CVBB + 2] = _fm(inp["conv_ln_b"][l], 2)
        cw = np.asarray(inp["conv_w"][l], np.float32)
        for cc in range(2):
            pv[:, b + PV_CVW + cc * 31:b + PV_CVW + cc * 31 + 31] = cw[:, cc * 128:(cc + 1) * 128].T
    pv[:, PV_FINAL:PV_FINAL + 8] = _fm(inp["final_norm"], 8)
    return pv


WNAMES = ["ffn1_w_up", "ffn1_w_down", "w_in", "w_out", "cross_wq", "cross_wkv", "cross_wo",
          "ffn2_w_up", "ffn2_w_down"]
WSHAPES = {"ffn1_w_up": [DEPTH, D, 2 * DFF], "ffn1_w_down": [DEPTH, DFF, D], "w_in": [DEPTH, D, INW],
           "w_out": [DEPTH, D, D], "cross_wq": [DEPTH, D, D], "cross_wkv": [DEPTH, D, 2 * D],
           "cross_wo": [DEPTH, D, D], "ffn2_w_up": [DEPTH, D, 2 * DFF], "ffn2_w_down": [DEPTH, DFF, D]}

NBB = 10
NTP = 6
NTB = 8
NWS = 3
NWB = 5

DP_G = 0
DP_L = 48
DP_OML = 40
DP_LBF = 42
DP_HGN = 44
DP_GLN = 46
DP_FINAL = DEPTH * DP_L
DP_N = DP_FINAL + 8


class Builder:
    def __init__(self, nseq=SEQ_PER_CORE, depth=DEPTH, stop=None, dbg=None, mixsel="abcd"):
        self.nseq, self.depth, self.stop, self.dbgspec, self.mixsel = nseq, depth, stop, dbg, mixsel
        self.em = Em()
        self.nc = bass.Bass("TRN2", target_bir_lowering=False)
        self.out_toks = []

    def E(self, eng, op, *args, reads=(), writes=(), signal=True, dma=False, **kw):
        return self.em.emit(eng, (op, args, kw), reads, writes, signal=signal, dma=dma)

    def bbk(self, i, a=0, b=S):
        return [("BB", i, t) for t in range(a // TT, (b - 1) // TT + 1)]

    def bbf(self, i):
        return self.BB[:, i:i + 2, :].rearrange("p a b -> p (a b)").bitcast(F32)

    def bbfk(self, i):
        return self.bbk(i) + self.bbk(i + 1)

    def xk(self, c, tt):
        return ("X", c, tt)

    def hk(self, c, tt):
        return ("H", c, tt)

    def hks(self, tt):
        return [("H", c, tt) for c in range(8)]

    def load_slab(self, src_ap, kc, cols):
        assert kc * cols <= 1024
        n = kc * cols
        i = self.ws_rr
        self.ws_rr = (i + 1) % NWS
        j = self.wbp.get()
        ws = self.WS[:, i, 0:n].rearrange("p (k c) -> p k c", k=kc)
        self.E("sp", "dma_start", out=ws, in_=src_ap, writes=[("WS", i)], dma=True)
        self.E("pool", "tensor_copy", self.WB[:, j, 0:n], self.WS[:, i, 0:n], reads=[("WS", i)], writes=[("WB", j)])
        return j, self.WB[:, j, 0:n].rearrange("p (k c) -> p k c", k=kc), ("WB", j)

    def wcols(self, name, l, c0, ncols, r0=0, nrows=D):
        w = self.W[name]
        return w[l, r0:r0 + nrows, c0:c0 + ncols].rearrange("(k p) c -> p k c", p=128)

    def ps_get(self):
        i = self.psp.get()
        return i, self.PS[i], ("PS", i)

    def tp_get(self):
        i = self.tpp.get()
        return i, self.TP[:, i, :], ("TP", i)

    def tb_get(self):
        i = self.tbp.get()
        return i, self.TB[:, i, :], ("TB", i)

    def rstd(self, ps, pk, n, ncols=TT):
        ri, rap, rk = self.tp_get()
        self.E("act", "activation", rap[:, 0:ncols], ps[:, 0:ncols], AF.Ln, bias=self.CF[:, CF_EPS:CF_EPS + 1], scale=1.0 / n,
               reads=[pk, "CF"], writes=[rk])
        self.E("act", "activation", rap[:, 0:ncols], rap[:, 0:ncols], AF.Exp, scale=-0.5, reads=[rk], writes=[rk])
        return ri, rap, rk

    def proj_fm(self, w, wk, tt, m=128):
        sl = slice(tt * TT, (tt + 1) * TT)
        pi, ps, pk = self.ps_get()
        for k in range(8):
            self.E("pe", "matmul", ps[0:m, :], w[:, k, 0:m], self.H[:, k, sl], start=(k == 0), stop=(k == 7),
                   reads=[wk, self.hk(k, tt)], writes=[pk], signal=(k == 7))
        return pi, ps, pk

    def build(self):
        nc = self.nc
        ns = self.nseq
        self.x_d = nc.dram_tensor("x", [ns, S, D], F32, kind="ExternalInput").ap()
        self.mem_d = nc.dram_tensor("mem", [ns, MEM, D], F32, kind="ExternalInput").ap()
        self.pos_d = nc.dram_tensor("pos", [ns, S], I32, kind="ExternalInput").ap()
        self.W = {n: nc.dram_tensor(n, WSHAPES[n], F32, kind="ExternalInput").ap() for n in WNAMES}
        self.gw_d = nc.dram_tensor("gla_gate_w", [DEPTH, 16, 128], F32, kind="ExternalInput").ap()
        self.pv_d = nc.dram_tensor("pv", [128, PV_N], F32, kind="ExternalInput").ap()
        self.cf_d = nc.dram_tensor("cf", [128, CF_N], F32, kind="ExternalInput").ap()
        self.cb_d = nc.dram_tensor("cb", [128, CB_N], BF16, kind="ExternalInput").ap()
        self.y_d = nc.dram_tensor("y", [ns, S, D], F32, kind="ExternalOutput").ap()
        with ExitStack() as st:
            def sb(name, shape, dt):
                return st.enter_context(nc.sbuf_tensor(name, shape, dt))
            self.X = sb("X", [128, 8, S], F32)
            self.H = sb("H", [128, 8, S], BF16)
            self.BB = sb("BB", [128, NBB, S], BF16)
            self.WS = sb("WS", [128, NWS, 1024], F32)
            self.WB = sb("WB", [128, NWB, 1024], BF16)
            self.TP = sb("TP", [128, NTP, TT], F32)
            self.TB = sb("TB", [128, NTB, TT], BF16)
            self.ROPE = sb("ROPE", [128, 2, S], BF16)
            self.KX = sb("KX", [128, 8, MEM], BF16)
            self.VX = sb("VX", [128, 2, D], BF16)
            self.CF = sb("CF", [128, CF_N], F32)
            self.CB = sb("CB", [128, CB_N], BF16)
            self.PV = sb("PV", [128, PV_N], F32)
            self.DP = sb("DP", [128, DP_N], F32)
            self.GWS = sb("GWS", [16, DEPTH, 128], F32)
            self.GW = sb("GW", [16, DEPTH, 128], BF16)
            self.SS = sb("SS", [128, 256], F32)
            self.D32 = sb("D32", [128, 2, 256], F32)
            self.D16 = sb("D16", [128, 4, 256], BF16)
            self.EB = sb("EB", [128, 64], F32)
            self.SM = sb("SM", [128, 64], F32)
            self.PS = [st.enter_context(nc.psum_tensor("ps%d" % i, [128, TT], F32)) for i in range(8)]
            self.psp = Pool("PS", self.PS)
            self.tpp = Pool("TP", list(range(NTP)))
            self.tbp = Pool("TB", list(range(NTB)))
            self.wbp = Pool("WB", list(range(NWB)))
            self.bbp = Pool("BB", list(range(NBB)))
            self.ws_rr = 0
            sems = {n: st.enter_context(nc.semaphore(n)) for n in self.em.sem_names()}
            self.program()
            self.em.finalize()
            block = st.enter_context(nc.Block())
            em = self.em

            @block.sync
            def _(e):
                em.replay("sp", e, sems)

            @block.gpsimd
            def _(e):
                em.replay("pool", e, sems)

            @block.tensor
            def _(e):
                em.replay("pe", e, sems)

            @block.vector
            def _(e):
                em.replay("dve", e, sems)

            @block.scalar
            def _(e):
                em.replay("act", e, sems)
        return nc

    def program(self):
        E = self.E
        CF, CB, PV, DP = self.CF, self.CB, self.PV, self.DP
        E("sp", "dma_start", out=CF[:, :], in_=self.cf_d[:, :], writes=["CF"], dma=True)
        E("sp", "dma_start", out=CB[:, :], in_=self.cb_d[:, :], writes=["CB"], dma=True)
        E("sp", "dma_start", out=PV[:, :], in_=self.pv_d[:, :], writes=["PV"], dma=True)
        E("sp", "dma_start", out=self.GWS[:, :, :], in_=self.gw_d.rearrange("l r c -> r l c"), writes=["GWS"], dma=True)
        E("pool", "tensor_copy", self.GW[:, :, :], self.GWS[:, :, :], reads=["GWS"], writes=["GW"])
        for l in range(self.depth):
            pb, db = l * PV_L, l * DP_L
            E("dve", "tensor_copy", DP[:, db:db + 40], PV[:, pb:pb + 40], reads=["PV"], writes=["DP"])
            E("dve", "tensor_copy", DP[:, db + DP_HGN:db + DP_HGN + 4], PV[:, pb + PV_HGN:pb + PV_HGN + 4], reads=["PV"], writes=["DP"])
            if l == 0:
                E("dve", "memset", DP[:, db + DP_OML:db + DP_OML + 2], 1.0, writes=["DP"])
                E("dve", "memset", DP[:, db + DP_LBF:db + DP_LBF + 2], 1e-20, writes=["DP"])
            else:
                E("dve", "tensor_tensor", self.SM[:, 0:2], PV[:, pb + PV_LBLOG:pb + PV_LBLOG + 2], PV[:, PV_LBLOG:PV_LBLOG + 2], ALU.subtract,
                  reads=["PV"], writes=["SM"])
                E("act", "activation", self.SM[:, 2:4], self.SM[:, 0:2], AF.Sigmoid, reads=["SM"], writes=["SM"])
                E("dve", "tensor_scalar", DP[:, db + DP_OML:db + DP_OML + 2], self.SM[:, 2:4], -1.0, 1.0, ALU.mult, ALU.add,
                  reads=["SM"], writes=["DP"])
                E("dve", "tensor_scalar", DP[:, db + DP_LBF:db + DP_LBF + 2], self.SM[:, 2:4], 1e-20, None, ALU.max,
                  reads=["SM"], writes=["DP"])
        E("dve", "tensor_copy", DP[:, DP_FINAL:DP_FINAL + 8], PV[:, PV_FINAL:PV_FINAL + 8], reads=["PV"], writes=["DP"])

        for s in range(self.nseq):
            self.load_x(s)
            if self.stop == "load":
                self.final(s, norm=False)
                continue
            self.rope_tables(s)
            for l in range(self.depth):
                last = (l == self.depth - 1)
                self.ffn(l, "ffn1", PV_FFN1)
                if last and self.stop == "ffn1":
                    break
                self.mixer(l, s)
                if last and self.stop == "mixer":
                    break
                self.cross(l, s)
                if last and self.stop == "cross":
                    break
                self.ffn(l, "ffn2", PV_FFN2)
            self.final(s)
        self.em.wait_all("sp", self.out_toks)

    def load_x(self, s):
        E = self.E
        ident = self.CF[:, CF_IDENT:CF_IDENT + 128]
        for tb in range(S // 128):
            halves = []
            for hf in range(2):
                i, ap, key = self.tp_get()
                E("sp", "dma_start", out=ap, in_=self.x_d[s, tb * 128:(tb + 1) * 128, hf * 512:(hf + 1) * 512], writes=[key], dma=True)
                halves.append((i, ap, key))
            for hf in range(2):
                i, ap, key = halves[hf]
                pi, ps, pk = self.ps_get()
                for q in range(4):
                    E("pe", "transpose", ps[:, q * 128:(q + 1) * 128], ap[:, q * 128:(q + 1) * 128], ident,
                      reads=[key, "CF"], writes=[pk], signal=(q == 3))
                c0 = hf * 4
                tt = tb // 4
                dst = self.X[:, c0:c0 + 4, tb * 128:(tb + 1) * 128]
                src = ps.rearrange("p (c t) -> p c t", c=4)
                wr = [self.xk(c, tt) for c in range(c0, c0 + 4)]
                if hf == 0:
                    E("act", "activation", dst, src, AF.Copy, reads=[pk], writes=wr)
                else:
                    E("dve", "tensor_copy", dst, src, reads=[pk], writes=wr)
                self.psp.free(pi)
                self.tpp.free(i)

    def rmsnorm_x(self, gcol):
        E = self.E
        ones = self.CB[:, CB_ONES:CB_ONES + 128]
        for tt in range(NT):
            sl = slice(tt * TT, (tt + 1) * TT)
            pi, ps, pk = self.ps_get()
            for c in range(8):
                bi, bap, bk = self.tb_get()
                E("act", "activation", bap, self.X[:, c, sl], AF.Square, reads=[self.xk(c, tt)], writes=[bk])
                E("pe", "matmul", ps[:, :], ones, bap, start=(c == 0), stop=(c == 7), reads=[bk, "CB"], writes=[pk], signal=(c == 7))
                self.tbp.free(bi)
            ri, rap, rk = self.rstd(ps, pk, D)
            self.psp.free(pi)
            for c in range(8):
                E("dve", "scalar_tensor_tensor", self.H[:, c, sl], self.X[:, c, sl], self.DP[:, gcol + c:gcol + c + 1], rap, ALU.mult, ALU.mult,
                  reads=[self.xk(c, tt), rk, "DP"], writes=[self.hk(c, tt)])
            self.tpp.free(ri)

    def out_proj(self, wname, l, r0, bufs, scale=1.0):
        E = self.E
        n = len(bufs)
        for dc in range(8):
            di, dw, dk = self.load_slab(self.wcols(wname, l, dc * 128, 128, r0=r0, nrows=n * 128), n, 128)
            for tt in range(NT):
                sl = slice(tt * TT, (tt + 1) * TT)
                pa, psa, pka = self.ps_get()
                for k in range(n):
                    E("pe", "matmul", psa[:, :], dw[:, k, :], self.BB[:, bufs[k], sl], start=(k == 0), stop=(k == n - 1),
                      reads=[dk, ("BB", bufs[k], tt)], writes=[pka], signal=(k == n - 1))
                E("dve", "scalar_tensor_tensor", self.X[:, dc, sl], psa[:, :], float(scale), self.X[:, dc, sl], ALU.mult, ALU.add,
                  reads=[pka, self.xk(dc, tt)], writes=[self.xk(dc, tt)])
                self.psp.free(pa)
            self.wbp.free(di)

    def ffn(self, l, pre, pvcol):
        E = self.E
        self.rmsnorm_x(l * DP_L + pvcol)
        wup, wdn = pre + "_w_up", pre + "_w_down"
        groups = [(0, 8), (8, 16), (16, 22)]
        for (fa, fb) in groups:
            n = fb - fa
            bufs = [self.bbp.get() for _ in range(n)]
            for j in range(fa, fb):
                gi, gw, gk = self.load_slab(self.wcols(wup, l, j * 128, 128), 8, 128)
                ui, uw, uk = self.load_slab(self.wcols(wup, l, DFF + j * 128, 128), 8, 128)
                bbi = bufs[j - fa]
                for tt in range(NT):
                    sl = slice(tt * TT, (tt + 1) * TT)
                    pa, psa, pka = self.proj_fm(gw, gk, tt)
                    pb, psb, pkb = self.proj_fm(uw, uk, tt)
                    ti, tap, tk = self.tp_get()
                    E("act", "activation", tap, psa[:, :], AF.Silu, reads=[pka], writes=[tk])
                    E("dve", "tensor_tensor", self.BB[:, bbi, sl], psb[:, :], tap, ALU.mult, reads=[tk, pkb], writes=[("BB", bbi, tt)])
                    self.psp.free(pa)
                    self.psp.free(pb)
                    self.tpp.free(ti)
                self.wbp.free(gi)
                self.wbp.free(ui)
            self.out_proj(wdn, l, fa * 128, bufs, scale=0.5)
            for b in bufs:
                self.bbp.free(b)

    def final(self, s, norm=True):
        E = self.E
        identf = self.CF[:, CF_IDENT:CF_IDENT + 128]
        ones = self.CB[:, CB_ONES:CB_ONES + 128]
        gcol = DP_FINAL
        for tt in range(NT):
            sl = slice(tt * TT, (tt + 1) * TT)
            if norm:
                pi, ps, pk = self.ps_get()
                for c in range(8):
                    bi, bap, bk = self.tb_get()
                    E("act", "activation", bap, self.X[:, c, sl], AF.Square, reads=[self.xk(c, tt)], writes=[bk])
                    E("pe", "matmul", ps[:, :], ones, bap, start=(c == 0), stop=(c == 7), reads=[bk, "CB"], writes=[pk], signal=(c == 7))
                    self.tbp.free(bi)
                ri, rap, rk = self.rstd(ps, pk, D)
                self.psp.free(pi)
                for c in range(8):
                    E("dve", "scalar_tensor_tensor", self.X[:, c, sl], self.X[:, c, sl], self.DP[:, gcol + c:gcol + c + 1], rap, ALU.mult, ALU.mult,
                      reads=[self.xk(c, tt), rk, "DP"], writes=[self.xk(c, tt)])
                self.tpp.free(ri)
            for q in range(4):
                tb = tt * 4 + q
                for hf in range(2):
                    pi, ps, pk = self.ps_get()
                    for c4 in range(4):
                        c = hf * 4 + c4
                        E("pe", "transpose", ps[:, c4 * 128:(c4 + 1) * 128], self.X[:, c, tb * 128:(tb + 1) * 128], identf,
                          reads=[self.xk(c, tt), "CF"], writes=[pk], signal=(c4 == 3))
                    oi, oap, ok = self.tp_get()
                    if hf == 0:
                        E("act", "activation", oap, ps[:, :], AF.Copy, reads=[pk], writes=[ok])
                    else:
                        E("dve", "tensor_copy", oap, ps[:, :], reads=[pk], writes=[ok])
                    self.psp.free(pi)
                    t = E("sp", "dma_start", out=self.y_d[s, tb * 128:(tb + 1) * 128, hf * 512:(hf + 1) * 512], in_=oap, reads=[ok], dma=True)
                    self.out_toks.append(t)
                    self.tpp.free(oi)

    def rope_tables(self, s):
        E = self.E
        b = list(self.bbp.get_pair()) + list(self.bbp.get_pair()) + list(self.bbp.get_pair())
        posi = self.BB[:, b[0]:b[0] + 2, :].rearrange("p a b -> p (a b)").bitcast(I32)
        kint = self.BB[:, b[4]:b[4] + 2, :].rearrange("p a b -> p (a b)").bitcast(I32)
        posf = self.bbf(b[2])
        u = self.bbf(b[0])
        kf = self.bbf(b[4])
        k01, k23, k45 = self.bbfk(b[0]), self.bbfk(b[2]), self.bbfk(b[4])
        assert b[1] == b[0] + 1 and b[3] == b[2] + 1 and b[5] == b[4] + 1
        E("sp", "dma_start", out=posi, in_=self.pos_d[s:s + 1, :].to_broadcast([128, S]), writes=k01, dma=True)
        E("dve", "tensor_copy", posf, posi, reads=k01, writes=k23)
        twopi = float(2 * np.pi)
        for j in range(2):
            invc = self.CF[:, CF_ROPE + 2 * j:CF_ROPE + 2 * j + 1]
            phic = self.CF[:, CF_ROPE + 2 * j + 1:CF_ROPE + 2 * j + 2]
            E("dve", "tensor_scalar", u, posf, invc, phic, ALU.mult, ALU.add, reads=k23 + ["CF"], writes=k01)
            E("dve", "tensor_scalar", kint, u, 1.0 / twopi, None, ALU.mult, reads=k01, writes=k45)
            E("dve", "tensor_copy", kf, kint, reads=k45, writes=k45)
            E("dve", "scalar_tensor_tensor", u, kf, -twopi, u, ALU.mult, ALU.add, reads=k45 + k01, writes=k01)
            E("dve", "tensor_scalar", u, u, float(np.pi), float(-np.pi), ALU.min, ALU.max, reads=k01, writes=k01)
            E("act", "activation", self.ROPE[:, j, :], u, AF.Sin, reads=k01, writes=[("ROPE", j)])
        for x in b:
            self.bbp.free(x)

    def mixer(self, l, s):
        self.rmsnorm_x(l * DP_L + PV_MIX)
        self.cross_kv(l, s)
        if "a" in self.mixsel:
            self._oa = {}
            gens = [self.gla_group(l, "A", 0, 0), self.gla_group(l, "A", 1, 1)]
            while gens:
                for gen in list(gens):
                    try:
                        next(gen)
                    except StopIteration:
                        gens.remove(gen)
            self.out_proj("w_out", l, 0, [self._oa[0], self._oa[1]])
            self.bbp.free(self._oa[0])
            self.bbp.free(self._oa[1])
        if "b" in self.mixsel:
            for _ in self.gla_group(l, "B", 0, 0):
                pass
        if "c" in self.mixsel:
            oc0 = self.attn_group(l, 0)
            oc1 = self.attn_group(l, 1)
            self.out_proj("w_out", l, 512, [oc0, oc1])
            self.bbp.free(oc0)
            self.bbp.free(oc1)
        if "d" in self.mixsel:
            self.conv_group(l)

    def gla_group(self, l, kind, cc, slot=0):
        E = self.E
        pb, db = l * PV_L, l * DP_L
        CF, CB, DP, PV = self.CF, self.CB, self.DP, self.PV
        if kind == "A":
            nh, nv = 2, 128
            qc0, zc0, vc0, gc0 = 0 + cc * 128, 256 + cc * 128, 512 + cc * 128, 768 + cc * 128
            wrow = cc * 128
            gain0 = db + DP_HGN + cc
            hmcol = CF_HMA
            smask = CF[:, CF_MASKA:CF_MASKA + 128]
            sc_e = 1.0
            qscale = 1.0
        else:
            nh, nv = 4, 256
            qc0, kc0, vc0, lrc0, gc0 = 1024, 1152, 1280, 1536, 1552
            wrow = 256
            gain0 = db + DP_GLN
            hmcol = CF_HMB
            smask = CF[:, CF_MASKB:CF_MASKB + 256]
            sc_e = 1.0 / 16.0
            qscale = float(32 ** -0.5)
        noc = nv // 128
        QT, KT, KTT = self.bbp.get(), self.bbp.get(), self.bbp.get()
        VT = [self.bbp.get() for _ in range(noc)]
        OA = [self.bbp.get() for _ in range(noc)]
        ktt = self.BB[:, KTT, :].rearrange("p (b c) -> p b c", c=128)
        vkeys_all = [k for v in VT for k in self.bbk(v)]
        scanm = CB[:, CB_SCANM:CB_SCANM + TT]
        identb = CB[:, CB_IDENT:CB_IDENT + 128]

        wq_i, wq, wqk = self.load_slab(self.wcols("w_in", l, qc0, 128), 8, 128)
        if kind == "A":
            wz_i, wz, wzk = self.load_slab(self.wcols("w_in", l, zc0, 128), 8, 128)
        else:
            wk_i, wk, wkk = self.load_slab(self.wcols("w_in", l, kc0, 128), 8, 128)
            wl_i, wl, wlk = self.load_slab(self.wcols("w_in", l, lrc0, 16), 8, 16)
        wv = [self.load_slab(self.wcols("w_in", l, vc0 + a * 128, 128), 8, 128) for a in range(noc)]
        for tt in range(NT):
            sl = slice(tt * TT, (tt + 1) * TT)
            t0i, t0, t0k = self.tp_get()
            if kind == "A":
                t1i, t1, t1k = self.tp_get()
                pi, ps, pk = self.proj_fm(wz, wzk, tt)
                E("act", "activation", t0, ps[:, :], AF.Sigmoid, reads=[pk], writes=[t0k])
                E("act", "activation", t1, ps[:, :], AF.Sigmoid, scale=-1.0, reads=[pk], writes=[t1k])
                self.psp.free(pi)
                E("dve", "tensor_scalar", t0, t0, DP[:, db + DP_OML + cc:db + DP_OML + cc + 1], DP[:, db + DP_LBF + cc:db + DP_LBF + cc + 1],
                  ALU.mult, ALU.add, reads=[t0k, "DP"], writes=[t0k])
                E("act", "activation", t0, t0, AF.Ln, reads=[t0k], writes=[t0k])
            else:
                pi, ps, pk = self.proj_fm(wl, wlk, tt, m=16)
                li, lap, lk = self.tb_get()
                E("act", "activation", lap[0:16, :], ps[0:16, :], AF.Copy, reads=[pk], writes=[lk])
                self.psp.free(pi)
                pi, ps, pk = self.ps_get()
                E("pe", "matmul", ps[:, :], self.GW[:, l, :], lap[0:16, :], start=True, stop=True, reads=["GW", lk], writes=[pk])
                self.tbp.free(li)
                E("act", "activation", t0, ps[:, :], AF.Sigmoid, bias=PV[:, pb + PV_GLB:pb + PV_GLB + 1], reads=[pk, "PV"], writes=[t0k])
                self.psp.free(pi)
                E("act", "activation", t0, t0, AF.Ln, reads=[t0k], writes=[t0k])
            t2i, t2, t2k = self.tp_get()
            t3i, t3, t3k = self.tp_get()
            E("dve", "tensor_tensor_scan", t2, scanm, t0, 0.0, ALU.mult, ALU.add, reads=[t0k, "CB"], writes=[t2k])
            b3 = t2.rearrange("p (a b) -> p a b", b=64)
            E("act", "activation", self.EB[:, slot * 32 + tt * 8:slot * 32 + (tt + 1) * 8], t2[:, 63:TT:64], AF.Exp, scale=sc_e, reads=[t2k], writes=[("EB", slot, tt)])
            E("dve", "tensor_tensor", t3.rearrange("p (a b) -> p a b", b=64), b3, b3[:, :, 63:64].to_broadcast([128, 8, 64]), ALU.subtract,
              reads=[t2k], writes=[t3k])
            E("act", "activation", t0, t3, AF.Exp, scale=sc_e, reads=[t3k], writes=[t0k])
            E("act", "activation", t2, t3, AF.Exp, scale=-sc_e, reads=[t3k], writes=[t2k])
            self.tpp.free(t3i)
            pi, ps, pk = self.proj_fm(wq, wqk, tt)
            if qscale == 1.0:
                E("dve", "tensor_tensor", self.BB[:, QT, sl], ps[:, :], t0, ALU.mult, reads=[pk, t0k], writes=[("BB", QT, tt)])
            else:
                E("dve", "scalar_tensor_tensor", self.BB[:, QT, sl], ps[:, :], qscale, t0, ALU.mult, ALU.mult, reads=[pk, t0k], writes=[("BB", QT, tt)])
            self.psp.free(pi)
            if kind == "A":
                E("dve", "scalar_tensor_tensor", self.BB[:, KT, sl], t1, DP[:, db + DP_OML + cc:db + DP_OML + cc + 1], t2, ALU.mult, ALU.mult,
                  reads=[t1k, t2k, "DP"], writes=[("BB", KT, tt)])
                self.tpp.free(t1i)
            else:
                pi, ps, pk = self.proj_fm(wk, wkk, tt)
                E("dve", "tensor_tensor", self.BB[:, KT, sl], ps[:, :], t2, ALU.mult, reads=[pk, t2k], writes=[("BB", KT, tt)])
                self.psp.free(pi)
            self.tpp.free(t0i)
            self.tpp.free(t2i)
            pi, ps, pk = self.ps_get()
            psb = ps[:, 0:256].bitcast(BF16)
            for q4 in range(4):
                E("pe", "transpose", psb[:, q4 * 128:(q4 + 1) * 128], self.BB[:, KT, tt * TT + q4 * 128:tt * TT + (q4 + 1) * 128], identb,
                  reads=[("BB", KT, tt), "CB"], writes=[pk], signal=(q4 == 3))
            E("act", "activation", ktt[:, tt * 4:(tt + 1) * 4, :], psb.rearrange("p (b c) -> p b c", c=128), AF.Copy,
              reads=[pk], writes=[("BB", KTT, tt)])
            self.psp.free(pi)
            for a in range(noc):
                pi, ps, pk = self.ps_get()
                for q4 in range(4):
                    tb = tt * 4 + q4
                    for k in range(8):
                        E("pe", "matmul", ps[:, q4 * 128:(q4 + 1) * 128], self.H[:, k, tb * 128:(tb + 1) * 128], wv[a][1][:, k, :],
                          start=(k == 0), stop=(k == 7), reads=[wv[a][2], self.hk(k, tt)], writes=[pk], signal=(k == 7))
                dstv = self.BB[:, VT[a], :].rearrange("p (b c) -> p b c", c=128)[:, tt * 4:(tt + 1) * 4, :]
                E("act", "activation", dstv, ps.rearrange("p (b c) -> p b c", c=128), AF.Copy, reads=[pk], writes=[("BB", VT[a], tt)])
                self.psp.free(pi)
        self.wbp.free(wq_i)
        if kind == "A":
            self.wbp.free(wz_i)
        else:
            self.wbp.free(wk_i)
            self.wbp.free(wl_i)
        for a in range(noc):
            self.wbp.free(wv[a][0])

        wg = [self.load_slab(self.wcols("w_in", l, gc0 + a * 128, 128), 8, 128) for a in range(noc)]
        blk64 = CB[:, CB_BLK64:CB_BLK64 + 128]
        cmask = CB[:, CB_CMASK:CB_CMASK + 128]
        hm = CF[:, hmcol:hmcol + nh]
        so = slot * 128
        slots = [slot] if nv == 128 else [0, 1]
        SS = self.SS[:, so:so + nv]
        EB = self.EB[:, slot * 32:(slot + 1) * 32]
        D32 = [self.D32[:, i, so:so + nv] for i in range(2)]
        D16 = [self.D16[:, i, so:so + nv] for i in range(4)]
        ssk = [("SS", x) for x in slots]

        def d32k(i):
            return [("D32", i, x) for x in slots]

        def d16k(i):
            return [("D16", i, x) for x in slots]
        for i in range(2):
            E("dve", "memset", D32[i], 0.0, writes=d32k(i))
        for i in range(4):
            E("dve", "memset", D16[i], 0.0, writes=d16k(i))
        yield

        def vtap(blk, p0, p1, c0, c1):
            a = c0 // 128
            assert (c1 - 1) // 128 == a
            return self.BB[:, VT[a], :].rearrange("p (b c) -> p b c", c=128)[p0:p1, blk, c0 - a * 128:c1 - a * 128]

        state = {}

        def front(tt, blk):
            b = tt * 4 + blk
            g0 = tt * TT + blk * 128
            kmi, kmap, kmk = self.tb_get()
            km = kmap[:, 0:nh * 128].rearrange("p (h t) -> p h t", h=nh)
            E("pool", "tensor_tensor", km, self.BB[:, KT, g0:g0 + 128].unsqueeze(1).to_broadcast([128, nh, 128]),
              hm.unsqueeze(2).to_broadcast([128, nh, 128]), ALU.mult, reads=[("BB", KT, tt), "CF"], writes=[kmk])
            si, sps, sk = self.ps_get()
            for h in range(nh):
                E("pe", "matmul", sps[:, h * 128:(h + 1) * 128], km[:, h, :], self.BB[:, QT, g0:g0 + 128], start=True, stop=True,
                  reads=[kmk, ("BB", QT, tt)], writes=[sk], signal=(h == nh - 1))
            self.tbp.free(kmi)
            tpsl = []
            for half in range(2):
                pr = half * 64
                ti, tps, tk = self.ps_get()
                for a in range(noc):
                    E("pe", "matmul", tps[:, a * 128:(a + 1) * 128], ktt[pr:pr + 64, b, :], vtap(b, pr, pr + 64, a * 128, (a + 1) * 128),
                      start=True, stop=True, reads=[("BB", KTT, tt), ("BB", VT[a], tt)], writes=[tk], signal=(a == noc - 1))
                tpsl.append((ti, tps, tk))
            for half in range(2):
                c = 2 * b + half
                ti, tps, tk = tpsl[half]
                if c > 0:
                    E("dve", "scalar_tensor_tensor", D32[c % 2], SS, EB[:, c:c + 1], smask, ALU.mult, ALU.mult,
                      reads=ssk + [("EB", slot, c // 8), "CF"], writes=d32k(c % 2))
                    E("act", "activation", D16[c % 4], D32[c % 2], AF.Copy, reads=d32k(c % 2), writes=d16k(c % 4))
                    E("dve", "tensor_tensor", SS, D32[c % 2], tps[:, 0:nv], ALU.add, reads=d32k(c % 2) + [tk], writes=ssk)
                else:
                    E("dve", "tensor_copy", SS, tps[:, 0:nv], reads=[tk], writes=ssk)
                self.psp.free(ti)
            sci, scap, sck = self.tb_get()
            sc = scap[:, 0:nh * 128].rearrange("p (h t) -> p h t", h=nh)
            E("dve", "tensor_tensor", sc, sps[:, 0:nh * 128].rearrange("p (h t) -> p h t", h=nh),
              cmask.unsqueeze(1).to_broadcast([128, nh, 128]), ALU.mult, reads=[sk, "CB"], writes=[sck])
            self.psp.free(si)
            state[b] = (sci, sc, sck)

        def back(tt, blk, ops):
            b = tt * 4 + blk
            c0 = blk * 128
            sci, sc, sck = state.pop(b)
            for h in range(nh):
                oc, po = h // 2, (h % 2) * 64
                E("pe", "matmul", ops[oc][1][po:po + 64, c0:c0 + 128], vtap(b, 0, 128, h * 64, (h + 1) * 64), sc[:, h, :],
                  start=False, stop=False, skip_group_check=True,
                  reads=[sck, ("BB", VT[(h * 64) // 128], tt)], writes=[ops[oc][2]], signal=(h == nh - 1))
            self.tbp.free(sci)
            for half in range(2):
                c = 2 * b + half
                tc = c0 + half * 64
                gtc = tt * TT + tc
                if c > 0:
                    for oc in range(noc):
                        E("pe", "matmul", ops[oc][1][:, tc:tc + 64], D16[c % 4][:, oc * 128:(oc + 1) * 128], self.BB[:, QT, gtc:gtc + 64],
                          start=False, stop=False, skip_group_check=True,
                          reads=d16k(c % 4) + [("BB", QT, tt)], writes=[ops[oc][2]])

        for tt in range(NT):
            sl = slice(tt * TT, (tt + 1) * TT)
            ops = [self.ps_get() for _ in range(noc)]
            for (oi, op, ok) in ops:
                E("dve", "memset", op[:, :], 0.0, writes=[ok])
            front(tt, 0)
            yield
            for blk in range(1, 4):
                front(tt, blk)
                yield
                back(tt, blk - 1, ops)
                yield
            back(tt, 3, ops)
            yield
            for oc in range(noc):
                oi, op, ok = ops[oc]
                o32i, o32, o32k = self.tp_get()
                qi, qap, qk = self.tb_get()
                E("act", "activation", o32, op[:, :], AF.Copy, reads=[ok], writes=[o32k])
                E("act", "activation", qap, op[:, :], AF.Square, reads=[ok], writes=[qk])
                self.psp.free(oi)
                ni, nps, nk = self.ps_get()
                E("pe", "matmul", nps[:, :], blk64, qap, start=True, stop=True, reads=[qk, "CB"], writes=[nk])
                self.tbp.free(qi)
                ri, rap, rk = self.rstd(nps, nk, 64)
                self.psp.free(ni)
                E("dve", "scalar_tensor_tensor", o32, o32, DP[:, gain0 + oc:gain0 + oc + 1], rap, ALU.mult, ALU.mult,
                  reads=[o32k, rk, "DP"], writes=[o32k])
                self.tpp.free(ri)
                gi, gps, gk = self.proj_fm(wg[oc][1], wg[oc][2], tt)
                sgi, sg, sgk = self.tp_get()
                E("act", "activation", sg, gps[:, :], AF.Silu, reads=[gk], writes=[sgk])
                self.psp.free(gi)
                E("dve", "tensor_tensor", self.BB[:, OA[oc], sl], o32, sg, ALU.mult, reads=[o32k, sgk], writes=[("BB", OA[oc], tt)])
                self.tpp.free(o32i)
                self.tpp.free(sgi)
            yield
        for a in range(noc):
            self.wbp.free(wg[a][0])
        self.bbp.free(QT)
        self.bbp.free(KT)
        self.bbp.free(KTT)
        for v in VT:
            self.bbp.free(v)
        if kind == "A":
            self._oa[slot] = OA[0]
            return
        self.out_proj("w_out", l, wrow, OA)
        for o in OA:
            self.bbp.free(o)

    def attn_group(self, l, cc):
        E = self.E
        CF, CB = self.CF, self.CB
        qc0, kc0, vc0 = 1808 + cc * 128, 2064 + cc * 128, 2320 + cc * 128
        NUMb, n2 = self.bbp.get_pair()
        DENb, d2 = self.bbp.get_pair()
        QR = self.bbp.get()
        KM = [self.bbp.get(), self.bbp.get()]
        VT = self.bbp.get()
        assert n2 == NUMb + 1 and d2 == DENb + 1
        NUM, DEN = self.bbf(NUMb), self.bbf(DENb)
        numk, denk = self.bbfk(NUMb), self.bbfk(DENb)
        def swapped(src_ap, src_key):
            j = self.wbp.get()
            dst = self.WB[:, j, 0:1024].rearrange("p (k c) -> p k c", k=8)
            E("pool", "tensor_copy", dst, src_ap, reads=[src_key], writes=[("WB", j)])
            s4 = src_ap.rearrange("p k (h d) -> p k h d", h=2)
            d4 = dst.rearrange("p k (h d) -> p k h d", h=2)
            E("pool", "tensor_copy", d4[:, :, :, 0:8], s4[:, :, :, 8:16], reads=[src_key], writes=[("WB", j)])
            E("pool", "tensor_copy", d4[:, :, :, 8:16], s4[:, :, :, 0:8], reads=[src_key], writes=[("WB", j)])
            return j, dst, ("WB", j)
        hm = CF[:, CF_HMA:CF_HMA + 2]
        for which, c0 in (("q", qc0), ("k", kc0)):
            wi, w, wk = self.load_slab(self.wcols("w_in", l, c0, 128), 8, 128)
            si, sw, swk = swapped(w, wk)
            for tt in range(NT):
                sl = slice(tt * TT, (tt + 1) * TT)
                p1i, p1, p1k = self.proj_fm(w, wk, tt)
                p2i, p2, p2k = self.proj_fm(sw, swk, tt)
                t0i, t0, t0k = self.tp_get()
                t1i, t1, t1k = self.tp_get()
                E("dve", "tensor_tensor", t0, p1[:, :], self.ROPE[:, 0, sl], ALU.mult, reads=[p1k, ("ROPE", 0)], writes=[t0k])
                E("dve", "tensor_tensor", t1, p2[:, :], self.ROPE[:, 1, sl], ALU.mult, reads=[p2k, ("ROPE", 1)], writes=[t1k])
                self.psp.free(p1i)
                self.psp.free(p2i)
                if which == "q":
                    E("dve", "tensor_tensor", self.BB[:, QR, sl], t0, t1, ALU.add, reads=[t0k, t1k], writes=[("BB", QR, tt)])
                else:
                    E("dve", "tensor_tensor", t0, t0, t1, ALU.add, reads=[t0k, t1k], writes=[t0k])
                    for h in range(2):
                        E("dve", "tensor_scalar", self.BB[:, KM[h], sl], t0, hm[:, h:h + 1], None, ALU.mult, reads=[t0k, "CF"],
                          writes=[("BB", KM[h], tt)])
                self.tpp.free(t0i)
                self.tpp.free(t1i)
            self.wbp.free(wi)
            self.wbp.free(si)
        wvi, wv, wvk = self.load_slab(self.wcols("w_in", l, vc0, 128), 8, 128)
        ones64 = CB[:, CB_ONES:CB_ONES + 64]
        amask = CB[:, CB_AMASK:CB_AMASK + 512].rearrange("p (h k q) -> p h k q", h=2, k=2)
        vt = self.BB[:, VT, :].rearrange("p (b c) -> p b c", c=128)
        allq = self.bbk(QR)
        allk = [self.bbk(KM[0]), self.bbk(KM[1])]
        allv = self.bbk(VT)
        allh = [self.hk(k, t) for k in range(8) for t in range(NT)]
        for pat, d in enumerate((1, 4, 16)):
            nbs = 16 // d

            def tok(r, n):
                st0 = r + d * 128 * n
                return slice(st0, st0 + d * 127 + 1, d) if d > 1 else slice(st0, st0 + 128)
            for b4 in range(4):
                pi, ps, pk = self.ps_get()
                for q4 in range(4):
                    bid = b4 * 4 + q4
                    r, n = bid // nbs, bid % nbs
                    for k in range(8):
                        E("pe", "matmul", ps[:, q4 * 128:(q4 + 1) * 128], self.H[:, k, tok(r, n)], wv[:, k, :], start=(k == 0), stop=(k == 7),
                          reads=[wvk] + [self.hk(k, t) for t in range(NT)], writes=[pk], signal=(k == 7))
                E("act", "activation", vt[:, b4 * 4:(b4 + 1) * 4, :], ps.rearrange("p (b c) -> p b c", c=128), AF.Copy,
                  reads=[pk], writes=[("BB", VT, b4)])
                self.psp.free(pi)
            pend = {}
            banks = {}

            def front(bid):
                r, n = bid // nbs, bid % nbs
                kbs = [1] if n == 0 else [0, 1]
                si, sps, sk = self.ps_get()
                s4 = sps.rearrange("p (h k q) -> p h k q", h=2, k=2)
                for h in range(2):
                    for kb in kbs:
                        kn = n - 1 + kb
                        E("pe", "matmul", s4[:, h, kb, :], self.BB[:, KM[h], tok(r, kn)], self.BB[:, QR, tok(r, n)], start=True, stop=True,
                          reads=allk[h] + allq, writes=[sk], signal=(h == 1 and kb == 1))
                pi, pap, pk_ = self.tb_get()
                p4 = pap.rearrange("p (h k q) -> p h k q", h=2, k=2)
                k0 = kbs[0]
                E("act", "activation", p4[:, :, k0:2, :], s4[:, :, k0:2, :], AF.Exp, scale=0.125, reads=[sk], writes=[pk_])
                self.psp.free(si)
                E("dve", "tensor_tensor", p4[:, :, k0:2, :], p4[:, :, k0:2, :], amask[:, :, k0:2, :], ALU.mult, reads=[pk_, "CB"], writes=[pk_])
                pend[bid] = (pi, p4, pk_, kbs)

            def back(bid):
                g4, q4 = bid // 4, bid % 4
                if q4 == 0:
                    banks[g4] = (self.ps_get(), self.ps_get())
                (ni, nps, nk), (di, dps, dk) = banks[g4]
                pi, p4, pk_, kbs = pend.pop(bid)
                for h in range(2):
                    po = h * 64
                    for idx, kb in enumerate(kbs):
                        kbid = bid - 1 + kb
                        E("pe", "matmul", nps[po:po + 64, q4 * 128:(q4 + 1) * 128], vt[:, kbid, h * 64:(h + 1) * 64], p4[:, h, kb, :],
                          start=(idx == 0), stop=(idx == len(kbs) - 1), reads=[pk_] + allv, writes=[nk], signal=False)
                    for idx, kb in enumerate(kbs):
                        E("pe", "matmul", dps[po:po + 64, q4 * 128:(q4 + 1) * 128], ones64, p4[:, h, kb, :],
                          start=(idx == 0), stop=(idx == len(kbs) - 1), reads=[pk_, "CB"], writes=[dk],
                          signal=(h == 1 and idx == len(kbs) - 1))
                self.tbp.free(pi)
                if q4 < 3:
                    return
                if d == 1:
                    sl = slice(g4 * TT, (g4 + 1) * TT)
                    E("act", "activation", NUM[:, sl], nps[:, :], AF.Copy, reads=[nk], writes=numk)
                    E("dve", "tensor_copy", DEN[:, sl], dps[:, :], reads=[dk], writes=denk)
                else:
                    if d == 4:
                        nv_ = NUM[:, g4:S:4]
                        dv_ = DEN[:, g4:S:4]
                        pn, pd = nps[:, :], dps[:, :]
                    else:
                        nv_ = NUM.rearrange("p (i r) -> p r i", r=16)[:, g4 * 4:(g4 + 1) * 4, :]
                        dv_ = DEN.rearrange("p (i r) -> p r i", r=16)[:, g4 * 4:(g4 + 1) * 4, :]
                        pn, pd = nps.rearrange("p (r i) -> p r i", r=4), dps.rearrange("p (r i) -> p r i", r=4)
                    E("dve", "tensor_tensor", nv_, nv_, pn, ALU.add, reads=[nk] + numk, writes=numk)
                    E("dve", "tensor_tensor", dv_, dv_, pd, ALU.add, reads=[dk] + denk, writes=denk)
                self.psp.free(ni)
                self.psp.free(di)
                del banks[g4]

            front(0)
            for bid in range(1, 16):
                front(bid)
                back(bid - 1)
            back(15)
        self.wbp.free(wvi)
        for tt in range(NT):
            sl = slice(tt * TT, (tt + 1) * TT)
            E("act", "activation", DEN[:, sl], DEN[:, sl], AF.Ln, reads=denk, writes=denk)
            E("act", "activation", DEN[:, sl], DEN[:, sl], AF.Exp, scale=-1.0, reads=denk, writes=denk)
            E("dve", "tensor_tensor", self.BB[:, QR, sl], NUM[:, sl], DEN[:, sl], ALU.mult, reads=numk + denk, writes=[("BB", QR, tt)])
        for x in (KM[0], KM[1], VT, NUMb, n2, DENb, d2):
            self.bbp.free(x)
        return QR

    def conv_group(self, l):
        E = self.E
        pb = l * PV_L
        CF, CB, PV = self.CF, self.CB, self.PV
        DGb = [self.bbp.get_pair(), self.bbp.get_pair()]
        UB = [self.bbp.get(), self.bbp.get()]
        OD = [self.bbp.get(), self.bbp.get()]
        identb = CB[:, CB_IDENT:CB_IDENT + 128]
        DG, dgk = [], []
        for cc in range(2):
            dg = self.BB[:, DGb[cc][0]:DGb[cc][0] + 2, :].rearrange("p a b -> p (a b)")[:, 0:31 * 128].rearrange("p (j c) -> p j c", c=128)
            keys = self.bbk(DGb[cc][0]) + self.bbk(DGb[cc][1])
            wc = pb + PV_CVW + cc * 31
            E("dve", "tensor_tensor", dg, identb.unsqueeze(1).to_broadcast([128, 31, 128]),
              PV[:, wc:wc + 31].unsqueeze(2).to_broadcast([128, 31, 128]), ALU.mult, reads=["CB", "PV"], writes=keys)
            DG.append(dg)
            dgk.append(keys)
        for cc in range(2):
            wa_i, wa, wak = self.load_slab(self.wcols("w_in", l, 2576 + cc * 128, 128), 8, 128)
            wg_i, wg, wgk = self.load_slab(self.wcols("w_in", l, 2832 + cc * 128, 128), 8, 128)
            for tt in range(NT):
                sl = slice(tt * TT, (tt + 1) * TT)
                pgi, pg, pgk = self.proj_fm(wg, wgk, tt)
                pai, pa, pak = self.proj_fm(wa, wak, tt)
                ti, t, tk = self.tp_get()
                E("act", "activation", t, pg[:, :], AF.Sigmoid, reads=[pgk], writes=[tk])
                E("dve", "tensor_tensor", self.BB[:, UB[cc], sl], pa[:, :], t, ALU.mult, reads=[pak, tk], writes=[("BB", UB[cc], tt)])
                self.psp.free(pgi)
                self.psp.free(pai)
                self.tpp.free(ti)
            self.wbp.free(wa_i)
            self.wbp.free(wg_i)
        ones = CB[:, CB_ONES:CB_ONES + 128]
        for tt in range(NT):
            sl = slice(tt * TT, (tt + 1) * TT)
            t0 = tt * TT
            s1i, s1, s1k = self.ps_get()
            s2i, s2, s2k = self.ps_get()
            ys = []
            for cc in range(2):
                yi, yps, ypk = self.ps_get()
                order = [30] + list(range(30))
                for idx, j in enumerate(order):
                    sh = 30 - j
                    lo = max(0, sh - t0)
                    rd = [("BB", UB[cc], tt)] + ([("BB", UB[cc], tt - 1)] if tt > 0 else [])
                    E("pe", "matmul", yps[:, lo:TT], DG[cc][:, j, :], self.BB[:, UB[cc], t0 + lo - sh:t0 + TT - sh],
                      start=(idx == 0), stop=(idx == 30), reads=dgk[cc] + rd, writes=[ypk], signal=(idx == 30))
                bcol = PV[:, pb + PV_CVB + cc:pb + PV_CVB + cc + 1]
                ai, a, ak = self.tb_get()
                bi, b, bk = self.tb_get()
                y32i, y32, y32k = self.tp_get()
                E("dve", "tensor_scalar", y32, yps[:, :], bcol, None, ALU.add, reads=[ypk, "PV"], writes=[y32k])
                self.psp.free(yi)
                E("act", "activation", a, y32, AF.Copy, reads=[y32k], writes=[ak])
                E("act", "activation", b, y32, AF.Square, reads=[y32k], writes=[bk])
                E("pe", "matmul", s1[:, :], ones, a, start=(cc == 0), stop=(cc == 1), reads=[ak, "CB"], writes=[s1k])
                E("pe", "matmul", s2[:, :], ones, b, start=(cc == 0), stop=(cc == 1), reads=[bk, "CB"], writes=[s2k])
                self.tbp.free(ai)
                self.tbp.free(bi)
                ys.append((y32i, y32, y32k))
            mi, m, mk = self.tp_get()
            vi, v, vk = self.tp_get()
            E("dve", "tensor_scalar", m, s1[:, :], 1.0 / 256, None, ALU.mult, reads=[s1k], writes=[mk])
            E("dve", "tensor_tensor", v, m, m, ALU.mult, reads=[mk], writes=[vk])
            E("dve", "scalar_tensor_tensor", v, s2[:, :], 1.0 / 256, v, ALU.mult, ALU.subtract, reads=[s2k, vk], writes=[vk])
            self.psp.free(s1i)
            self.psp.free(s2i)
            E("act", "activation", v, v, AF.Ln, bias=CF[:, CF_EPS:CF_EPS + 1], reads=[vk, "CF"], writes=[vk])
            E("act", "activation", v, v, AF.Exp, scale=-0.5, reads=[vk], writes=[vk])
            for cc in range(2):
                y32i, y32, y32k = ys[cc]
                E("dve", "tensor_tensor", y32, y32, m, ALU.subtract, reads=[y32k, mk], writes=[y32k])
                E("dve", "tensor_tensor", y32, y32, v, ALU.mult, reads=[y32k, vk], writes=[y32k])
                E("act", "activation", self.BB[:, OD[cc], sl], y32, AF.Silu, bias=PV[:, pb + PV_CVBB + cc:pb + PV_CVBB + cc + 1],
                  scale=PV[:, pb + PV_CVG + cc:pb + PV_CVG + cc + 1], reads=[y32k, "PV"], writes=[("BB", OD[cc], tt)])
                self.tpp.free(y32i)
            self.tpp.free(mi)
            self.tpp.free(vi)
        for p in DGb:
            self.bbp.free(p[0])
            self.bbp.free(p[1])
        for x in UB:
            self.bbp.free(x)
        self.out_proj("w_out", l, 768, OD)
        for x in OD:
            self.bbp.free(x)

    def cross_kv(self, l, s):
        E = self.E
        pb, db = l * PV_L, l * DP_L
        CF, CB, DP = self.CF, self.CB, self.DP
        identf = CF[:, CF_IDENT:CF_IDENT + 128]
        ones = CB[:, CB_ONES:CB_ONES + 128]
        m0, m1 = self.bbp.get_pair()
        mh = self.bbp.get()
        assert m1 == m0 + 1
        MT = self.bbf(m0).rearrange("p (c t) -> p c t", c=8)
        mtk = self.bbfk(m0)
        MH = self.BB[:, mh, :].rearrange("p (c t) -> p c t", c=8)
        mhk = self.bbk(mh)
        for mb in range(2):
            for hf in range(2):
                i, ap, key = self.tp_get()
                E("sp", "dma_start", out=ap, in_=self.mem_d[s, mb * 128:(mb + 1) * 128, hf * 512:(hf + 1) * 512], writes=[key], dma=True)
                pi, ps, pk = self.ps_get()
                for q in range(4):
                    E("pe", "transpose", ps[:, q * 128:(q + 1) * 128], ap[:, q * 128:(q + 1) * 128], identf,
                      reads=[key, "CF"], writes=[pk], signal=(q == 3))
                E("act", "activation", MT[:, hf * 4:(hf + 1) * 4, mb * 128:(mb + 1) * 128], ps.rearrange("p (c t) -> p c t", c=4), AF.Copy,
                  reads=[pk], writes=mtk)
                self.psp.free(pi)
                self.tpp.free(i)
        pi, ps, pk = self.ps_get()
        for c in range(8):
            bi, bap, bk = self.tb_get()
            E("act", "activation", bap[:, 0:MEM], MT[:, c, :], AF.Square, reads=mtk, writes=[bk])
            E("pe", "matmul", ps[:, 0:MEM], ones, bap[:, 0:MEM], start=(c == 0), stop=(c == 7), reads=[bk, "CB"], writes=[pk], signal=(c == 7))
            self.tbp.free(bi)
        ri, rap, rk = self.rstd(ps, pk, D, ncols=MEM)
        self.psp.free(pi)
        for c in range(8):
            E("dve", "scalar_tensor_tensor", MH[:, c, :], MT[:, c, :], DP[:, db + PV_MEMN + c:db + PV_MEMN + c + 1], rap[:, 0:MEM], ALU.mult, ALU.mult,
              reads=mtk + [rk, "DP"], writes=mhk)
        self.tpp.free(ri)
        for c in range(8):
            wi, w, wk = self.load_slab(self.wcols("cross_wkv", l, c * 128, 128), 8, 128)
            pi, ps, pk = self.ps_get()
            for k in range(8):
                E("pe", "matmul", ps[:, 0:MEM], w[:, k, :], MH[:, k, :], start=(k == 0), stop=(k == 7), reads=[wk] + mhk, writes=[pk], signal=(k == 7))
            E("act", "activation", self.KX[:, c, :], ps[:, 0:MEM], AF.Copy, reads=[pk], writes=[("KX", c)])
            self.psp.free(pi)
            self.wbp.free(wi)
        for vc in range(8):
            wi, w, wk = self.load_slab(self.wcols("cross_wkv", l, D + vc * 128, 128), 8, 128)
            pi, ps, pk = self.ps_get()
            for mb in range(2):
                for k in range(8):
                    E("pe", "matmul", ps[:, mb * 128:(mb + 1) * 128], MH[:, k, mb * 128:(mb + 1) * 128], w[:, k, :], start=(k == 0), stop=(k == 7),
                      reads=[wk] + mhk, writes=[pk], signal=(k == 7))
            E("dve", "tensor_copy", self.VX[:, :, vc * 128:(vc + 1) * 128], ps[:, 0:256].rearrange("p (m c) -> p m c", m=2), reads=[pk], writes=[("VX", vc)])
            self.psp.free(pi)
            self.wbp.free(wi)
        for x in (m0, m1, mh):
            self.bbp.free(x)

    def cross(self, l, s):
        E = self.E
        pb, db = l * PV_L, l * DP_L
        CF, CB, DP = self.CF, self.CB, self.DP
        self.rmsnorm_x(db + PV_CROSS)
        ones = CB[:, CB_ONES:CB_ONES + 128]
        for hp in range(2):
            QX = [self.bbp.get() for _ in range(4)]
            OX = [self.bbp.get() for _ in range(4)]
            for qi_ in range(4):
                qc = hp * 4 + qi_
                wi, w, wk = self.load_slab(self.wcols("cross_wq", l, qc * 128, 128), 8, 128)
                for tt in range(NT):
                    sl = slice(tt * TT, (tt + 1) * TT)
                    pi, ps, pk = self.proj_fm(w, wk, tt)
                    E("act", "activation", self.BB[:, QX[qi_], sl], ps[:, :], AF.Copy, reads=[pk], writes=[("BB", QX[qi_], tt)])
                    self.psp.free(pi)
                self.wbp.free(wi)
            for hh in range(2):
                h = hp * 2 + hh
                for tt in range(NT):
                    sl = slice(tt * TT, (tt + 1) * TT)
                    P = []
                    for mb in range(2):
                        si, sps, sk = self.ps_get()
                        for dc in range(2):
                            E("pe", "matmul", sps[:, :], self.KX[:, 2 * h + dc, mb * 128:(mb + 1) * 128], self.BB[:, QX[hh * 2 + dc], sl],
                              start=(dc == 0), stop=(dc == 1), reads=[("KX", 2 * h + dc), ("BB", QX[hh * 2 + dc], tt)], writes=[sk], signal=(dc == 1))
                        pi, pap, pk_ = self.tb_get()
                        E("act", "activation", pap, sps[:, :], AF.Exp, scale=1.0 / 16, reads=[sk], writes=[pk_])
                        self.psp.free(si)
                        P.append((pi, pap, pk_))
                    di, dps, dk = self.ps_get()
                    for mb in range(2):
                        E("pe", "matmul", dps[:, :], ones, P[mb][1], start=(mb == 0), stop=(mb == 1), reads=[P[mb][2], "CB"], writes=[dk], signal=(mb == 1))
                    ri, rap, rk = self.tp_get()
                    E("act", "activation", rap, dps[:, :], AF.Ln, reads=[dk], writes=[rk])
                    E("act", "activation", rap, rap, AF.Exp, scale=-1.0, reads=[rk], writes=[rk])
                    self.psp.free(di)
                    for vc in range(2):
                        oi, ops_, ok = self.ps_get()
                        col = h * 256 + vc * 128
                        for mb in range(2):
                            E("pe", "matmul", ops_[:, :], self.VX[:, mb, col:col + 128], P[mb][1], start=(mb == 0), stop=(mb == 1),
                              reads=[P[mb][2], ("VX", col // 128)], writes=[ok], signal=(mb == 1))
                        E("dve", "tensor_tensor", self.BB[:, OX[hh * 2 + vc], sl], ops_[:, :], rap, ALU.mult, reads=[ok, rk],
                          writes=[("BB", OX[hh * 2 + vc], tt)])
                        self.psp.free(oi)
                    self.tpp.free(ri)
                    for mb in range(2):
                        self.tbp.free(P[mb][0])
            for x in QX:
                self.bbp.free(x)
            self.out_proj("cross_wo", l, hp * 512, OX)
            for x in OX:
                self.bbp.free(x)


_CACHE = {}


def _get_nc(nseq=SEQ_PER_CORE, depth=DEPTH, stop=None, dbg=None, mixsel="abcd"):
    key = (nseq, depth, stop, dbg, mixsel)
    if key not in _CACHE:
        b = Builder(nseq, depth, stop, dbg, mixsel)
        _CACHE[key] = (b.build(), b)
    return _CACHE[key]


def kernel(**inputs):
    nc, _ = _get_nc()
    cf, cb = _consts()
    pv = _pack_params(inputs)
    x = np.ascontiguousarray(np.asarray(inputs["x"], np.float32))
    mem = np.ascontiguousarray(np.asarray(inputs["mem"], np.float32))
    pos = np.ascontiguousarray(np.asarray(inputs["positions"], np.int32))
    shared = {n: np.ascontiguousarray(np.asarray(inputs[n], np.float32)) for n in WNAMES}
    shared["gla_gate_w"] = np.ascontiguousarray(np.asarray(inputs["gla_gate_w"], np.float32))
    shared["pv"] = pv
    shared["cf"] = cf
    shared["cb"] = cb
    in_maps = []
    for c in range(NCORES):
        m = dict(shared)
        sl = slice(c * SEQ_PER_CORE, (c + 1) * SEQ_PER_CORE)
        m["x"] = x[sl]
        m["mem"] = mem[sl]
        m["pos"] = pos[sl]
        in_maps.append(m)
    res = run_bass_kernel_spmd(nc, in_maps, core_ids=list(range(NCORES)))
    return np.concatenate([r["y"] for r in res.results], axis=0)
```
